# Optimizing a Trainium2 kernel written in Bass

```python
import math
import jax, jax.numpy as jnp
from jax import lax
import numpy as np

D_MODEL = 1024
BATCH = 8
SEQ = 4096
DEPTH = 2

N_MIXERS = 2
N_CONV_LAYERS = (DEPTH + 1) // 2
N_ATTN_LAYERS = DEPTH // 2
CONV_CH = D_MODEL
CONV_WIDTH = 31
DA_HEADS = D_MODEL // 128
DA_HEAD_DIM = 64
DA_V_DIM = 2 * DA_HEAD_DIM
ROPE_DIM = DA_HEAD_DIM // 4
ROPE_THETA = 500000.0
Q_BLOCK = 128
N_GROUPS = 4
EXPERTS_PER_GROUP = 8
N_EXPERTS = N_GROUPS * EXPERTS_PER_GROUP
TOP_K_IN_GROUP = 2
D_EXPERT = D_MODEL // 4
PLE_DIM = 256
EPS = 1e-6

kernel_name = "hybrid_conformer_diffattn_hmoe"


def rms_norm(x, g):
    xf = x.astype(jnp.float32)
    y = xf * lax.rsqrt(jnp.mean(xf * xf, axis=-1, keepdims=True) + EPS)
    return (y * g.astype(jnp.float32)).astype(x.dtype)


def layer_norm(x, g, b):
    xf = x.astype(jnp.float32)
    mu = jnp.mean(xf, axis=-1, keepdims=True)
    var = jnp.mean(jnp.square(xf - mu), axis=-1, keepdims=True)
    y = (xf - mu) * lax.rsqrt(var + EPS)
    return (y * g.astype(jnp.float32) + b.astype(jnp.float32)).astype(x.dtype)


def conformer_conv(h, w_pw1, b_pw1, w_dw, b_dw, ln_g, ln_b, w_pw2, b_pw2):
    u = h @ w_pw1 + b_pw1
    a, g = jnp.split(u, 2, axis=-1)
    v = a * jax.nn.sigmoid(g)
    v = lax.conv_general_dilated(
        v, w_dw[:, None, :], window_strides=(1,),
        padding=[(CONV_WIDTH - 1, 0)],
        dimension_numbers=("NWC", "WIO", "NWC"),
        feature_group_count=CONV_CH) + b_dw
    v = jax.nn.silu(layer_norm(v, ln_g, ln_b))
    return v @ w_pw2 + b_pw2


def rope_tables(positions):
    inv_freq = ROPE_THETA ** (-jnp.arange(0, ROPE_DIM, 2, dtype=jnp.float32) / ROPE_DIM)
    ang = positions.astype(jnp.float32)[..., None] * inv_freq
    return jnp.cos(ang)[:, :, None, None, :], jnp.sin(ang)[:, :, None, None, :]


def partial_rope(x, cos, sin):
    half = ROPE_DIM // 2
    x1 = x[..., :half].astype(jnp.float32)
    x2 = x[..., half:ROPE_DIM].astype(jnp.float32)
    rot = jnp.concatenate([x1 * cos - x2 * sin, x2 * cos + x1 * sin], axis=-1)
    return jnp.concatenate([rot.astype(x.dtype), x[..., ROPE_DIM:]], axis=-1)


def diff_attention(h, cos, sin, w_qkv, lam_params, subln_g, w_o, lam_init):
    B, S, _ = h.shape
    qkv = h @ w_qkv
    nq = DA_HEADS * 2 * DA_HEAD_DIM
    q = qkv[..., :nq].reshape(B, S, DA_HEADS, 2, DA_HEAD_DIM)
    k = qkv[..., nq:2 * nq].reshape(B, S, DA_HEADS, 2, DA_HEAD_DIM)
    v = qkv[..., 2 * nq:].reshape(B, S, DA_HEADS, DA_V_DIM)
    q = partial_rope(q, cos, sin)
    k = partial_rope(k, cos, sin)
    lp = lam_params.astype(jnp.float32)
    lam = jnp.exp(jnp.sum(lp[0] * lp[1])) - jnp.exp(jnp.sum(lp[2] * lp[3])) + lam_init
    scale = DA_HEAD_DIM ** -0.5
    neg = jnp.finfo(jnp.float32).min
    outs = []
    for blk in range(S // Q_BLOCK):
        q0 = blk * Q_BLOCK
        kend = q0 + Q_BLOCK
        qb = q[:, q0:kend]
        kb = k[:, :kend]
        vb = v[:, :kend]
        s = jnp.einsum("bqhcd,bkhcd->bhcqk", qb, kb).astype(jnp.float32) * scale
        mask = (q0 + jnp.arange(Q_BLOCK))[:, None] >= jnp.arange(kend)[None, :]
        a = jax.nn.softmax(jnp.where(mask, s, neg), axis=-1)
        wgt = (a[:, :, 0] - lam * a[:, :, 1]).astype(vb.dtype)
        outs.append(jnp.einsum("bhqk,bkhd->bqhd", wgt, vb))
    o = jnp.concatenate(outs, axis=1)
    o = rms_norm(o, subln_g) * (1.0 - lam_init)
    return o.reshape(B, S, DA_HEADS * DA_V_DIM) @ w_o


def hier_moe(h, w_rg, b_rg, w_re, b_re, w_gate, w_up, w_down):
    gl = (h @ w_rg).astype(jnp.float32) + b_rg
    g_idx = jnp.argmax(gl, axis=-1)
    g_oh = jax.nn.one_hot(g_idx, N_GROUPS, dtype=jnp.float32)
    g_w = jnp.sum(jax.nn.softmax(gl, axis=-1) * g_oh, axis=-1, keepdims=True)
    el = jnp.einsum("bsd,gde->bsge", h, w_re).astype(jnp.float32) + b_re
    el_sel = jnp.einsum("bsge,bsg->bse", el, g_oh)
    top_v, top_i = lax.top_k(el_sel, TOP_K_IN_GROUP)
    top_w = jax.nn.softmax(top_v, axis=-1) * g_w
    e_oh = jax.nn.one_hot(top_i, EXPERTS_PER_GROUP, dtype=jnp.float32)
    within = jnp.sum(e_oh * top_w[..., None], axis=-2)
    combine = (g_oh[..., :, None] * within[..., None, :]).astype(h.dtype)
    out = jnp.zeros_like(h)
    for g in range(N_GROUPS):
        hg = jnp.einsum("bsd,edf->bsef", h, w_gate[g])
        ug = jnp.einsum("bsd,edf->bsef", h, w_up[g])
        act = jax.nn.silu(hg) * ug * combine[:, :, g, :, None]
        out = out + jnp.einsum("bsef,efd->bsd", act, w_down[g])
    return out


def setup_inputs(seed: int = 0) -> dict:
    key = jax.random.key(seed)
    ks = iter(jax.random.split(key, 40))
    nrm = lambda shape, scale: jax.random.normal(next(ks), shape, jnp.float32) * scale
    gain = lambda shape: 1.0 + nrm(shape, 0.02)
    D, C = D_MODEL, CONV_CH
    nc, na = N_CONV_LAYERS, N_ATTN_LAYERS
    nq = DA_HEADS * 2 * DA_HEAD_DIM
    G, E, F = N_GROUPS, EXPERTS_PER_GROUP, D_EXPERT
    x = jax.random.normal(next(ks), (BATCH, SEQ, D), jnp.float32)
    p = jax.random.normal(next(ks), (DEPTH, BATCH, SEQ, PLE_DIM), jnp.float32)
    offs = jax.random.randint(next(ks), (BATCH, 1), 0, 1024, dtype=jnp.int32)
    positions = offs + jnp.arange(SEQ, dtype=jnp.int32)[None, :]
    return {
        "x": x, "p": p, "positions": positions,
        "norm_mix": gain((DEPTH, D)), "norm_ffn": gain((DEPTH, D)),
        "conv_w_pw1": nrm((nc, D, 2 * C), D ** -0.5), "conv_b_pw1": nrm((nc, 2 * C), 0.01),
        "conv_w_dw": nrm((nc, CONV_WIDTH, C), CONV_WIDTH ** -0.5), "conv_b_dw": nrm((nc, C), 0.01),
        "conv_ln_g": gain((nc, C)), "conv_ln_b": nrm((nc, C), 0.01),
        "conv_w_pw2": nrm((nc, C, D), C ** -0.5), "conv_b_pw2": nrm((nc, D), 0.01),
        "da_w_qkv": nrm((na, D, 2 * nq + DA_HEADS * DA_V_DIM), D ** -0.5),
        "da_lambda": nrm((na, 4, DA_HEAD_DIM), 0.1),
        "da_subln": gain((na, DA_V_DIM)),
        "da_w_o": nrm((na, DA_HEADS * DA_V_DIM, D), (DA_HEADS * DA_V_DIM) ** -0.5),
        "moe_w_rg": nrm((DEPTH, D, G), D ** -0.5), "moe_b_rg": nrm((DEPTH, G), 0.01),
        "moe_w_re": nrm((DEPTH, G, D, E), D ** -0.5), "moe_b_re": nrm((DEPTH, G, E), 0.01),
        "moe_w_gate": nrm((DEPTH, G, E, D, F), D ** -0.5),
        "moe_w_up": nrm((DEPTH, G, E, D, F), D ** -0.5),
        "moe_w_down": nrm((DEPTH, G, E, F, D), F ** -0.5),
        "ple_norm": gain((DEPTH, D)),
        "ple_w_gate": nrm((DEPTH, D, D), D ** -0.5),
        "ple_w_proj": nrm((DEPTH, PLE_DIM, D), PLE_DIM ** -0.5),
        "final_norm": gain((D,)),
    }


def reference(x, p, positions, norm_mix, norm_ffn,
              conv_w_pw1, conv_b_pw1, conv_w_dw, conv_b_dw, conv_ln_g, conv_ln_b,
              conv_w_pw2, conv_b_pw2,
              da_w_qkv, da_lambda, da_subln, da_w_o,
              moe_w_rg, moe_b_rg, moe_w_re, moe_b_re, moe_w_gate, moe_w_up, moe_w_down,
              ple_norm, ple_w_gate, ple_w_proj, final_norm):
    cos, sin = rope_tables(positions)
    for i in range(DEPTH):
        j = i // N_MIXERS
        h = rms_norm(x, norm_mix[i])
        if i % N_MIXERS == 0:
            y = conformer_conv(h, conv_w_pw1[j], conv_b_pw1[j], conv_w_dw[j], conv_b_dw[j],
                               conv_ln_g[j], conv_ln_b[j], conv_w_pw2[j], conv_b_pw2[j])
        else:
            lam_init = 0.8 - 0.6 * math.exp(-0.3 * i)
            y = diff_attention(h, cos, sin, da_w_qkv[j], da_lambda[j], da_subln[j],
                               da_w_o[j], lam_init)
        x = x + y
        h = rms_norm(x, norm_ffn[i])
        x = x + hier_moe(h, moe_w_rg[i], moe_b_rg[i], moe_w_re[i], moe_b_re[i],
                         moe_w_gate[i], moe_w_up[i], moe_w_down[i])
        gate = jax.nn.sigmoid(rms_norm(x, ple_norm[i]) @ ple_w_gate[i])
        x = x + gate * (p[i] @ ple_w_proj[i])
    return rms_norm(x, final_norm)
```

```python
import numpy as np
import math
from contextlib import ExitStack
import concourse.bass as bass
import concourse.mybir as mybir
from concourse.alu_op_type import AluOpType as ALU
from concourse.bass_utils import run_bass_kernel_spmd

F32 = mybir.dt.float32
BF16 = mybir.dt.bfloat16
I32 = mybir.dt.int32
U32 = mybir.dt.uint32
AF = mybir.ActivationFunctionType
AX = mybir.AxisListType

D = 1024
KC = 8
P = 128
CW = 31
NG = 4
NE = 8
NEXP = 32
FE = 256
PLE = 256
EPS = 1e-6
NH = 8
DH = 64
DV = 128
ROPE = 16
THETA = 500000.0
NDSEM = 8


class Res:
    __slots__ = ("name", "w", "rd", "excl")

    def __init__(self, name="", excl=False):
        self.name = name
        self.w = {}
        self.rd = {}
        self.excl = excl


class Sched:
    def __init__(self, nc, es):
        self.nc = nc
        self.E = {"pe": nc.tensor, "dve": nc.vector, "act": nc.scalar, "pool": nc.gpsimd, "sp": nc.sync}
        self.sems = []
        self.esem = {}
        self.cnt = {}
        self.seen = {}
        for e in self.E:
            self.esem[e] = len(self.sems)
            self.sems.append(es.enter_context(nc.semaphore("s_" + e)))
            self.cnt[e] = 0
            self.seen[e] = {}
        self.dsem = {}
        self.dnext = {}
        self.dval = {}
        for q in ("sp", "pool", "act"):
            self.dsem[q] = []
            for i in range(NDSEM):
                self.dsem[q].append(len(self.sems))
                self.dval[len(self.sems)] = 0
                self.sems.append(es.enter_context(nc.semaphore("d_%s%d" % (q, i))))
            self.dnext[q] = 0
        self.ninst = 0

    def _wait(self, eng, s, v):
        if v <= 0 or self.seen[eng].get(s, 0) >= v:
            return
        self.E[eng].wait_ge(self.sems[s], v)
        self.seen[eng][s] = v

    def _deps(self, eng, rd, wr, own):
        for r in rd:
            for s, v in r.w.items():
                self._wait(eng, s, v)
            if r.excl:
                for s, v in r.rd.items():
                    if s != own:
                        self._wait(eng, s, v)
        pe_own = self.esem["pe"]
        for w in wr:
            for s, v in w.w.items():
                if s != own or own != pe_own:
                    self._wait(eng, s, v)
            for s, v in w.rd.items():
                if s != own or own != pe_own:
                    self._wait(eng, s, v)

    def _mark(self, tok, rd, wr):
        s, v = tok
        for r in rd:
            if r.rd.get(s, 0) < v:
                r.rd[s] = v
        for w in wr:
            if w.w.get(s, 0) < v:
                w.w[s] = v

    def op(self, eng, fn, rd=(), wr=(), inc=True):
        own = self.esem[eng]
        self._deps(eng, rd, wr, own)
        ins = fn()
        self.ninst += 1
        if inc:
            self.cnt[eng] += 1
            ins.then_inc(self.sems[own], 1)
            tok = (own, self.cnt[eng])
        else:
            tok = (own, self.cnt[eng] + 1)
        self._mark(tok, rd, wr)
        return tok

    def dma(self, q, out, in_, rd=(), wr=(), **kw):
        self._deps(q, rd, wr, -1)
        i = self.dnext[q]
        self.dnext[q] = (i + 1) % NDSEM
        s = self.dsem[q][i]
        self._wait(q, s, self.dval[s])
        self.dval[s] += 16
        self.E[q].dma_start(out=out, in_=in_, **kw).then_inc(self.sems[s], 16)
        self.ninst += 1
        tok = (s, self.dval[s])
        self._mark(tok, rd, wr)
        return tok

    def dma_indirect(self, fn, rd=(), wr=()):
        q = "pool"
        self._deps(q, rd, wr, -1)
        i = self.dnext[q]
        self.dnext[q] = (i + 1) % NDSEM
        s = self.dsem[q][i]
        self._wait(q, s, self.dval[s])
        self.dval[s] += 16
        fn(None).then_inc(self.sems[s], 16)
        self.ninst += 1
        tok = (s, self.dval[s])
        self._mark(tok, rd, wr)
        return tok

    def barrier(self):
        for x in self.E:
            for e in self.E:
                if e != x or True:
                    self._wait(x, self.esem[e], self.cnt[e])
            for s, v in self.dval.items():
                self._wait(x, s, v)

    def final_wait(self, eng="sp"):
        for e in self.E:
            self._wait(eng, self.esem[e], self.cnt[e])
        for s, v in self.dval.items():
            self._wait(eng, s, v)


def build_program(S, layer_kinds, lam_inits, debug=None):
    NT = S // P
    NM = S // 512
    L = len(layer_kinds)
    nconv = sum(1 for k_ in layer_kinds if k_ == "conv")
    nattn = L - nconv
    nc = bass.Bass("TRN2", target_bir_lowering=False)

    def din(name, shape, dt=F32):
        return nc.dram_tensor(name, list(shape), dt, kind="ExternalInput").ap()

    def dscr(name, shape, dt):
        return nc.dram_tensor(name, list(shape), dt, kind=("ExternalOutput" if debug else "Internal")).ap()

    x_in = din("x", [S, D])
    p_in = din("p", [L, S, PLE])
    pos_in = din("positions", [NT, P], I32)
    norm_mix = din("norm_mix", [L, D])
    norm_ffn = din("norm_ffn", [L, D])
    c_w1 = din("conv_w_pw1", [max(nconv, 1), D, 2 * D])
    c_b1 = din("conv_b_pw1", [max(nconv, 1), 2 * D])
    c_wdw = din("conv_w_dw", [max(nconv, 1), CW, D])
    c_bdw = din("conv_b_dw", [max(nconv, 1), D])
    c_lng = din("conv_ln_g", [max(nconv, 1), D])
    c_lnb = din("conv_ln_b", [max(nconv, 1), D])
    c_w2 = din("conv_w_pw2", [max(nconv, 1), D, D])
    c_b2 = din("conv_b_pw2", [max(nconv, 1), D])
    a_wqkv = din("da_w_qkv", [max(nattn, 1), D, 3 * D])
    a_lam = din("da_lambda", [max(nattn, 1), 4 * DH])
    a_subln = din("da_subln", [max(nattn, 1), DV])
    a_wo = din("da_w_o", [max(nattn, 1), D, D])
    m_wrg = din("moe_w_rg", [L, D, NG])
    m_brg = din("moe_b_rg", [L, NG])
    m_wre = din("moe_w_re", [L, NG, D, NE])
    m_bre = din("moe_b_re", [L, NG * NE])
    m_wg = din("moe_w_gate", [L, NEXP, D, FE])
    m_wu = din("moe_w_up", [L, NEXP, D, FE])
    m_wd = din("moe_w_down", [L, NEXP, FE, D])
    ple_norm = din("ple_norm", [L, D])
    ple_wg = din("ple_w_gate", [L, D, D])
    ple_wp = din("ple_w_proj", [L, PLE, D])
    fin_norm = din("final_norm", [D])
    y_out = nc.dram_tensor("y", [S, D], F32, kind="ExternalOutput").ap()
    xres = dscr("xres", [S, D], F32)
    vscr = dscr("vscr", [KC, P, 32 + S], BF16)
    hscr = dscr("hscr", [KC, P, S], BF16)
    qscr = dscr("qscr", [NH, P, S], BF16)
    kscr = dscr("kscr", [NH, P, S], BF16)
    vtok = dscr("vtok", [S, D], BF16)
    NTL = (2 * S) // P + NEXP
    NSLOT = NTL * P
    SPARSE = True
    wgb = [dscr("wgb%d" % l_, [NEXP * P, KC * FE], BF16) for l_ in range(L)]
    wub = [dscr("wub%d" % l_, [NEXP * P, KC * FE], BF16) for l_ in range(L)]
    wdb = [dscr("wdb%d" % l_, [NEXP * P, 2 * D], BF16) for l_ in range(L)]
    hsort = dscr("hsort", [NSLOT, D], BF16)
    ysort = dscr("ysort", [NSLOT, D], F32)
    dbg_lg = dscr("dbg_lg", [S, 256], F32) if debug else None

    es = ExitStack()
    with es:
        k = Sched(nc, es)

        uniq = [0]

        def sb(name, shape, dt, stack=None):
            uniq[0] += 1
            return (stack or es).enter_context(nc.sbuf_tensor("%s_%d" % (name, uniq[0]), list(shape), dt))

        ps = es.enter_context(nc.psum_tensor("ps", [P, 8, 512], F32))
        pbank = [Res("bank%d" % i, excl=True) for i in range(8)]

        ident_b = sb("ident_b", [P, P], BF16)
        ident_f = sb("ident_f", [P, P], F32)
        ones_b = sb("ones_b", [P, P], BF16)
        eps_col = sb("eps_col", [P, 1], F32)
        r_const = Res("const")
        k.op("pool", lambda: nc.gpsimd.memset(ident_f[:], 0.0), wr=[r_const])
        k.op("pool", lambda: nc.gpsimd.affine_select(out=ident_f[:], in_=ident_f[:], pattern=[[-1, P]],
                                                     compare_op=ALU.not_equal, fill=1.0, base=0, channel_multiplier=1),
             rd=[r_const], wr=[r_const])
        k.op("pool", lambda: nc.gpsimd.tensor_copy(out=ident_b[:], in_=ident_f[:]), rd=[r_const], wr=[r_const])
        k.op("pool", lambda: nc.gpsimd.memset(ones_b[:], 1.0), wr=[r_const])
        k.op("pool", lambda: nc.gpsimd.memset(eps_col[:], EPS), wr=[r_const])

        def load_featvec(name, rows_ap, nrows, stack=None):
            out = sb(name, [P, KC, 8], F32, stack)
            with ExitStack() as st:
                tmp = sb(name + "_tmp", [8, D], F32, st)
                r_t = Res(name)
                k.op("pool", lambda: nc.gpsimd.memset(tmp[:], 0.0), wr=[r_t])
                k.dma("sp", tmp[0:nrows, :], rows_ap, wr=[r_t])
                for c in range(KC):
                    bk = c % 2
                    k.op("pe", lambda: nc.tensor.transpose(out=ps[:, bk, 0:8], in_=tmp[:, c * P:(c + 1) * P],
                                                          identity=ident_f[0:8, 0:8]), rd=[r_const, r_t], wr=[pbank[bk]])
                    k.op("dve", lambda: nc.vector.tensor_copy(out=out[:, c, :], in_=ps[:, bk, 0:8]), rd=[pbank[bk]], wr=[r_const])
                k.barrier()
            return out

        g_mix = load_featvec("g_mix", norm_mix, L)
        g_ffn = load_featvec("g_ffn", norm_ffn, L)
        g_ple = load_featvec("g_ple", ple_norm, L)

        NSTG = 2
        stage_bufs = [sb("stage%d" % i, [P, 2048], F32) for i in range(NSTG)]
        stage_res = [Res("stage%d" % i) for i in range(NSTG)]
        stage_i = [0]
        cast_rr = [0]

        def cast(eng, out, in_, rd, wr, scale=None):
            if eng == "act":
                if scale is None:
                    k.op("act", lambda: nc.scalar.copy(out=out, in_=in_), rd=rd, wr=wr)
                else:
                    k.op("act", lambda: nc.scalar.activation(out=out, in_=in_, func=AF.Identity, scale=scale), rd=rd, wr=wr)
            elif eng == "pool":
                if scale is None:
                    k.op("pool", lambda: nc.gpsimd.tensor_copy(out=out, in_=in_), rd=rd, wr=wr)
                else:
                    k.op("pool", lambda: nc.gpsimd.tensor_scalar(out=out, in0=in_, scalar1=scale, scalar2=1.0,
                                                                 op0=ALU.mult, op1=ALU.mult), rd=rd, wr=wr)
            else:
                if scale is None:
                    k.op("dve", lambda: nc.vector.tensor_copy(out=out, in_=in_), rd=rd, wr=wr)
                else:
                    k.op("dve", lambda: nc.vector.tensor_scalar(out=out, in0=in_, scalar1=scale, scalar2=None,
                                                                op0=ALU.mult), rd=rd, wr=wr)

        def load_w_bf16(dst3, src2, kc, n, gain=None, r_dst=None, engines=("act", "pool", "dve")):
            if n > 2048:
                for n0 in range(0, n, 1024):
                    load_w_bf16(dst3[:, :, n0:n0 + 1024], src2[:, n0:n0 + 1024], kc, 1024, gain, r_dst, engines)
                return
            per = max(1, 2048 // n)
            c0 = 0
            while c0 < kc:
                cn = min(per, kc - c0)
                si = stage_i[0] % NSTG
                stage_i[0] += 1
                stv = stage_bufs[si][:, 0:cn * n].rearrange("p (c n) -> p c n", n=n)
                k.dma("sp", stv, src2[c0 * P:(c0 + cn) * P, :].rearrange("(c p) n -> p c n", p=P), wr=[stage_res[si]])
                eng = engines[cast_rr[0] % len(engines)]
                cast_rr[0] += 1
                if gain is None:
                    cast(eng, dst3[:, c0:c0 + cn, :], stv, [stage_res[si]], [r_dst])
                else:
                    for c in range(cn):
                        cast(eng, dst3[:, c0 + c, :], stv[:, c, :], [stage_res[si], r_const], [r_dst],
                             scale=gain[:, c0 + c:c0 + c + 1])
                c0 += cn

        junk = sb("junk", [P, D], BF16)
        r_junk = Res("junk")
        stat = sb("stat", [P, 64], F32)
        r_stat = [Res("stat%d" % i) for i in range(16)]
        stat_i = [0]

        nhalf = sb("nhalf", [P, 1], F32)
        k.op("pool", lambda: nc.gpsimd.memset(nhalf[:], -0.5), wr=[r_const])

        def rms_rstd(x_ap, r_x, n=D):
            i = stat_i[0] % 16
            stat_i[0] += 1
            ssq = stat[:, 4 * i:4 * i + 1]
            ms = stat[:, 4 * i + 1:4 * i + 2]
            rstd = stat[:, 4 * i + 2:4 * i + 3]
            r = r_stat[i]
            k.op("act", lambda: nc.scalar.activation(out=junk[:, 0:n], in_=x_ap, func=AF.Square, accum_out=ssq),
                 rd=[r_x], wr=[r_junk, r])
            k.op("dve", lambda: nc.vector.tensor_scalar(out=ms, in0=ssq, scalar1=1.0 / n, scalar2=EPS, op0=ALU.mult, op1=ALU.add),
                 rd=[r], wr=[r])
            k.op("pool", lambda: nc.gpsimd.tensor_tensor(out=rstd, in0=ms, in1=nhalf[:], op=ALU.pow), rd=[r, r_const], wr=[r])
            return rstd, r

        combT = sb("combT", [32, S], BF16)
        comb_tok = sb("comb_tok", [P, NT, 32], F32)
        r_ctok = Res("comb_tok")
        slotA_i = sb("slotA_i", [P, NT], I32)
        slotB_i = sb("slotB_i", [P, NT], I32)
        wAB = sb("wAB", [P, 2, NT], F32)
        r_route = Res("route")
        r_htok = Res("htok")
        r_ysort = Res("ysort")
        r_comb = [Res("comb%d" % i) for i in range(NM)]
        r_xres = [Res("xres%d" % i) for i in range(NM)]
        r_hscr = [Res("hscr%d" % i) for i in range(NM)]
        r_vscr = [Res("vscr%d" % i) for i in range(NM + 1)]

        def run_pipeline(n, stages):
            ns = len(stages)
            for step in range(n + ns - 1):
                for i, f in enumerate(stages):
                    t = step - i
                    if 0 <= t < n:
                        f(t)

        class HStage:
            def __init__(self, stack, flush=True):
                self.flush = flush
                self.buf = [sb("hstage%d" % i, [P, KC, 512], BF16, stack) for i in range(2)]
                self.res = [Res("hstage0"), Res("hstage1")]
                self.xnb = [sb("hs_xnb%d" % i, [P, D], BF16, stack) for i in range(2)]
                self.r_xnb = [Res("hs_xnb0"), Res("hs_xnb1")]

            def put_T(self, t, src_bf16, r_src, bank, evac_eng="act"):
                mt, sub = t // 4, t % 4
                i = mt % 2
                pb = ps[:, bank, :].bitcast(BF16)
                for c in range(KC):
                    k.op("pe", lambda: nc.tensor.transpose(out=pb[:, c * P:(c + 1) * P], in_=src_bf16[:, c * P:(c + 1) * P],
                                                          identity=ident_b[:]), rd=[r_src, r_const], wr=[pbank[bank]], inc=(c == KC - 1))
                dst = self.buf[i][:, :, sub * P:(sub + 1) * P]
                src = pb.rearrange("p (c n) -> p c n", n=P)
                if evac_eng == "act":
                    k.op("act", lambda: nc.scalar.copy(out=dst, in_=src), rd=[pbank[bank]], wr=[self.res[i]])
                else:
                    k.op("dve", lambda: nc.vector.tensor_copy(out=dst, in_=src), rd=[pbank[bank]], wr=[self.res[i]])
                if sub == 3 and self.flush:
                    k.dma("pool", hscr[:, :, mt * 512:(mt + 1) * 512].rearrange("c p t -> p c t"), self.buf[i][:],
                          rd=[self.res[i]], wr=[r_hscr[mt]])

            def norm_put(self, t, x_ap, r_x, bank):
                i = t % 2
                rstd, r_rs = rms_rstd(x_ap, r_x)
                k.op("dve", lambda: nc.vector.tensor_scalar(out=self.xnb[i][:], in0=x_ap, scalar1=rstd, scalar2=None, op0=ALU.mult),
                     rd=[r_x, r_rs], wr=[self.r_xnb[i]])
                self.put_T(t, self.xnb[i], self.r_xnb[i], bank)

        class MoePrep:
            def __init__(self, l, stack, banks, depth=1, rdepth=4):
                self.l = l
                self.banks = banks
                self.depth = depth
                self.hs = HStage(stack)
                self.wr_f = sb("wr_f", [P, KC, 36], F32, stack)
                self.r_wr = Res("wr")
                self.rb_bc = sb("rb_bc", [P, 36], F32, stack)
                tmp = sb("wr_tmp", [P, KC, 36], F32, stack)
                r_wr = self.r_wr
                with nc.allow_non_contiguous_dma(reason="tiny router weights"):
                    k.dma("sp", tmp[:, :, 0:NG], m_wrg[l].rearrange("(c p) n -> p c n", p=P), wr=[r_wr])
                    for g in range(NG):
                        k.dma("sp", tmp[:, :, NG + g * NE:NG + (g + 1) * NE], m_wre[l, g].rearrange("(c p) n -> p c n", p=P), wr=[r_wr])
                for c in range(KC):
                    k.op("dve", lambda: nc.vector.tensor_scalar(out=self.wr_f[:, c, :], in0=tmp[:, c, :], scalar1=g_ffn[:, c, l:l + 1],
                                                                scalar2=None, op0=ALU.mult), rd=[r_wr, r_const], wr=[r_wr])
                k.dma("sp", self.rb_bc[:, 0:NG], m_brg[l].partition_broadcast(P), wr=[r_wr])
                k.dma("sp", self.rb_bc[:, NG:36], m_bre[l].partition_broadcast(P), wr=[r_wr])
                self.xn_fs = [sb("xn_f%d" % i, [P, D], F32, stack) for i in range(depth)]
                self.r_xns = [Res("xn%d" % i) for i in range(depth)]
                self.hTfs = [sb("hTf%d" % i, [P, KC, P], F32, stack) for i in range(depth)]
                self.r_hTfs = [Res("hTf%d" % i) for i in range(depth)]
                self.rdepth = rdepth
                self.rs = {}
                self.split_logits = False
                self.rt = sb("rt", [P, self.rdepth, 256], F32, stack)
                self.r_rt = [Res("rt%d" % i) for i in range(self.rdepth)]
                self.ctr = 0

            def tile(self, x_ap, r_x, t):
                self.p1(x_ap, r_x, t)
                self.p2(t)

            def p1(self, x_ap, r_x, t):
                self.p1a(x_ap, r_x, t)
                self.p1b(t)

            def p1a(self, x_ap, r_x, t):
                self.p1a1(x_ap, r_x, t)
                self.p1a2(x_ap, r_x, t)

            def p1a1(self, x_ap, r_x, t):
                self.rs[t % 4] = rms_rstd(x_ap, r_x)

            def p1a2(self, x_ap, r_x, t):
                xn_f, r_xn = self.xn_fs[t % self.depth], self.r_xns[t % self.depth]
                rstd, r_rs = self.rs[t % 4]
                k.op("act", lambda: nc.scalar.activation(out=xn_f[:], in_=x_ap, func=AF.Identity, scale=rstd),
                     rd=[r_x, r_rs], wr=[r_xn])
                xb = self.hs.xnb[t % 2]
                r_xb = self.hs.r_xnb[t % 2]
                k.op("pool", lambda: nc.gpsimd.tensor_copy(out=xb[:], in_=xn_f[:]), rd=[r_xn], wr=[r_xb])
                if SPARSE:
                    k.dma("pool", vtok[t * P:(t + 1) * P, :], xb[:], rd=[r_xb], wr=[r_htok])

            def p1b(self, t):
                i = t % self.rdepth
                b0, b1, b2 = self.banks
                xn_f, r_xn = self.xn_fs[t % self.depth], self.r_xns[t % self.depth]
                hTf, r_hTf = self.hTfs[t % self.depth], self.r_hTfs[t % self.depth]
                xb = self.hs.xnb[t % 2]
                r_xb = self.hs.r_xnb[t % 2]
                self.hs.put_T(t, xb, r_xb, b0, evac_eng="dve")
                for c in range(KC):
                    bk = b1 if c < 4 else b2
                    k.op("pe", lambda: nc.tensor.transpose(out=ps[:, bk, (c % 4) * P:(c % 4 + 1) * P], in_=xn_f[:, c * P:(c + 1) * P],
                                                          identity=ident_f[:]), rd=[r_xn, r_const], wr=[pbank[bk]], inc=(c % 4 == 3))
                for h in range(2):
                    bk = b1 if h == 0 else b2
                    src = ps[:, bk, :].rearrange("p (c n) -> p c n", n=P)
                    k.op("act", lambda: nc.scalar.copy(out=hTf[:, 4 * h:4 * h + 4, :], in_=src), rd=[pbank[bk]], wr=[r_hTf])
                if self.split_logits:
                    return
                self.p1b2(t)

            def p1b2(self, t):
                i = t % self.rdepth
                b0, b1, b2 = self.banks
                hTf, r_hTf = self.hTfs[t % self.depth], self.r_hTfs[t % self.depth]
                for c in range(KC):
                    k.op("pe", lambda: nc.tensor.matmul(ps[:, b1, 0:36], lhsT=hTf[:, c, :], rhs=self.wr_f[:, c, :],
                                                       start=(c == 0), stop=(c == KC - 1)),
                         rd=[r_hTf, self.r_wr], wr=[pbank[b1]], inc=(c == KC - 1))
                R = self.rt[:, i, :]
                rr = self.r_rt[i]
                Lg = R[:, 0:36]
                gmax = R[:, 36:37]
                ngmax = R[:, 37:38]
                gsum = R[:, 38:39]
                gw = R[:, 39:40]
                goh = R[:, 40:44]
                gex = R[:, 44:48]
                top8 = R[:, 48:80]
                nt1 = R[:, 80:84]
                mask = R[:, 84:116]
                ex = R[:, 116:148]
                den = R[:, 148:152]
                coef = R[:, 152:156]
                comb = R[:, 160:192]
                V = nc.vector
                k.op("dve", lambda: V.tensor_tensor(out=Lg, in0=ps[:, b1, 0:36], in1=self.rb_bc[:], op=ALU.add),
                     rd=[pbank[b1], self.r_wr], wr=[rr])

            def p2(self, t):
                if t % 4 != 3:
                    return
                lists = [self.p2_ops(tt) for tt in range(t - 3, t + 1)]
                n = max(len(x) for x in lists)
                for j in range(n):
                    for lst in lists:
                        if j < len(lst):
                            lst[j]()

            def p2_ops(self, t):
                i = t % self.rdepth
                mt = t // 4
                col0 = t * P
                b0, b1, b2 = self.banks
                R = self.rt[:, i, :]
                rr = self.r_rt[i]
                Lg = R[:, 0:36]
                gmax = R[:, 36:37]
                ngmax = R[:, 37:38]
                gsum = R[:, 38:39]
                gw = R[:, 39:40]
                goh = R[:, 40:44]
                gex = R[:, 44:48]
                top8 = R[:, 48:80]
                nt1 = R[:, 80:84]
                mask = R[:, 84:116]
                ex = R[:, 116:148]
                den = R[:, 148:152]
                coef = R[:, 152:156]
                comb = R[:, 160:192]
                V = nc.vector
                ops = []
                A = ops.append
                A(lambda: k.op("dve", lambda: V.tensor_reduce(out=gmax, in_=Lg[:, 0:NG], axis=AX.X, op=ALU.max), rd=[rr], wr=[rr]))
                A(lambda: k.op("dve", lambda: V.tensor_scalar(out=goh, in0=Lg[:, 0:NG], scalar1=gmax, scalar2=None, op0=ALU.is_equal),
                               rd=[rr], wr=[rr]))
                A(lambda: k.op("dve", lambda: V.tensor_scalar(out=ngmax, in0=gmax, scalar1=-1.0, scalar2=None, op0=ALU.mult), rd=[rr], wr=[rr]))
                A(lambda: k.op("act", lambda: nc.scalar.activation(out=gex, in_=Lg[:, 0:NG], func=AF.Exp, bias=ngmax, accum_out=gsum),
                               rd=[rr], wr=[rr]))

                def top_all():
                    for g in range(NG):
                        k.op("dve", lambda: V.max(out=top8[:, g * 8:(g + 1) * 8], in_=Lg[:, NG + g * NE:NG + (g + 1) * NE]), rd=[rr], wr=[rr])
                A(top_all)
                t8 = top8.rearrange("p (g e) -> p g e", e=8)
                A(lambda: k.op("dve", lambda: V.tensor_scalar(out=nt1, in0=t8[:, :, 0], scalar1=-1.0, scalar2=None, op0=ALU.mult), rd=[rr], wr=[rr]))

                def mask_all():
                    for g in range(NG):
                        le = Lg[:, NG + g * NE:NG + (g + 1) * NE]
                        k.op("dve", lambda: V.tensor_scalar(out=mask[:, g * 8:(g + 1) * 8], in0=le, scalar1=top8[:, g * 8 + 1:g * 8 + 2],
                                                            scalar2=None, op0=ALU.is_ge), rd=[rr], wr=[rr])
                A(mask_all)

                def ex_all():
                    for g in range(NG):
                        le = Lg[:, NG + g * NE:NG + (g + 1) * NE]
                        k.op("act", lambda: nc.scalar.activation(out=ex[:, g * 8:(g + 1) * 8], in_=le, func=AF.Exp, bias=nt1[:, g:g + 1]),
                             rd=[rr], wr=[rr])
                A(ex_all)
                A(lambda: k.op("dve", lambda: V.reciprocal(out=gw, in_=gsum), rd=[rr], wr=[rr]))
                A(lambda: k.op("dve", lambda: V.tensor_tensor(out=ex, in0=ex, in1=mask, op=ALU.mult), rd=[rr], wr=[rr]))
                A(lambda: k.op("dve", lambda: V.tensor_reduce(out=den, in_=ex.rearrange("p (g e) -> p g e", e=8), axis=AX.X, op=ALU.add),
                               rd=[rr], wr=[rr]))
                A(lambda: k.op("dve", lambda: V.reciprocal(out=coef, in_=den), rd=[rr], wr=[rr]))
                A(lambda: k.op("dve", lambda: V.tensor_tensor(out=coef, in0=coef, in1=goh, op=ALU.mult), rd=[rr], wr=[rr]))
                A(lambda: k.op("dve", lambda: V.tensor_scalar(out=coef, in0=coef, scalar1=gw, scalar2=None, op0=ALU.mult), rd=[rr], wr=[rr]))

                def comb_all():
                    for g in range(NG):
                        k.op("dve", lambda: V.tensor_scalar(out=comb[:, g * 8:(g + 1) * 8], in0=ex[:, g * 8:(g + 1) * 8],
                                                            scalar1=coef[:, g:g + 1], scalar2=None, op0=ALU.mult), rd=[rr], wr=[rr])
                A(comb_all)
                if SPARSE:
                    A(lambda: k.op("pool", lambda: nc.gpsimd.tensor_copy(out=comb_tok[:, t, :], in_=comb), rd=[rr], wr=[r_ctok]))

                def fin():
                    k.op("pe", lambda: nc.tensor.transpose(out=ps[0:32, b2, 0:P], in_=comb, identity=ident_f[:]),
                         rd=[rr, r_const], wr=[pbank[b2]])
                    if debug:
                        k.dma("pool", dbg_lg[t * P:(t + 1) * P, 0:36], R[:, 0:36], rd=[rr], wr=[Res("dbg")])
                    k.op("dve", lambda: V.tensor_copy(out=combT[:, col0:col0 + P], in_=ps[0:32, b2, 0:P]),
                         rd=[pbank[b2]], wr=[r_comb[mt]])
                A(fin)
                return ops

        EC = 4

        def moe_dense(l, pstack):
            sel = sb("sel", [32, NEXP, P], BF16, pstack)
            r_sel = Res("sel")
            k.op("pool", lambda: nc.gpsimd.memset(sel[:], 0.0), wr=[r_sel])
            k.op("pool", lambda: nc.gpsimd.affine_select(out=sel[:], in_=sel[:], pattern=[[-1, NEXP], [0, P]],
                                                         compare_op=ALU.not_equal, fill=1.0, base=0, channel_multiplier=1),
                 rd=[r_sel], wr=[r_sel])
            wg = [sb("wg%d" % i, [P, EC, KC, FE], BF16, pstack) for i in range(2)]
            wu = [sb("wu%d" % i, [P, EC, KC, FE], BF16, pstack) for i in range(2)]
            wd = [sb("wd%d" % i, [P, EC, 2, D], BF16, pstack) for i in range(2)]
            r_w = [Res("moew0"), Res("moew1")]
            hT = [sb("mhT%d" % i, [P, KC, 512], BF16, pstack) for i in range(2)]
            r_hT = [Res("mhT0"), Res("mhT1")]
            act = [sb("actb%d" % i, [P, EC, 2, 512], BF16, pstack) for i in range(2)]
            r_act = [Res("act0"), Res("act1")]
            cmb_sb = [sb("cmb_sb%d" % i, [P, 512], BF16, pstack) for i in range(2)]
            r_cmb = [Res("cmb0"), Res("cmb1")]
            sg = [sb("sg%d" % i, [P, 512], BF16, pstack) for i in range(3)]
            r_sg = [Res("sg%d" % i) for i in range(3)]
            ub = [sb("ub%d" % i, [P, 512], BF16, pstack) for i in range(3)]
            r_ub = [Res("ub%d" % i) for i in range(3)]
            ob = [sb("ob%d" % i, [P, D], F32, pstack) for i in range(3)]
            r_ob = [Res("ob%d" % i) for i in range(3)]
            nchunk = NEXP // EC

            def chunk_jobs(ci):
                bi = ci % 2
                jobs = []
                for e in range(EC):
                    eg = ci * EC + e
                    for which in range(3):
                        def mk(e=e, eg=eg, which=which):
                            st = {}

                            def dma():
                                si = stage_i[0] % NSTG
                                stage_i[0] += 1
                                st["si"] = si
                                if which < 2:
                                    src = (m_wg if which == 0 else m_wu)[l, eg]
                                    stv = stage_bufs[si][:, 0:KC * FE].rearrange("p (c n) -> p c n", n=FE)
                                else:
                                    src = m_wd[l, eg]
                                    stv = stage_bufs[si][:, 0:2 * D].rearrange("p (c n) -> p c n", n=D)
                                st["stv"] = stv
                                k.dma("sp", stv, src.rearrange("(c p) n -> p c n", p=P), wr=[stage_res[si]])

                            def cst():
                                si, stv = st["si"], st["stv"]
                                if which < 2:
                                    dst = (wg if which == 0 else wu)[bi][:, e]
                                    for c in range(KC):
                                        eng = "act" if which == 0 else ("pool" if c % 2 == 0 else "dve")
                                        cast(eng, dst[:, c, :], stv[:, c, :], [stage_res[si], r_const], [r_w[bi]], scale=g_ffn[:, c, l:l + 1])
                                else:
                                    cast("dve", wd[bi][:, e, 0, :], stv[:, 0, :], [stage_res[si]], [r_w[bi]])
                                    cast("act", wd[bi][:, e, 1, :], stv[:, 1, :], [stage_res[si]], [r_w[bi]])
                            return dma, cst
                        jobs.append(mk())
                return jobs

            cnt3 = [0]
            ocnt = [0]
            dcnt = [0]
            hcnt = [0]
            for d_, c_ in chunk_jobs(0):
                d_()
                c_()
            nslots = NM * EC
            for ci in range(nchunk):
                bi = ci % 2
                jobs = chunk_jobs(ci + 1) if ci + 1 < nchunk else []
                sched_d = {}
                sched_c = {}
                for j, (d_, c_) in enumerate(jobs):
                    sd = (j * nslots) // len(jobs)
                    sc = min(nslots - 1, sd + 1)
                    sched_d.setdefault(sd, []).append(d_)
                    sched_c.setdefault(sc, []).append(c_)
                for I in range(NM):
                    hi = hcnt[0] % 2
                    hcnt[0] += 1
                    k.dma("sp", hT[hi][:], hscr[:, :, I * 512:(I + 1) * 512].rearrange("c p t -> p c t"), rd=[r_hscr[I]], wr=[r_hT[hi]])
                    ai = (ci * NM + I) % 2
                    tok = slice(I * 512, (I + 1) * 512)
                    for e in range(EC):
                        slot = I * EC + e
                        late = []
                        for c_ in sched_c.get(slot, []):
                            try:
                                c_()
                            except KeyError:
                                late.append(c_)
                        for d_ in sched_d.get(slot, []):
                            d_()
                        for c_ in late:
                            c_()
                        eg = ci * EC + e
                        ci2 = (ci * NM * EC + I * EC + e) % 2
                        k.op("pe", lambda: nc.tensor.matmul(ps[:, 0, :], lhsT=sel[:, eg, :], rhs=combT[:, tok], start=True, stop=True),
                             rd=[r_sel, r_comb[I]], wr=[pbank[0]])
                        k.op("act", lambda: nc.scalar.copy(out=cmb_sb[ci2][:], in_=ps[:, 0, :]), rd=[pbank[0]], wr=[r_cmb[ci2]])
                        for fc in range(2):
                            j3 = cnt3[0] % 3
                            gset = cnt3[0] % 2
                            cnt3[0] += 1
                            bg = 1 + 2 * gset
                            bu = 2 + 2 * gset
                            for c in range(KC):
                                k.op("pe", lambda: nc.tensor.matmul(ps[:, bg, :], lhsT=wg[bi][:, e, c, fc * P:(fc + 1) * P],
                                                                   rhs=hT[hi][:, c, :], start=(c == 0), stop=(c == KC - 1)),
                                     rd=[r_w[bi], r_hT[hi]], wr=[pbank[bg]], inc=(c == KC - 1))
                            for c in range(KC):
                                k.op("pe", lambda: nc.tensor.matmul(ps[:, bu, :], lhsT=wu[bi][:, e, c, fc * P:(fc + 1) * P],
                                                                   rhs=hT[hi][:, c, :], start=(c == 0), stop=(c == KC - 1)),
                                     rd=[r_w[bi], r_hT[hi]], wr=[pbank[bu]], inc=(c == KC - 1))
                            k.op("act", lambda: nc.scalar.activation(out=sg[j3][:], in_=ps[:, bg, :], func=AF.Silu),
                                 rd=[pbank[bg]], wr=[r_sg[j3]])
                            k.op("dve", lambda: nc.vector.tensor_tensor(out=ub[j3][:], in0=ps[:, bu, :], in1=cmb_sb[ci2][:], op=ALU.mult),
                                 rd=[pbank[bu], r_cmb[ci2]], wr=[r_ub[j3]])
                            k.op("pool", lambda: nc.gpsimd.tensor_tensor(out=act[ai][:, e, fc, :], in0=ub[j3][:], in1=sg[j3][:], op=ALU.mult),
                                 rd=[r_ub[j3], r_sg[j3]], wr=[r_act[ai]])
                    for sub in range(4):
                        oi = ocnt[0] % 3
                        ocnt[0] += 1
                        for half in range(2):
                            bd = 5 + dcnt[0] % 2
                            dcnt[0] += 1
                            n = 0
                            for e in range(EC):
                                for fc in range(2):
                                    k.op("pe", lambda: nc.tensor.matmul(ps[:, bd, :], lhsT=act[ai][:, e, fc, sub * P:(sub + 1) * P],
                                                                       rhs=wd[bi][:, e, fc, half * 512:(half + 1) * 512],
                                                                       start=(n == 0), stop=(n == 2 * EC - 1)),
                                         rd=[r_act[ai], r_w[bi]], wr=[pbank[bd]], inc=(n == 2 * EC - 1))
                                    n += 1
                            if half == 0:
                                k.op("dve", lambda: nc.vector.tensor_copy(out=ob[oi][:, 0:512], in_=ps[:, bd, :]),
                                     rd=[pbank[bd]], wr=[r_ob[oi]])
                            else:
                                k.op("act", lambda: nc.scalar.copy(out=ob[oi][:, 512:1024], in_=ps[:, bd, :]),
                                     rd=[pbank[bd]], wr=[r_ob[oi]])
                        r0 = I * 512 + sub * P
                        k.dma("pool", xres[r0:r0 + P, :], ob[oi][:], rd=[r_ob[oi]], wr=[r_xres[I]], accum_op=ALU.add)

        def make_prepass(l, pstack, engines=("act", "dve", "pool"), NPS=4, NWB=4):
            wb = [sb("wb%d" % i, [P, 2048], BF16, pstack) for i in range(NWB)]
            r_wb = [Res("wb%d" % i) for i in range(NWB)]
            pst = [sb("pst%d" % i, [P, 2048], F32, pstack) for i in range(NPS)]
            r_pst = [Res("pst%d" % i) for i in range(NPS)]
            jobs = [(e, which) for e in range(NEXP) for which in range(3)]
            state = {"n": 0, "pend": []}

            def issue(n):
                e, which = jobs[n]
                si = n % NPS
                if which < 2:
                    src = (m_wg if which == 0 else m_wu)[l, e]
                    stv = pst[si][:, 0:KC * FE].rearrange("p (c n) -> p c n", n=FE)
                else:
                    src = m_wd[l, e]
                    stv = pst[si][:, 0:2 * D].rearrange("p (c n) -> p c n", n=D)
                k.dma("sp", stv, src.rearrange("(c p) n -> p c n", p=P), wr=[r_pst[si]])
                return (n, si, stv)

            def finish(n, si, stv):
                e, which = jobs[n]
                bi = n % NWB
                if which < 2:
                    dstv = wb[bi][:].rearrange("p (c n) -> p c n", n=FE)
                    for c in range(KC):
                        eng = engines[(n + c) % len(engines)]
                        cast(eng, dstv[:, c, :], stv[:, c, :], [r_pst[si], r_const], [r_wb[bi]], scale=g_ffn[:, c, l:l + 1])
                else:
                    dstv = wb[bi][:].rearrange("p (c n) -> p c n", n=D)
                    cast(engines[n % len(engines)], dstv[:, 0, :], stv[:, 0, :], [r_pst[si]], [r_wb[bi]])
                    cast(engines[(n + 1) % len(engines)], dstv[:, 1, :], stv[:, 1, :], [r_pst[si]], [r_wb[bi]])
                dst = (wgb, wub, wdb)[which][l]
                k.dma("pool", dst[e * P:(e + 1) * P, :], wb[bi][:], rd=[r_wb[bi]], wr=[r_wdram])

            def emit(nslots):
                for _ in range(nslots):
                    if len(state["pend"]) >= 2 or (state["n"] >= len(jobs) and state["pend"]):
                        finish(*state["pend"].pop(0))
                    if state["n"] < len(jobs):
                        state["pend"].append(issue(state["n"]))
                        state["n"] += 1

            def flush():
                emit(len(jobs) + 3)
            emit.flush = flush
            return emit

        r_wdram = Res("wdram")
        r_hsort_g = Res("hsort")
        hs_filled = [False]

        def initial_zero_fill(pstack):
            if not SPARSE or hs_filled[0]:
                return
            z0 = sb("z0", [P, D], BF16, pstack)
            r_z0 = Res("z0")
            k.op("pool", lambda: nc.gpsimd.memset(z0[:], 0.0), wr=[r_z0])
            for j in range(NTL):
                k.dma("pool", hsort[j * P:(j + 1) * P, :], z0[:], rd=[r_z0], wr=[r_hsort_g])
            hs_filled[0] = True

        def moe_sparse(l, pstack):
            V = nc.vector
            G = nc.gpsimd
            Mf = sb("Mf", [P, NT, 32], F32, pstack)
            Mb = sb("Mb", [P, NT, 32], BF16, pstack)
            U = sb("Utri", [P, P], BF16, pstack)
            rr = Res("rt_tab")
            k.op("dve", lambda: V.tensor_scalar(out=Mf[:], in0=comb_tok[:], scalar1=0.0, scalar2=None, op0=ALU.is_gt), rd=[r_ctok], wr=[rr])
            k.op("dve", lambda: V.tensor_copy(out=Mb[:], in_=Mf[:]), rd=[rr], wr=[rr])
            k.op("pool", lambda: G.memset(U[:], 1.0), wr=[rr])
            k.op("pool", lambda: G.affine_select(out=U[:], in_=U[:], pattern=[[1, P]], compare_op=ALU.is_ge, fill=0.0, base=-1,
                                                 channel_multiplier=-1), rd=[rr], wr=[rr])
            tab = sb("rtab", [P, 8, 32], F32, pstack)
            C_ = tab[:, 0, :]
            nt_ = tab[:, 1, :]
            pad = tab[:, 2, :]
            incl = tab[:, 3, :]
            offs = tab[:, 4, :]
            tmp = tab[:, 5, :]
            for i in range(NT):
                k.op("pe", lambda: nc.tensor.matmul(ps[:, 0, 0:32], lhsT=ones_b[:], rhs=Mb[:, i, :], start=(i == 0), stop=(i == NT - 1)),
                     rd=[rr, r_const], wr=[pbank[0]], inc=(i == NT - 1))
            k.op("dve", lambda: V.tensor_copy(out=C_, in_=ps[:, 0, 0:32]), rd=[pbank[0]], wr=[rr])
            k.op("dve", lambda: V.memset(nt_, 0.0), wr=[rr])
            for m in range(NT):
                k.op("dve", lambda: V.tensor_scalar(out=tmp, in0=C_, scalar1=float(P * m), scalar2=None, op0=ALU.is_gt), rd=[rr], wr=[rr])
                k.op("dve", lambda: V.tensor_tensor(out=nt_, in0=nt_, in1=tmp, op=ALU.add), rd=[rr], wr=[rr])
            k.op("dve", lambda: V.tensor_scalar(out=pad, in0=nt_, scalar1=float(P), scalar2=None, op0=ALU.mult), rd=[rr], wr=[rr])
            k.op("dve", lambda: V.tensor_copy(out=incl[:, 0:1], in_=pad[:, 0:1]), rd=[rr], wr=[rr])
            for e in range(1, NEXP):
                k.op("dve", lambda: V.tensor_tensor(out=incl[:, e:e + 1], in0=incl[:, e - 1:e], in1=pad[:, e:e + 1], op=ALU.add), rd=[rr], wr=[rr])
            k.op("dve", lambda: V.tensor_tensor(out=offs, in0=incl, in1=pad, op=ALU.subtract), rd=[rr], wr=[rr])
            jt_i = sb("jt_i", [P, NTL], I32, pstack)
            jthr = sb("jthr", [P, NTL], F32, pstack)
            te = sb("te", [P, NTL], F32, pstack)
            tmp2 = sb("tmp2", [P, NTL], F32, pstack)
            pid_i = sb("pid_i", [P, 1], I32, pstack)
            pid_f = sb("pid_f", [P, 1], F32, pstack)
            widx = sb("widx", [P, NTL], I32, pstack)
            k.op("pool", lambda: G.iota(jt_i[:], pattern=[[P, NTL]], base=0, channel_multiplier=0), wr=[rr])
            k.op("pool", lambda: G.iota(pid_i[:], pattern=[[0, 1]], base=0, channel_multiplier=1), wr=[rr])
            k.op("dve", lambda: V.tensor_copy(out=jthr[:], in_=jt_i[:]), rd=[rr], wr=[rr])
            k.op("dve", lambda: V.tensor_copy(out=pid_f[:], in_=pid_i[:]), rd=[rr], wr=[rr])
            k.op("dve", lambda: V.memset(te[:], 0.0), wr=[rr])
            for e in range(NEXP):
                k.op("dve", lambda: V.tensor_scalar(out=tmp2[:], in0=jthr[:], scalar1=incl[:, e:e + 1], scalar2=None, op0=ALU.is_ge), rd=[rr], wr=[rr])
                k.op("dve", lambda: V.tensor_tensor(out=te[:], in0=te[:], in1=tmp2[:], op=ALU.add), rd=[rr], wr=[rr])
            k.op("dve", lambda: V.tensor_scalar(out=te[:], in0=te[:], scalar1=float(NEXP - 1), scalar2=float(P), op0=ALU.min, op1=ALU.mult),
                 rd=[rr], wr=[rr])
            k.op("dve", lambda: V.tensor_scalar(out=te[:], in0=te[:], scalar1=pid_f[:, 0:1], scalar2=None, op0=ALU.add), rd=[rr], wr=[rr])
            k.op("dve", lambda: V.tensor_copy(out=widx[:], in_=te[:]), rd=[rr], wr=[rr])
            slf = sb("slf", [P, 2, NT], F32, pstack)
            sc = sb("sc", [P, 4, 6, 32], F32, pstack)
            r_sc = [Res("sc%d" % i) for i in range(4)]
            ht = [sb("ht%d" % i, [P, D], BF16, pstack) for i in range(4)]
            r_ht = [Res("ht%d" % i) for i in range(4)]
            r_hsort = r_hsort_g
            zrow = sb("zrow", [P, D], BF16, pstack)
            r_z = Res("zrow")
            k.op("pool", lambda: G.memset(zrow[:], 0.0), wr=[r_z])
            if not hs_filled[0]:
                for j in range(NTL):
                    k.dma("sp", hsort[j * P:(j + 1) * P, :], zrow[:], rd=[r_z], wr=[r_hsort])

            def tile_ops(i):
                bk = 1 + (i % 4)
                q = i % 4
                rq = r_sc[q]
                sl = sc[:, q, 0, :]
                slm = sc[:, q, 1, :]
                big = sc[:, q, 2, :]
                isA = sc[:, q, 3, :]
                wa = sc[:, q, 4, :]
                ops = []
                A = ops.append

                def mm():
                    for i2 in range(i):
                        k.op("pe", lambda: nc.tensor.matmul(ps[:, bk, 0:32], lhsT=ones_b[:], rhs=Mb[:, i2, :], start=(i2 == 0), stop=False),
                             rd=[rr, r_const], wr=[pbank[bk]], inc=False)
                    k.op("pe", lambda: nc.tensor.matmul(ps[:, bk, 0:32], lhsT=U[:], rhs=Mb[:, i, :], start=(i == 0), stop=True),
                         rd=[rr, r_const], wr=[pbank[bk]])
                A(mm)
                A(lambda: k.op("dve", lambda: V.tensor_tensor(out=sl, in0=ps[:, bk, 0:32], in1=offs, op=ALU.add), rd=[pbank[bk], rr], wr=[rq]))
                A(lambda: k.op("dve", lambda: V.tensor_tensor(out=slm, in0=sl, in1=Mf[:, i, :], op=ALU.mult), rd=[rq, rr], wr=[rq]))
                A(lambda: k.op("dve", lambda: V.tensor_reduce(out=slf[:, 1, i:i + 1], in_=slm, axis=AX.X, op=ALU.max), rd=[rq], wr=[rq, r_route]))
                A(lambda: k.op("dve", lambda: V.tensor_scalar(out=big, in0=Mf[:, i, :], scalar1=-1.0e9, scalar2=1.0e9, op0=ALU.mult, op1=ALU.add),
                               rd=[rr], wr=[rq]))
                A(lambda: k.op("dve", lambda: V.tensor_tensor(out=big, in0=big, in1=slm, op=ALU.add), rd=[rq], wr=[rq]))
                A(lambda: k.op("dve", lambda: V.tensor_reduce(out=slf[:, 0, i:i + 1], in_=big, axis=AX.X, op=ALU.min), rd=[rq], wr=[rq, r_route]))
                A(lambda: k.op("dve", lambda: V.tensor_scalar(out=isA, in0=big, scalar1=slf[:, 0, i:i + 1], scalar2=None, op0=ALU.is_equal),
                               rd=[rq, r_route], wr=[rq]))
                A(lambda: k.op("dve", lambda: V.tensor_tensor(out=wa, in0=isA, in1=comb_tok[:, i, :], op=ALU.mult), rd=[rq, r_ctok], wr=[rq]))
                A(lambda: k.op("dve", lambda: V.tensor_reduce(out=wAB[:, 0, i:i + 1], in_=wa, axis=AX.X, op=ALU.add), rd=[rq], wr=[r_route]))
                A(lambda: k.op("dve", lambda: V.tensor_reduce(out=wAB[:, 1, i:i + 1], in_=comb_tok[:, i, :], axis=AX.X, op=ALU.add), rd=[r_ctok], wr=[r_route]))
                A(lambda: k.op("dve", lambda: V.tensor_tensor(out=wAB[:, 1, i:i + 1], in0=wAB[:, 1, i:i + 1], in1=wAB[:, 0, i:i + 1], op=ALU.subtract),
                               rd=[r_route], wr=[r_route]))
                return ops

            for g0 in range(0, NT, 4):
                grp = list(range(g0, min(NT, g0 + 4)))
                for i in grp:
                    k.dma("sp", ht[i % 4][:], vtok[i * P:(i + 1) * P, :], rd=[r_htok], wr=[r_ht[i % 4]])
                lists = [tile_ops(i) for i in grp]
                for j in range(max(len(x) for x in lists)):
                    for lst in lists:
                        if j < len(lst):
                            lst[j]()
                g1 = grp[-1] + 1
                k.op("dve", lambda: V.tensor_copy(out=slotA_i[:, g0:g1], in_=slf[:, 0, g0:g1]), rd=[r_route], wr=[r_route])
                k.op("dve", lambda: V.tensor_copy(out=slotB_i[:, g0:g1], in_=slf[:, 1, g0:g1]), rd=[r_route], wr=[r_route])
                for i in grp:
                    for sl_i in (slotA_i, slotB_i):
                        k.dma_indirect(lambda s_: G.indirect_dma_start(out=hsort, out_offset=bass.IndirectOffsetOnAxis(ap=sl_i[:, i:i + 1], axis=0),
                                                                       in_=ht[i % 4][:, :], in_offset=None), [r_ht[i % 4], r_route], [r_hsort])
            wgt = [sb("wgt%d" % i, [P, KC * FE], BF16, pstack) for i in range(2)]
            wut = [sb("wut%d" % i, [P, KC * FE], BF16, pstack) for i in range(2)]
            wdt = [sb("wdt%d" % i, [P, 2 * D], BF16, pstack) for i in range(3)]
            r_wgu = [Res("wgu0"), Res("wgu1")]
            r_wdt = [Res("wdt%d" % i) for i in range(3)]
            hsb = [sb("hsb%d" % i, [P, D], BF16, pstack) for i in range(2)]
            r_hsb = [Res("hsb0"), Res("hsb1")]
            hsT = [sb("hsT%d" % i, [P, KC, P], BF16, pstack) for i in range(2)]
            r_hsT = [Res("hsT0"), Res("hsT1")]
            sg = [sb("ssg%d" % i, [P, 2 * P], BF16, pstack) for i in range(2)]
            r_sg = [Res("ssg0"), Res("ssg1")]
            aT = [sb("aT%d" % i, [P, 2 * P], BF16, pstack) for i in range(2)]
            r_aT = [Res("aT0"), Res("aT1")]
            ysb = [sb("ysb%d" % i, [P, D], F32, pstack) for i in range(2)]
            r_ysb = [Res("ysb0"), Res("ysb1")]

            def stA(j):
                b2_, b3_ = j % 2, j % 3
                k.dma_indirect(lambda s_: G.indirect_dma_start(out=wgt[b2_][:, :], out_offset=None, in_=wgb[l],
                                                               in_offset=bass.IndirectOffsetOnAxis(ap=widx[:, j:j + 1], axis=0)), [rr, r_wdram], [r_wgu[b2_]])
                k.dma_indirect(lambda s_: G.indirect_dma_start(out=wut[b2_][:, :], out_offset=None, in_=wub[l],
                                                               in_offset=bass.IndirectOffsetOnAxis(ap=widx[:, j:j + 1], axis=0)), [rr, r_wdram], [r_wgu[b2_]])
                k.dma_indirect(lambda s_: G.indirect_dma_start(out=wdt[b3_][:, :], out_offset=None, in_=wdb[l],
                                                               in_offset=bass.IndirectOffsetOnAxis(ap=widx[:, j:j + 1], axis=0)), [rr, r_wdram], [r_wdt[b3_]])
                k.dma("sp", hsb[b2_][:], hsort[j * P:(j + 1) * P, :], rd=[r_hsort], wr=[r_hsb[b2_]])
                bk = j % 2
                pb = ps[:, bk, :].bitcast(BF16)
                for c in range(KC):
                    k.op("pe", lambda: nc.tensor.transpose(out=pb[:, c * P:(c + 1) * P], in_=hsb[b2_][:, c * P:(c + 1) * P], identity=ident_b[:]),
                         rd=[r_hsb[b2_], r_const], wr=[pbank[bk]], inc=(c == KC - 1))
                k.op("dve", lambda: V.tensor_copy(out=hsT[b2_][:], in_=pb.rearrange("p (c n) -> p c n", n=P)), rd=[pbank[bk]], wr=[r_hsT[b2_]])

            def stB(j):
                b2_ = j % 2
                bk = 2 + j % 2
                n = 0
                for (wt, off) in ((wgt[b2_], 0), (wut[b2_], 2 * P)):
                    for fc in range(2):
                        for c in range(KC):
                            k.op("pe", lambda: nc.tensor.matmul(ps[:, bk, off + fc * P:off + (fc + 1) * P],
                                                               lhsT=wt[:, c * FE + fc * P:c * FE + (fc + 1) * P], rhs=hsT[b2_][:, c, :],
                                                               start=(n == 0), stop=(c == KC - 1), skip_group_check=True),
                                 rd=[r_wgu[b2_], r_hsT[b2_]], wr=[pbank[bk]], inc=(c == KC - 1 and fc == 1))
                            n += 1
                k.op("act", lambda: nc.scalar.activation(out=sg[b2_][:], in_=ps[:, bk, 0:2 * P], func=AF.Silu), rd=[pbank[bk]], wr=[r_sg[b2_]])
                k.op("dve", lambda: V.tensor_tensor(out=aT[b2_][:], in0=ps[:, bk, 2 * P:4 * P], in1=sg[b2_][:], op=ALU.mult),
                     rd=[pbank[bk], r_sg[b2_]], wr=[r_aT[b2_]])

            def stC(j):
                b2_, b3_ = j % 2, j % 3
                for half in range(2):
                    bk = 4 + 2 * (j % 2) + half
                    for fc in range(2):
                        k.op("pe", lambda: nc.tensor.matmul(ps[:, bk, :], lhsT=aT[b2_][:, fc * P:(fc + 1) * P],
                                                           rhs=wdt[b3_][:, fc * D + half * 512:fc * D + (half + 1) * 512],
                                                           start=(fc == 0), stop=(fc == 1)), rd=[r_aT[b2_], r_wdt[b3_]], wr=[pbank[bk]], inc=(fc == 1))
                    if half == 0:
                        k.op("act", lambda: nc.scalar.copy(out=ysb[b2_][:, 0:512], in_=ps[:, bk, :]), rd=[pbank[bk]], wr=[r_ysb[b2_]])
                    else:
                        k.op("dve", lambda: V.tensor_copy(out=ysb[b2_][:, 512:1024], in_=ps[:, bk, :]), rd=[pbank[bk]], wr=[r_ysb[b2_]])
                k.dma("sp", ysort[j * P:(j + 1) * P, :], ysb[b2_][:], rd=[r_ysb[b2_]], wr=[r_ysort])

            run_pipeline(NTL, [stA, stB, stC])
            if l + 1 < L:
                for j in range(NTL):
                    k.dma("sp", hsort[j * P:(j + 1) * P, :], zrow[:], rd=[r_z], wr=[r_hsort])
                hs_filled[0] = True

        def conv_c1(l, j, pstack, src_ap, r_src):
            w1 = sb("w1", [P, KC, 2 * D], BF16, pstack)
            r_w1 = Res("w1")
            load_w_bf16(w1[:], c_w1[j], KC, 2 * D, gain=g_mix[:, :, l], r_dst=r_w1)
            b1 = load_featvec("b1v%d" % l, c_b1[j].rearrange("(r d) -> r d", d=D), 2, pstack)
            zt = sb("zpad", [P, KC, 32], BF16, pstack)
            r_z = Res("zpad")
            k.op("pool", lambda: nc.gpsimd.memset(zt[:], 0.0), wr=[r_z])
            k.dma("pool", vscr[:, :, 0:32].rearrange("c p t -> p c t"), zt[:], rd=[r_z], wr=[r_vscr[0]])
            hs = HStage(pstack, flush=False)
            initial_zero_fill(pstack)
            pp = make_prepass(l, pstack) if SPARSE else None
            xt = [sb("xt%d" % i, [P, D], F32, pstack) for i in range(3)]
            r_xt = [Res("xt%d" % i) for i in range(3)]
            sgb = [sb("sgb%d" % i, [P, 512], F32, pstack) for i in range(2)]
            r_sgb = [Res("sgb0"), Res("sgb1")]
            vb = [sb("vb%d" % i, [P, KC, 512], BF16, pstack) for i in range(2)]
            r_vb = [Res("vb0"), Res("vb1")]
            def st_A(I):
                for sub in range(4):
                    t = I * 4 + sub
                    xi = t % 3
                    k.dma("sp", xt[xi][:], src_ap[t * P:(t + 1) * P, :], rd=[r_src[I]], wr=[r_xt[xi]])
                    hs.norm_put(t, xt[xi][:], r_xt[xi], 7)
            def st_B(I):
                hi = I % 2
                hT_mt = hs.buf[hi]
                r_hmt = hs.res[hi]
                for cc in range(KC):
                    ba = 0 + 2 * (cc % 2)
                    bg = 1 + 2 * (cc % 2)
                    si = cc % 2
                    for c in range(KC):
                        k.op("pe", lambda: nc.tensor.matmul(ps[:, ba, :], lhsT=w1[:, c, cc * P:(cc + 1) * P], rhs=hT_mt[:, c, :],
                                                           start=(c == 0), stop=(c == KC - 1)), rd=[r_w1, r_hmt], wr=[pbank[ba]],
                             inc=(c == KC - 1))
                    for c in range(KC):
                        k.op("pe", lambda: nc.tensor.matmul(ps[:, bg, :], lhsT=w1[:, c, D + cc * P:D + (cc + 1) * P], rhs=hT_mt[:, c, :],
                                                           start=(c == 0), stop=(c == KC - 1)), rd=[r_w1, r_hmt], wr=[pbank[bg]],
                             inc=(c == KC - 1))
                    k.op("act", lambda: nc.scalar.activation(out=sgb[si][:], in_=ps[:, bg, :], func=AF.Sigmoid, bias=b1[:, cc, 1:2]),
                         rd=[pbank[bg], r_const], wr=[r_sgb[si]])
                    k.op("dve", lambda: nc.vector.scalar_tensor_tensor(out=vb[hi][:, cc, :], in0=ps[:, ba, :], scalar=b1[:, cc, 0:1],
                                                                       in1=sgb[si][:], op0=ALU.add, op1=ALU.mult),
                         rd=[pbank[ba], r_sgb[si], r_const], wr=[r_vb[hi]])
                k.dma("pool", vscr[:, :, 32 + I * 512:32 + (I + 1) * 512].rearrange("c p t -> p c t"), vb[hi][:],
                      rd=[r_vb[hi]], wr=[r_vscr[I + 1]])

            def st_P(I):
                if pp is not None:
                    pp(-(-97 // NM))

            run_pipeline(NM, [st_A, st_B, st_P])
            if pp is not None:
                pp.flush()

        def conv_c2(l, j, pstack, src_ap, r_src):
            w2 = sb("w2", [P, KC, D], BF16, pstack)
            r_w2 = Res("w2")
            load_w_bf16(w2[:], c_w2[j], KC, D, r_dst=r_w2)
            cv = sb("cv_vec", [P, KC, 40], F32, pstack)
            r_cv = Res("cv")
            with ExitStack() as st:
                rows = sb("cv_rows", [40, D], F32, st)
                r_rows = Res("rows")
                k.op("pool", lambda: nc.gpsimd.memset(rows[:], 0.0), wr=[r_rows])
                k.dma("sp", rows[0:CW, :], c_wdw[j], wr=[r_rows])
                k.dma("sp", rows[32:33, :], c_bdw[j:j + 1, :], wr=[r_rows])
                k.dma("sp", rows[33:34, :], c_lng[j:j + 1, :], wr=[r_rows])
                k.dma("sp", rows[34:35, :], c_lnb[j:j + 1, :], wr=[r_rows])
                for c in range(KC):
                    bk = c % 2
                    k.op("pe", lambda: nc.tensor.transpose(out=ps[:, bk, 0:36], in_=rows[0:36, c * P:(c + 1) * P],
                                                          identity=ident_f[0:36, 0:36]), rd=[r_rows, r_const], wr=[pbank[bk]])
                    k.op("dve", lambda: nc.vector.tensor_copy(out=cv[:, c, 0:36], in_=ps[:, bk, 0:36]), rd=[pbank[bk]], wr=[r_cv])
                k.barrier()
            b2bc = sb("b2bc", [P, D], F32, pstack)
            k.dma("sp", b2bc[:], c_b2[j].partition_broadcast(P), wr=[r_cv])
            diag = sb("diag", [P, KC, CW, P], BF16, pstack)
            r_diag = Res("diag")
            n = 0
            for c in range(KC):
                for t_ in range(CW):
                    if n % 2 == 0:
                        k.op("pool", lambda: nc.gpsimd.tensor_scalar(out=diag[:, c, t_, :], in0=ident_f[:], scalar1=cv[:, c, t_:t_ + 1],
                                                                     scalar2=1.0, op0=ALU.mult, op1=ALU.mult),
                             rd=[r_cv, r_const], wr=[r_diag])
                    else:
                        k.op("dve", lambda: nc.vector.tensor_scalar(out=diag[:, c, t_, :], in0=ident_f[:], scalar1=cv[:, c, t_:t_ + 1],
                                                                    scalar2=None, op0=ALU.mult), rd=[r_cv, r_const], wr=[r_diag])
                    n += 1
            prep = MoePrep(l, pstack, (2, 3, 4))
            ring = [sb("ring%d" % i, [P, KC, 32 + 512], BF16, pstack) for i in range(1)] * 2
            r_ring = [Res("ring0")] * 2
            xt = [sb("xt%d" % i, [P, D], F32, pstack) for i in range(3)]
            r_xt = [Res("xt%d" % i) for i in range(3)]
            ybuf = sb("ybuf", [P, KC, 512], F32, pstack)
            r_y = Res("ybuf")
            yb = [sb("yb%d" % i, [P, 512], BF16, pstack) for i in range(2)]
            r_yb = [Res("yb0"), Res("yb1")]
            ysq = [sb("ysq%d" % i, [P, 512], BF16, pstack) for i in range(2)]
            r_ysq = [Res("ysq0"), Res("ysq1")]
            mean = sb("mean", [P, 512], F32, pstack)
            rstd_t = sb("rstd_t", [P, 512], F32, pstack)
            r_ln = Res("ln")
            zt = sb("ztmp", [P, 2, 512], F32, pstack)
            r_zt = [Res("zt0"), Res("zt1")]
            zT = sb("zT", [P, KC, 512], BF16, pstack)
            r_zT = Res("zT")
            xcnt = [0]
            for I in range(NM):
                ri = I % 2
                k.dma("sp", ring[ri][:], vscr[:, :, I * 512:I * 512 + 544].rearrange("c p t -> p c t"),
                      rd=[r_vscr[I], r_vscr[I + 1]], wr=[r_ring[ri]])
                pend_stats = []
                for cc in range(KC):
                    bc_ = 0 + (cc % 2)
                    si = cc % 2
                    for t_ in range(CW):
                        k.op("pe", lambda: nc.tensor.matmul(ps[:, bc_, :], lhsT=diag[:, cc, t_, :], rhs=ring[ri][:, cc, 2 + t_:2 + t_ + 512],
                                                           start=(t_ == 0), stop=(t_ == CW - 1)), rd=[r_diag, r_ring[ri]], wr=[pbank[bc_]],
                             inc=(t_ == CW - 1))
                    k.op("act", lambda: nc.scalar.activation(out=ybuf[:, cc, :], in_=ps[:, bc_, :], func=AF.Identity, bias=cv[:, cc, 32:33]),
                         rd=[pbank[bc_], r_cv], wr=[r_y])
                    k.op("act", lambda: nc.scalar.activation(out=ysq[si][:], in_=ps[:, bc_, :], func=AF.Square, bias=cv[:, cc, 32:33]),
                         rd=[pbank[bc_], r_cv], wr=[r_ysq[si]])
                    k.op("pool", lambda: nc.gpsimd.tensor_copy(out=yb[si][:], in_=ybuf[:, cc, :]), rd=[r_y], wr=[r_yb[si]])

                    def stats(c2):
                        s2 = c2 % 2
                        k.op("pe", lambda: nc.tensor.matmul(ps[:, 6, :], lhsT=ones_b[:], rhs=yb[s2][:], start=(c2 == 0), stop=(c2 == KC - 1),
                                                           skip_group_check=True), rd=[r_yb[s2], r_const], wr=[pbank[6]], inc=(c2 == KC - 1))
                        k.op("pe", lambda: nc.tensor.matmul(ps[:, 7, :], lhsT=ones_b[:], rhs=ysq[s2][:], start=(c2 == 0), stop=(c2 == KC - 1),
                                                           skip_group_check=True), rd=[r_ysq[s2], r_const], wr=[pbank[7]], inc=True)
                    pend_stats.append(cc)
                    if len(pend_stats) > 1:
                        stats(pend_stats.pop(0))
                while pend_stats:
                    stats(pend_stats.pop(0))
                k.op("act", lambda: nc.scalar.activation(out=mean[:], in_=ps[:, 6, :], func=AF.Copy, scale=1.0 / D), rd=[pbank[6]], wr=[r_ln])
                k.op("act", lambda: nc.scalar.activation(out=rstd_t[:], in_=ps[:, 6, :], func=AF.Square, scale=1.0 / D), rd=[pbank[6]], wr=[r_ln])
                k.op("dve", lambda: nc.vector.scalar_tensor_tensor(out=rstd_t[:], in0=ps[:, 7, :], scalar=1.0 / D, in1=rstd_t[:],
                                                                   op0=ALU.mult, op1=ALU.subtract), rd=[pbank[7], r_ln], wr=[r_ln])
                k.op("act", lambda: nc.scalar.activation(out=rstd_t[:], in_=rstd_t[:], func=AF.Sqrt, bias=eps_col[:, 0:1]),
                     rd=[r_ln, r_const], wr=[r_ln])
                k.op("dve", lambda: nc.vector.reciprocal(out=rstd_t[:], in_=rstd_t[:]), rd=[r_ln], wr=[r_ln])
                for cc in range(KC):
                    zi = cc % 2
                    k.op("pool", lambda: nc.gpsimd.tensor_tensor(out=zt[:, zi, :], in0=ybuf[:, cc, :], in1=mean[:], op=ALU.subtract),
                         rd=[r_y, r_ln], wr=[r_zt[zi]])
                    k.op("dve", lambda: nc.vector.tensor_tensor(out=zt[:, zi, :], in0=zt[:, zi, :], in1=rstd_t[:], op=ALU.mult),
                         rd=[r_zt[zi], r_ln], wr=[r_zt[zi]])
                    k.op("act", lambda: nc.scalar.activation(out=zT[:, cc, :], in_=zt[:, zi, :], func=AF.Silu, scale=cv[:, cc, 33:34],
                                                            bias=cv[:, cc, 34:35]), rd=[r_zt[zi], r_cv], wr=[r_zT])
                def st_X(sub, I=I):
                    t = I * 4 + sub
                    xi = t % 3
                    k.dma("sp", xt[xi][:], src_ap[t * P:(t + 1) * P, :], rd=[r_src[I]], wr=[r_xt[xi]])
                    k.op("pool", lambda: nc.gpsimd.tensor_tensor(out=xt[xi][:], in0=xt[xi][:], in1=b2bc[:], op=ALU.add),
                         rd=[r_xt[xi], r_cv], wr=[r_xt[xi]])
                    for half in range(2):
                        bk = 0 + half
                        for cc in range(KC):
                            k.op("pe", lambda: nc.tensor.matmul(ps[:, bk, :], lhsT=zT[:, cc, sub * P:(sub + 1) * P],
                                                               rhs=w2[:, cc, half * 512:(half + 1) * 512], start=(cc == 0), stop=(cc == KC - 1)),
                                 rd=[r_zT, r_w2], wr=[pbank[bk]], inc=(cc == KC - 1))
                        k.op("dve", lambda: nc.vector.tensor_tensor(out=xt[xi][:, half * 512:(half + 1) * 512], in0=ps[:, bk, :],
                                                                    in1=xt[xi][:, half * 512:(half + 1) * 512], op=ALU.add),
                             rd=[pbank[bk], r_xt[xi]], wr=[r_xt[xi]])
                    k.dma("pool", xres[t * P:(t + 1) * P, :], xt[xi][:], rd=[r_xt[xi]], wr=[r_xres[I]])

                run_pipeline(4, [st_X, lambda sub, I=I: prep.p1(xt[(I * 4 + sub) % 3][:], r_xt[(I * 4 + sub) % 3], I * 4 + sub),
                                 lambda sub, I=I: prep.p2(I * 4 + sub)])

        def ple_phase(l, pstack, last, next_kind=None):
            wpg = sb("wpg", [P, KC, D], BF16, pstack)
            wpp = sb("wpp", [P, 2, D], BF16, pstack)
            r_wp = Res("wp")
            load_w_bf16(wpg[:], ple_wg[l], KC, D, gain=g_ple[:, :, l], r_dst=r_wp)
            load_w_bf16(wpp[:], ple_wp[l], 2, D, r_dst=r_wp)
            if last:
                fn_bc = sb("fn_bc", [P, D], F32, pstack)
                k.dma("sp", fn_bc[:], fin_norm.partition_broadcast(P), wr=[r_wp])
            hs2 = None
            if next_kind == "attn":
                hs2 = HStage(pstack)
            NX = 7
            rs_d = {}
            xt = [sb("pxt%d" % i, [P, D], F32, pstack) for i in range(NX)]
            r_xt = [Res("pxt%d" % i) for i in range(NX)]
            pt = [sb("ppt%d" % i, [P, PLE], F32, pstack) for i in range(4)]
            r_pt = [Res("ppt%d" % i) for i in range(4)]
            xnb = [sb("pxnb%d" % i, [P, D + PLE], BF16, pstack) for i in range(2)]
            r_xnb = [Res("pxnb0"), Res("pxnb1")]
            hT = [sb("phT%d" % i, [P, KC + 2, P], BF16, pstack) for i in range(2)]
            r_h = [Res("phT0"), Res("phT1")]
            sig = [sb("psig%d" % i, [P, D], F32, pstack) for i in range(2)]
            r_sig = [Res("psig0"), Res("psig1")]
            yo = [sb("pyo%d" % i, [P, D], F32, pstack) for i in range(2)]
            r_yo = [Res("pyo0"), Res("pyo1")]
            if SPARSE:
                yab = [sb("yab%d" % i, [P, 2, D], F32, pstack) for i in range(2)]
                r_yab = [Res("yab0"), Res("yab1")]
            def st_A(t):
                xi = t % NX
                i2 = t % 2
                mt = t // 4
                k.dma("sp", xt[xi][:], xres[t * P:(t + 1) * P, :], rd=[r_xres[mt]], wr=[r_xt[xi]])
                k.dma("sp", pt[t % 4][:], p_in[l, t * P:(t + 1) * P, :], wr=[r_pt[t % 4]])
                if SPARSE:
                    for q_, sl_i in enumerate((slotA_i, slotB_i)):
                        k.dma_indirect(lambda s_: nc.gpsimd.indirect_dma_start(out=yab[i2][:, q_, :], out_offset=None, in_=ysort,
                                                                               in_offset=bass.IndirectOffsetOnAxis(ap=sl_i[:, t:t + 1], axis=0)),
                                       [r_ysort, r_route], [r_yab[i2]])

            def st_A1(t):
                xi = t % NX
                i2 = t % 2
                mt = t // 4
                if SPARSE:
                    for q_ in range(2):
                        k.op("dve", lambda: nc.vector.scalar_tensor_tensor(out=xt[xi][:], in0=yab[i2][:, q_, :], scalar=wAB[:, q_, t:t + 1],
                                                                           in1=xt[xi][:], op0=ALU.mult, op1=ALU.add),
                             rd=[r_yab[i2], r_xt[xi], r_route], wr=[r_xt[xi]])
                rs_d[t % 4] = rms_rstd(xt[xi][:], r_xt[xi])

            def st_Ab(t):
                xi = t % NX
                i2 = t % 2
                rstd, r_rs = rs_d[t % 4]
                k.op("dve", lambda: nc.vector.tensor_scalar(out=xnb[i2][:, 0:D], in0=xt[xi][:], scalar1=rstd, scalar2=None, op0=ALU.mult),
                     rd=[r_xt[xi], r_rs], wr=[r_xnb[i2]])
                k.op("pool", lambda: nc.gpsimd.tensor_copy(out=xnb[i2][:, D:D + PLE], in_=pt[t % 4][:]), rd=[r_pt[t % 4]], wr=[r_xnb[i2]])
            def st_A2(t):
                xi = t % NX
                i2 = t % 2
                bT = 6
                pb = ps[:, bT, :].bitcast(BF16)
                for c in range(KC):
                    k.op("pe", lambda: nc.tensor.transpose(out=pb[:, c * P:(c + 1) * P], in_=xnb[i2][:, c * P:(c + 1) * P],
                                                          identity=ident_b[:]), rd=[r_xnb[i2], r_const], wr=[pbank[bT]], inc=(c == KC - 1))
                k.op("act", lambda: nc.scalar.copy(out=hT[i2][:, 0:KC, :], in_=pb.rearrange("p (c n) -> p c n", n=P)),
                     rd=[pbank[bT]], wr=[r_h[i2]])
                bT2 = 4 + (t % 2)
                pb2 = ps[:, bT2, :].bitcast(BF16)
                for c in range(2):
                    k.op("pe", lambda: nc.tensor.transpose(out=pb2[:, c * P:(c + 1) * P], in_=xnb[i2][:, D + c * P:D + (c + 1) * P],
                                                          identity=ident_b[:]), rd=[r_xnb[i2], r_const], wr=[pbank[bT2]], inc=(c == 1))
                k.op("dve", lambda: nc.vector.tensor_copy(out=hT[i2][:, KC:KC + 2, :], in_=pb2[:, 0:2 * P].rearrange("p (c n) -> p c n", n=P)),
                     rd=[pbank[bT2]], wr=[r_h[i2]])
            def st_B(t):
                xi = t % NX
                i2 = t % 2
                mt = t // 4
                for half in range(2):
                    bgk = 0 + half
                    bpk = 2 + half
                    hs_ = slice(half * 512, (half + 1) * 512)
                    for c in range(KC):
                        k.op("pe", lambda: nc.tensor.matmul(ps[:, bgk, :], lhsT=hT[i2][:, c, :], rhs=wpg[:, c, hs_], start=(c == 0),
                                                           stop=(c == KC - 1)), rd=[r_h[i2], r_wp], wr=[pbank[bgk]], inc=(c == KC - 1))
                    for c in range(2):
                        k.op("pe", lambda: nc.tensor.matmul(ps[:, bpk, :], lhsT=hT[i2][:, KC + c, :], rhs=wpp[:, c, hs_], start=(c == 0),
                                                           stop=(c == 1)), rd=[r_h[i2], r_wp], wr=[pbank[bpk]], inc=(c == 1))
                    k.op("act", lambda: nc.scalar.activation(out=sig[i2][:, hs_], in_=ps[:, bgk, :], func=AF.Sigmoid),
                         rd=[pbank[bgk]], wr=[r_sig[i2]])
                    k.op("dve", lambda: nc.vector.tensor_tensor(out=sig[i2][:, hs_], in0=ps[:, bpk, :], in1=sig[i2][:, hs_], op=ALU.mult),
                         rd=[pbank[bpk], r_sig[i2]], wr=[r_sig[i2]])
            def st_B2(t):
                xi = t % NX
                i2 = t % 2
                mt = t // 4
                k.op("pool", lambda: nc.gpsimd.tensor_tensor(out=xt[xi][:], in0=xt[xi][:], in1=sig[i2][:], op=ALU.add),
                     rd=[r_xt[xi], r_sig[i2]], wr=[r_xt[xi]])
                if last:
                    rstd2, r_rs2 = rms_rstd(xt[xi][:], r_xt[xi])
                    k.op("dve", lambda: nc.vector.scalar_tensor_tensor(out=yo[i2][:], in0=xt[xi][:], scalar=rstd2, in1=fn_bc[:],
                                                                       op0=ALU.mult, op1=ALU.mult), rd=[r_xt[xi], r_rs2, r_wp], wr=[r_yo[i2]])
                    k.dma("pool", y_out[t * P:(t + 1) * P, :], yo[i2][:], rd=[r_yo[i2]], wr=[r_xres[mt]])
                else:
                    k.dma("pool", xres[t * P:(t + 1) * P, :], xt[xi][:], rd=[r_xt[xi]], wr=[r_xres[mt]])
                    if hs2 is not None:
                        hs2.norm_put(t, xt[xi][:], r_xt[xi], 7)

            run_pipeline(NT, [st_A, st_A1, st_Ab, st_A2, st_B, st_B2])

        def attn_a0(pstack, src_ap, r_src):
            initial_zero_fill(pstack)
            hs = HStage(pstack)
            xt = [sb("a0xt%d" % i, [P, D], F32, pstack) for i in range(3)]
            r_xt = [Res("a0xt%d" % i) for i in range(3)]
            for t in range(NT):
                xi = t % 3
                k.dma("sp", xt[xi][:], src_ap[t * P:(t + 1) * P, :], rd=[r_src[t // 4]], wr=[r_xt[xi]])
                hs.norm_put(t, xt[xi][:], r_xt[xi], 7)

        def attn_a1(l, j, pstack):
            wq = sb("wqkv", [P, KC, 3 * D], BF16, pstack)
            r_wq = Res("wqkv")
            load_w_bf16(wq[:], a_wqkv[j], KC, 3 * D, gain=g_mix[:, :, l], r_dst=r_wq)
            cosr = sb("cosr", [P, NT, 64], F32, pstack)
            sinr = sb("sinr", [P, NT, 64], F32, pstack)
            r_rope = Res("rope")
            with ExitStack() as st:
                pos_i = sb("pos_i", [NT, P], I32, st)
                pos_f = sb("pos_f", [NT, P], F32, st)
                posT = sb("posT", [P, NT], F32, st)
                ang = sb("ang", [P, 2, NT, 8], F32, st)
                tmpa = sb("tmpa", [P, 2, NT, 8], F32, st)
                tmpi = sb("tmpi", [P, 2, NT, 8], I32, st)
                r_p = Res("pos")
                k.dma("sp", pos_i[:], pos_in, wr=[r_p])
                k.op("dve", lambda: nc.vector.tensor_copy(out=pos_f[:], in_=pos_i[:]), rd=[r_p], wr=[r_p])
                k.op("pe", lambda: nc.tensor.transpose(out=ps[:, 0, 0:NT], in_=pos_f[:], identity=ident_f[0:NT, 0:NT]),
                     rd=[r_p, r_const], wr=[pbank[0]])
                k.op("dve", lambda: nc.vector.tensor_copy(out=posT[:], in_=ps[:, 0, 0:NT]), rd=[pbank[0]], wr=[r_p])
                V = nc.vector
                for i in range(8):
                    inv = THETA ** (-(2.0 * i) / ROPE)
                    k.op("dve", lambda: V.tensor_scalar(out=ang[:, 0, :, i], in0=posT[:], scalar1=float(inv), scalar2=None, op0=ALU.mult),
                         rd=[r_p], wr=[r_p])
                k.op("dve", lambda: V.tensor_scalar(out=ang[:, 1], in0=ang[:, 0], scalar1=float(math.pi / 2), scalar2=None, op0=ALU.add),
                     rd=[r_p], wr=[r_p])
                TWO_PI = float(2 * math.pi)
                A = ang[:].rearrange("p a t i -> p (a t i)")
                T_ = tmpa[:].rearrange("p a t i -> p (a t i)")
                TI = tmpi[:].rearrange("p a t i -> p (a t i)")
                k.op("dve", lambda: V.tensor_scalar(out=T_, in0=A, scalar1=float(1.0 / TWO_PI), scalar2=None, op0=ALU.mult), rd=[r_p], wr=[r_p])
                k.op("dve", lambda: V.tensor_copy(out=TI, in_=T_), rd=[r_p], wr=[r_p])
                k.op("dve", lambda: V.tensor_copy(out=T_, in_=TI), rd=[r_p], wr=[r_p])
                k.op("dve", lambda: V.scalar_tensor_tensor(out=A, in0=T_, scalar=-TWO_PI, in1=A, op0=ALU.mult, op1=ALU.add), rd=[r_p], wr=[r_p])
                k.op("dve", lambda: V.tensor_scalar(out=T_, in0=A, scalar1=float(math.pi), scalar2=-TWO_PI, op0=ALU.is_gt, op1=ALU.mult),
                     rd=[r_p], wr=[r_p])
                k.op("dve", lambda: V.tensor_tensor(out=A, in0=A, in1=T_, op=ALU.add), rd=[r_p], wr=[r_p])
                k.op("dve", lambda: V.tensor_scalar(out=T_, in0=A, scalar1=float(-math.pi), scalar2=TWO_PI, op0=ALU.is_lt, op1=ALU.mult),
                     rd=[r_p], wr=[r_p])
                k.op("dve", lambda: V.tensor_tensor(out=A, in0=A, in1=T_, op=ALU.add), rd=[r_p], wr=[r_p])
                k.op("dve", lambda: V.tensor_scalar(out=A, in0=A, scalar1=float(math.pi), scalar2=float(-math.pi), op0=ALU.min, op1=ALU.max),
                     rd=[r_p], wr=[r_p])
                k.op("act", lambda: nc.scalar.activation(out=T_, in_=A, func=AF.Sin), rd=[r_p], wr=[r_p])
                for r8 in range(8):
                    k.op("dve", lambda: V.tensor_copy(out=sinr[:, :, r8 * 8:(r8 + 1) * 8], in_=tmpa[:, 0]), rd=[r_p], wr=[r_rope])
                    k.op("dve", lambda: V.tensor_copy(out=cosr[:, :, r8 * 8:(r8 + 1) * 8], in_=tmpa[:, 1]), rd=[r_p], wr=[r_rope])
                k.barrier()
            if debug == "a1r":
                return
            hT = [sb("a1hT%d" % i, [P, KC, 512], BF16, pstack) for i in range(2)]
            r_hT = [Res("a1hT0"), Res("a1hT1")]
            qk = [sb("qk%d" % i, [P, 2 * D], BF16, pstack) for i in range(2)]
            r_qk = [Res("qk0"), Res("qk1")]
            vt = [sb("vt%d" % i, [P, D], BF16, pstack) for i in range(2)]
            r_vt = [Res("vt0"), Res("vt1")]
            rtmp = sb("rtmp", [P, 2, 4, 64], F32, pstack)
            r_rt = [Res("rtmp0"), Res("rtmp1")]
            qst = [sb("qst%d" % i, [P, NH, 512], BF16, pstack) for i in range(2)]
            kst = [sb("kst%d" % i, [P, NH, 512], BF16, pstack) for i in range(2)]
            r_qst = [Res("qst0"), Res("qst1")]
            r_kst = [Res("kst0"), Res("kst1")]
            V = nc.vector
            cnt = [0]

            def st_A(t):
                I, sub = t // 4, t % 4
                hi = I % 2
                i2 = t % 2
                if sub == 0:
                    k.dma("sp", hT[hi][:], hscr[:, :, I * 512:(I + 1) * 512].rearrange("c p t -> p c t"), rd=[r_hscr[I]], wr=[r_hT[hi]])
                for jc in range(6):
                    bk = cnt[0] % 4
                    cnt[0] += 1
                    for c in range(KC):
                        k.op("pe", lambda: nc.tensor.matmul(ps[:, bk, :], lhsT=hT[hi][:, c, sub * P:(sub + 1) * P],
                                                           rhs=wq[:, c, jc * 512:(jc + 1) * 512], start=(c == 0), stop=(c == KC - 1)),
                             rd=[r_hT[hi], r_wq], wr=[pbank[bk]], inc=(c == KC - 1))
                    if jc >= 4:
                        dstv = vt[i2][:, (jc - 4) * 512:(jc - 3) * 512]
                        k.op("act", lambda: nc.scalar.copy(out=dstv, in_=ps[:, bk, :]), rd=[pbank[bk]], wr=[r_vt[i2]])
                        continue
                    dst = qk[i2][:, jc * 512:(jc + 1) * 512]
                    k.op("act", lambda: nc.scalar.copy(out=dst, in_=ps[:, bk, :]), rd=[pbank[bk]], wr=[r_qk[i2]])
                    ri = cnt[0] % 2
                    pv = ps[:, bk, :].rearrange("p (s d) -> p s d", d=64)
                    dv = dst.rearrange("p (s d) -> p s d", d=64)
                    x1 = pv[:, :, 0:8]
                    x2 = pv[:, :, 8:16]
                    cs = cosr[:, t, :].rearrange("p (s i) -> p s i", i=8)
                    sn = sinr[:, t, :].rearrange("p (s i) -> p s i", i=8)
                    T4 = rtmp[:, ri]
                    a_ = T4[:, 0].rearrange("p (s i) -> p s i", i=8)
                    b_ = T4[:, 1].rearrange("p (s i) -> p s i", i=8)
                    c_ = T4[:, 2].rearrange("p (s i) -> p s i", i=8)
                    d_ = T4[:, 3].rearrange("p (s i) -> p s i", i=8)
                    k.op("dve", lambda: V.tensor_tensor(out=a_, in0=x1, in1=cs, op=ALU.mult), rd=[pbank[bk], r_rope], wr=[r_rt[ri]])
                    k.op("dve", lambda: V.tensor_tensor(out=b_, in0=x2, in1=sn, op=ALU.mult), rd=[pbank[bk], r_rope], wr=[r_rt[ri]])
                    k.op("dve", lambda: V.tensor_tensor(out=c_, in0=x2, in1=cs, op=ALU.mult), rd=[pbank[bk], r_rope], wr=[r_rt[ri]])
                    k.op("dve", lambda: V.tensor_tensor(out=d_, in0=x1, in1=sn, op=ALU.mult), rd=[pbank[bk], r_rope], wr=[r_rt[ri]])
                    k.op("pool", lambda: nc.gpsimd.tensor_tensor(out=dv[:, :, 0:8], in0=a_, in1=b_, op=ALU.subtract),
                         rd=[r_rt[ri]], wr=[r_qk[i2]])
                    k.op("pool", lambda: nc.gpsimd.tensor_tensor(out=dv[:, :, 8:16], in0=c_, in1=d_, op=ALU.add),
                         rd=[r_rt[ri]], wr=[r_qk[i2]])

            def st_B(t):
                I, sub = t // 4, t % 4
                hi = I % 2
                i2 = t % 2
                k.dma("pool", vtok[t * P:(t + 1) * P, :], vt[i2][:], rd=[r_vt[i2]], wr=[r_vscr[0]])
                for which in range(2):
                    bk = 4 + which * 2 + (t % 2)
                    pb = ps[:, bk, :].bitcast(BF16)
                    for h in range(NH):
                        k.op("pe", lambda: nc.tensor.transpose(out=pb[:, h * P:(h + 1) * P],
                                                              in_=qk[i2][:, which * D + h * P:which * D + (h + 1) * P], identity=ident_b[:]),
                             rd=[r_qk[i2], r_const], wr=[pbank[bk]], inc=(h == NH - 1))
                    stg = (qst if which == 0 else kst)[hi]
                    r_stg = (r_qst if which == 0 else r_kst)[hi]
                    if which == 0:
                        k.op("dve", lambda: V.tensor_copy(out=stg[:, :, sub * P:(sub + 1) * P], in_=pb.rearrange("p (h n) -> p h n", n=P)),
                             rd=[pbank[bk]], wr=[r_stg])
                    else:
                        k.op("act", lambda: nc.scalar.copy(out=stg[:, :, sub * P:(sub + 1) * P], in_=pb.rearrange("p (h n) -> p h n", n=P)),
                             rd=[pbank[bk]], wr=[r_stg])
                if sub == 3:
                    k.dma("pool", qscr[:, :, I * 512:(I + 1) * 512].rearrange("h p t -> p h t"), qst[hi][:], rd=[r_qst[hi]], wr=[r_vscr[0]])
                    k.dma("pool", kscr[:, :, I * 512:(I + 1) * 512].rearrange("h p t -> p h t"), kst[hi][:], rd=[r_kst[hi]], wr=[r_vscr[0]])

            run_pipeline(NT, [st_A, st_B])

        def attn_a2(l, j, pstack, o_all, r_o, lam_init):
            V = nc.vector
            lamt = sb("lamt", [P, 4 * DH + 8], F32, pstack)
            r_lam = Res("lam")
            k.dma("sp", lamt[:, 0:4 * DH], a_lam[j].partition_broadcast(P), wr=[r_lam])
            for q in range(2):
                k.op("dve", lambda: V.tensor_tensor(out=lamt[:, 2 * q * DH:(2 * q + 1) * DH], in0=lamt[:, 2 * q * DH:(2 * q + 1) * DH],
                                                    in1=lamt[:, (2 * q + 1) * DH:(2 * q + 2) * DH], op=ALU.mult), rd=[r_lam], wr=[r_lam])
                k.op("dve", lambda: V.tensor_reduce(out=lamt[:, 4 * DH + q:4 * DH + q + 1], in_=lamt[:, 2 * q * DH:(2 * q + 1) * DH],
                                                    axis=AX.X, op=ALU.add), rd=[r_lam], wr=[r_lam])
            k.op("act", lambda: nc.scalar.activation(out=lamt[:, 4 * DH + 2:4 * DH + 4], in_=lamt[:, 4 * DH:4 * DH + 2], func=AF.Exp),
                 rd=[r_lam], wr=[r_lam])
            nlam = lamt[:, 4 * DH + 4:4 * DH + 5]
            k.op("dve", lambda: V.tensor_tensor(out=nlam, in0=lamt[:, 4 * DH + 3:4 * DH + 4], in1=lamt[:, 4 * DH + 2:4 * DH + 3],
                                                op=ALU.subtract), rd=[r_lam], wr=[r_lam])
            k.op("dve", lambda: V.tensor_scalar(out=nlam, in0=nlam, scalar1=float(-lam_init), scalar2=None, op0=ALU.add), rd=[r_lam], wr=[r_lam])
            gsub = sb("gsub", [P, DV], F32, pstack)
            k.dma("sp", gsub[:], a_subln[j].partition_broadcast(P), wr=[r_lam])
            k.op("dve", lambda: V.tensor_scalar(out=gsub[:], in0=gsub[:], scalar1=float(1.0 - lam_init), scalar2=None, op0=ALU.mult),
                 rd=[r_lam], wr=[r_lam])
            QT = [sb("QT%d" % i, [P, 2, S], BF16, pstack) for i in range(2)]
            KT = [sb("KT%d" % i, [P, S], BF16, pstack) for i in range(2)]
            Va = [sb("Va%d" % i, [P, NT, 132], BF16, pstack) for i in range(2)]
            r_hd = [Res("hd0"), Res("hd1")]
            eT = [sb("eT%d" % i, [P, 2, 512], BF16, pstack) for i in range(3)]
            r_eT = [Res("eT%d" % i) for i in range(3)]
            of = sb("of", [P, 2, DV], F32, pstack)
            r_of = [Res("of0"), Res("of1")]
            fs = sb("fs", [P, 2, 8], F32, pstack)
            for i in range(2):
                k.op("pool", lambda: nc.gpsimd.memset(Va[i][:, :, 128:132], 1.0), wr=[r_hd[i]])
            ecnt = [0]
            scnt = [0]
            fcnt = [0]
            r_acc = [pbank[4 + i // 2] for i in range(8)]

            def load_head(h):
                bi = h % 2
                k.dma("sp", QT[bi][:, 0, :], qscr[h], rd=[r_vscr[0]], wr=[r_hd[bi]])
                k.dma("sp", QT[bi][:, 1, :], qscr[h], rd=[r_vscr[0]], wr=[r_hd[bi]])
                k.op("pool", lambda: nc.gpsimd.memset(QT[bi][64:128, 0, :], 0.0), wr=[r_hd[bi]])
                k.op("pool", lambda: nc.gpsimd.memset(QT[bi][0:64, 1, :], 0.0), wr=[r_hd[bi]])
                k.dma("sp", KT[bi][:], kscr[h], rd=[r_vscr[0]], wr=[r_hd[bi]])
                k.dma("sp", Va[bi][:, :, 0:DV], vtok[:, h * DV:(h + 1) * DV].rearrange("(j p) d -> p j d", p=P),
                      rd=[r_vscr[0]], wr=[r_hd[bi]])

            load_head(0)
            steps = [(h, I, jt) for h in range(NH) for I in range(NM) for jt in range(4 * I + 4)]
            st_info = {}

            def acc_ap(rp, c):
                return 4 + rp, c * 129

            def emit_S(n):
                h, I, jt = steps[n]
                bi = h % 2
                r = jt - 4 * I
                q0 = max(r, 0) * P
                sset = n % 2
                sb0 = 2 * sset
                for c in range(2):
                    k.op("pe", lambda: nc.tensor.matmul(ps[:, sb0 + c, q0:512], lhsT=KT[bi][:, jt * P:(jt + 1) * P],
                                                       rhs=QT[bi][:, c, I * 512 + q0:(I + 1) * 512], start=True, stop=True),
                         rd=[r_hd[bi]], wr=[pbank[sb0 + c]])
                ei = n % 3
                k.op("act", lambda: nc.scalar.activation(out=eT[ei][:, :, q0:512], in_=ps[:, sb0:sb0 + 2, q0:512], func=AF.Exp,
                                                        scale=float(DH ** -0.5)),
                     rd=[pbank[sb0], pbank[sb0 + 1]], wr=[r_eT[ei]])
                if r >= 0:
                    k.op("pool", lambda: nc.gpsimd.affine_select(out=eT[ei][:, :, q0:q0 + P], in_=eT[ei][:, :, q0:q0 + P],
                                                                 pattern=[[0, 2], [1, P]], compare_op=ALU.is_ge, fill=0.0, base=0,
                                                                 channel_multiplier=-1), rd=[r_eT[ei]], wr=[r_eT[ei]])

            def emit_V(n):
                h, I, jt = steps[n]
                bi = h % 2
                if I == 0 and jt == 0 and h + 1 < NH:
                    load_head(h + 1)
                r = jt - 4 * I
                ei = n % 3
                for rp in range(max(r, 0), 4):
                    for c in range(2):
                        bk, off = acc_ap(rp, c)
                        st = (jt == 0 and c == 0)
                        last = (jt == 4 * I + rp)
                        k.op("pe", lambda: nc.tensor.matmul(ps[:, bk, off:off + 129], lhsT=eT[ei][:, c, rp * P:(rp + 1) * P],
                                                           rhs=Va[bi][:, jt, 0:129], start=st, stop=last, skip_group_check=True),
                             rd=[r_eT[ei], r_hd[bi]], wr=[pbank[bk]], inc=(last and c == 1) or (rp == 3 and c == 1))
                if r >= 0:
                    t = 4 * I + r
                    fi = fcnt[0] % 2
                    fcnt[0] += 1
                    b0_, o0 = acc_ap(r, 0)
                    b1_, o1 = acc_ap(r, 1)
                    F = fs[:, fi, :]
                    rf = r_of[fi]
                    ra = pbank[b0_]
                    k.op("dve", lambda: V.reciprocal(out=F[:, 0:1], in_=ps[:, b0_, o0 + 128:o0 + 129]), rd=[ra], wr=[rf])
                    k.op("dve", lambda: V.reciprocal(out=F[:, 1:2], in_=ps[:, b1_, o1 + 128:o1 + 129]), rd=[ra], wr=[rf])
                    k.op("dve", lambda: V.tensor_tensor(out=F[:, 2:3], in0=F[:, 1:2], in1=nlam, op=ALU.mult), rd=[rf, r_lam], wr=[rf])
                    k.op("dve", lambda: V.tensor_scalar(out=of[:, fi, :], in0=ps[:, b0_, o0:o0 + 128], scalar1=F[:, 0:1], scalar2=None,
                                                        op0=ALU.mult), rd=[ra, rf], wr=[rf])
                    k.op("dve", lambda: V.scalar_tensor_tensor(out=of[:, fi, :], in0=ps[:, b1_, o1:o1 + 128], scalar=F[:, 2:3],
                                                               in1=of[:, fi, :], op0=ALU.mult, op1=ALU.add), rd=[ra, rf], wr=[rf])
                    k.op("pool", lambda: nc.gpsimd.tensor_tensor(out=sq[:, fi, :], in0=of[:, fi, :], in1=of[:, fi, :], op=ALU.mult),
                         rd=[rf], wr=[r_sq[fi]])
                    k.op("dve", lambda: V.tensor_reduce(out=F[:, 3:4], in_=sq[:, fi, :], axis=AX.X, op=ALU.add), rd=[r_sq[fi]], wr=[rf])
                    k.op("dve", lambda: V.tensor_scalar(out=F[:, 4:5], in0=F[:, 3:4], scalar1=1.0 / DV, scalar2=EPS, op0=ALU.mult, op1=ALU.add),
                         rd=[rf], wr=[rf])
                    k.op("pool", lambda: nc.gpsimd.tensor_tensor(out=F[:, 5:6], in0=F[:, 4:5], in1=nhalf[:], op=ALU.pow),
                         rd=[rf, r_const], wr=[rf])
                    k.op("dve", lambda: V.scalar_tensor_tensor(out=o_all[:, t, h * DV:(h + 1) * DV], in0=of[:, fi, :], scalar=F[:, 5:6],
                                                               in1=gsub[:], op0=ALU.mult, op1=ALU.mult), rd=[rf, r_lam], wr=[r_o[t // 4]])

            sq = sb("sq", [P, 2, DV], F32, pstack)
            r_sq = [Res("sq0"), Res("sq1")]
            pp = make_prepass(l, pstack, engines=("dve", "pool"), NPS=2, NWB=3) if SPARSE else None
            every = max(1, len(steps) // 100)
            LA = 1
            for n in range(len(steps) + LA):
                if n < len(steps):
                    emit_S(n)
                if n - LA >= 0:
                    emit_V(n - LA)
                if pp is not None and n % every == 0:
                    pp(1)
            if pp is not None:
                pp.flush()

        def attn_a3(l, j, pstack, o_all, r_o, src_ap, r_src):
            wo = sb("wo", [P, KC, D], BF16, pstack)
            r_wo = Res("wo")
            load_w_bf16(wo[:], a_wo[j], KC, D, r_dst=r_wo)
            prep = MoePrep(l, pstack, (2, 3, 4), depth=2, rdepth=8)
            xt = [sb("xt%d" % i, [P, D], F32, pstack) for i in range(4)]
            r_xt = [Res("xt%d" % i) for i in range(4)]
            oT = [sb("oT%d" % i, [P, KC, P], BF16, pstack) for i in range(2)]
            r_oT = [Res("oT0"), Res("oT1")]
            def st_X(t):
                xi = t % 4
                i2 = t % 2
                I = t // 4
                k.dma("sp", xt[xi][:], src_ap[t * P:(t + 1) * P, :], rd=[r_src[I]], wr=[r_xt[xi]])
                bT = 6 + (t % 2)
                pb = ps[:, bT, :].bitcast(BF16)
                for c in range(KC):
                    k.op("pe", lambda: nc.tensor.transpose(out=pb[:, c * P:(c + 1) * P], in_=o_all[:, t, c * P:(c + 1) * P],
                                                          identity=ident_b[:]), rd=[r_o[I], r_const], wr=[pbank[bT]], inc=(c == KC - 1))
                k.op("act", lambda: nc.scalar.copy(out=oT[i2][:], in_=pb.rearrange("p (c n) -> p c n", n=P)), rd=[pbank[bT]], wr=[r_oT[i2]])

            def st_X2(t):
                xi = t % 4
                i2 = t % 2
                I = t // 4
                for half in range(2):
                    bk = 0 + half
                    for c in range(KC):
                        k.op("pe", lambda: nc.tensor.matmul(ps[:, bk, :], lhsT=oT[i2][:, c, :], rhs=wo[:, c, half * 512:(half + 1) * 512],
                                                           start=(c == 0), stop=(c == KC - 1)), rd=[r_oT[i2], r_wo], wr=[pbank[bk]],
                             inc=(c == KC - 1))
                    k.op("dve", lambda: nc.vector.tensor_tensor(out=xt[xi][:, half * 512:(half + 1) * 512], in0=ps[:, bk, :],
                                                                in1=xt[xi][:, half * 512:(half + 1) * 512], op=ALU.add),
                         rd=[pbank[bk], r_xt[xi]], wr=[r_xt[xi]])
                k.dma("pool", xres[t * P:(t + 1) * P, :], xt[xi][:], rd=[r_xt[xi]], wr=[r_xres[I]])

            prep.split_logits = True
            run_pipeline(NT, [st_X, st_X2, lambda t: prep.p1a1(xt[t % 4][:], r_xt[t % 4], t),
                              lambda t: prep.p1a2(xt[t % 4][:], r_xt[t % 4], t), prep.p1b, prep.p1b2, prep.p2])

        jc = 0
        ja = 0
        r_xin = [Res("xin%d" % i) for i in range(NM)]
        for l, kind in enumerate(layer_kinds):
            src_ap, r_src = (x_in, r_xin) if l == 0 else (xres, r_xres)
            if kind == "conv":
                with ExitStack() as pstack:
                    conv_c1(l, jc, pstack, src_ap, r_src)
                    k.barrier()
                if debug == "c1":
                    break
                with ExitStack() as pstack:
                    conv_c2(l, jc, pstack, src_ap, r_src)
                    k.barrier()
                if debug == "c2":
                    break
                jc += 1
            else:
                if l == 0:
                    with ExitStack() as pstack:
                        attn_a0(pstack, src_ap, r_src)
                        k.barrier()
                if debug == "a0":
                    break
                with ExitStack() as pstack:
                    attn_a1(l, ja, pstack)
                    k.barrier()
                if debug in ("a1", "a1r", "a1x"):
                    break
                with ExitStack() as ostack:
                    o_all = sb("o_all", [P, NT, D], BF16, ostack)
                    r_o = [Res("o%d" % i) for i in range(NM)]
                    with ExitStack() as pstack:
                        attn_a2(l, ja, pstack, o_all, r_o, lam_inits[l])
                        k.barrier()
                    if debug == "a2":
                        break
                    with ExitStack() as pstack:
                        attn_a3(l, ja, pstack, o_all, r_o, src_ap, r_src)
                        k.barrier()
                ja += 1
            if debug in ("a1", "a2", "a3"):
                break
            with ExitStack() as pstack:
                if SPARSE:
                    moe_sparse(l, pstack)
                else:
                    moe_dense(l, pstack)
                k.barrier()
            if debug == "moe":
                break
            with ExitStack() as pstack:
                last = (l == L - 1)
                ple_phase(l, pstack, last, None if last else layer_kinds[l + 1])
                k.barrier()
        k.final_wait("sp")
        print("ninst", k.ninst, "cnt", k.cnt)
    return nc


_NC_CACHE = {}


def _in_map(inputs, b, S, L):
    f = lambda a: np.ascontiguousarray(a, dtype=np.float32)
    m = {
        "x": f(inputs["x"][b]),
        "p": f(inputs["p"][:, b]),
        "positions": np.ascontiguousarray(inputs["positions"][b].reshape(S // P, P).astype(np.int32)),
    }
    for name in ("norm_mix", "norm_ffn", "conv_w_pw1", "conv_b_pw1", "conv_w_dw", "conv_b_dw", "conv_ln_g", "conv_ln_b",
                 "conv_w_pw2", "conv_b_pw2", "da_w_qkv", "da_subln", "da_w_o", "moe_w_rg", "moe_b_rg",
                 "ple_norm", "ple_w_gate", "ple_w_proj", "final_norm"):
        m[name] = f(inputs[name])
    m["moe_w_re"] = f(inputs["moe_w_re"])
    m["da_lambda"] = f(inputs["da_lambda"]).reshape(-1, 4 * DH)
    m["moe_b_re"] = f(inputs["moe_b_re"]).reshape(L, NG * NE)
    m["moe_w_gate"] = f(inputs["moe_w_gate"]).reshape(L, NEXP, D, FE)
    m["moe_w_up"] = f(inputs["moe_w_up"]).reshape(L, NEXP, D, FE)
    m["moe_w_down"] = f(inputs["moe_w_down"]).reshape(L, NEXP, FE, D)
    return m


def run(inputs, layer_kinds, n_cores=None, lam_i0=0, debug=None):
    B, S, _ = inputs["x"].shape
    L = len(layer_kinds)
    lam_inits = [0.8 - 0.6 * math.exp(-0.3 * (i + lam_i0)) for i in range(L)]
    key = (S, tuple(layer_kinds), debug)
    if key not in _NC_CACHE:
        _NC_CACHE[key] = build_program(S, layer_kinds, lam_inits, debug)
    nc = _NC_CACHE[key]
    n = B if n_cores is None else n_cores
    in_maps = [_in_map(inputs, b, S, L) for b in range(n)]
    res = run_bass_kernel_spmd(nc, in_maps, core_ids=list(range(n)))
    if debug:
        return res.results
    return np.stack([np.asarray(r["y"], dtype=np.float32) for r in res.results], axis=0)


def kernel(**inputs):
    inputs = {k_: np.asarray(v) for k_, v in inputs.items()}
    return run(inputs, ["conv", "attn"])
```

```python
import numpy as np
import math
from contextlib import ExitStack
import concourse.bass as bass
import concourse.mybir as mybir
from concourse.alu_op_type import AluOpType as ALU
from concourse.bass_utils import run_bass_kernel_spmd

F32 = mybir.dt.float32
BF16 = mybir.dt.bfloat16
I32 = mybir.dt.int32
U32 = mybir.dt.uint32
AF = mybir.ActivationFunctionType
AX = mybir.AxisListType

D = 1024
KC = 8
P = 128
CW = 31
NG = 4
NE = 8
NEXP = 32
FE = 256
PLE = 256
EPS = 1e-6
NH = 8
DH = 64
DV = 128
ROPE = 16
THETA = 500000.0
NDSEM = 8


class Res:
    __slots__ = ("name", "w", "rd", "excl")

    def __init__(self, name="", excl=False):
        self.name = name
        self.w = {}
        self.rd = {}
        self.excl = excl


class Sched:
    def __init__(self, nc, es):
        self.nc = nc
        self.E = {"pe": nc.tensor, "dve": nc.vector, "act": nc.scalar, "pool": nc.gpsimd, "sp": nc.sync}
        self.sems = []
        self.esem = {}
        self.cnt = {}
        self.seen = {}
        for e in self.E:
            self.esem[e] = len(self.sems)
            self.sems.append(es.enter_context(nc.semaphore("s_" + e)))
            self.cnt[e] = 0
            self.seen[e] = {}
        self.dsem = {}
        self.dnext = {}
        self.dval = {}
        for q in ("sp", "pool", "act"):
            self.dsem[q] = []
            for i in range(NDSEM):
                self.dsem[q].append(len(self.sems))
                self.dval[len(self.sems)] = 0
                self.sems.append(es.enter_context(nc.semaphore("d_%s%d" % (q, i))))
            self.dnext[q] = 0
        self.ninst = 0

    def _wait(self, eng, s, v):
        if v <= 0 or self.seen[eng].get(s, 0) >= v:
            return
        self.E[eng].wait_ge(self.sems[s], v)
        self.seen[eng][s] = v

    def _deps(self, eng, rd, wr, own):
        for r in rd:
            for s, v in r.w.items():
                self._wait(eng, s, v)
            if r.excl:
                for s, v in r.rd.items():
                    if s != own:
                        self._wait(eng, s, v)
        pe_own = self.esem["pe"]
        for w in wr:
            for s, v in w.w.items():
                if s != own or own != pe_own:
                    self._wait(eng, s, v)
            for s, v in w.rd.items():
                if s != own or own != pe_own:
                    self._wait(eng, s, v)

    def _mark(self, tok, rd, wr):
        s, v = tok
        for r in rd:
            if r.rd.get(s, 0) < v:
                r.rd[s] = v
        for w in wr:
            if w.w.get(s, 0) < v:
                w.w[s] = v

    def op(self, eng, fn, rd=(), wr=(), inc=True):
        own = self.esem[eng]
        self._deps(eng, rd, wr, own)
        ins = fn()
        self.ninst += 1
        if inc:
            self.cnt[eng] += 1
            ins.then_inc(self.sems[own], 1)
            tok = (own, self.cnt[eng])
        else:
            tok = (own, self.cnt[eng] + 1)
        self._mark(tok, rd, wr)
        return tok

    def dma(self, q, out, in_, rd=(), wr=(), **kw):
        self._deps(q, rd, wr, -1)
        i = self.dnext[q]
        self.dnext[q] = (i + 1) % NDSEM
        s = self.dsem[q][i]
        self._wait(q, s, self.dval[s])
        self.dval[s] += 16
        self.E[q].dma_start(out=out, in_=in_, **kw).then_inc(self.sems[s], 16)
        self.ninst += 1
        tok = (s, self.dval[s])
        self._mark(tok, rd, wr)
        return tok

    def dma_indirect(self, fn, rd=(), wr=()):
        q = "pool"
        self._deps(q, rd, wr, -1)
        i = self.dnext[q]
        self.dnext[q] = (i + 1) % NDSEM
        s = self.dsem[q][i]
        self._wait(q, s, self.dval[s])
        self.dval[s] += 16
        fn(None).then_inc(self.sems[s], 16)
        self.ninst += 1
        tok = (s, self.dval[s])
        self._mark(tok, rd, wr)
        return tok

    def barrier(self):
        for x in self.E:
            for e in self.E:
                if e != x or True:
                    self._wait(x, self.esem[e], self.cnt[e])
            for s, v in self.dval.items():
                self._wait(x, s, v)

    def final_wait(self, eng="sp"):
        for e in self.E:
            self._wait(eng, self.esem[e], self.cnt[e])
        for s, v in self.dval.items():
            self._wait(eng, s, v)


def build_program(S, layer_kinds, lam_inits, debug=None):
    NT = S // P
    NM = S // 512
    L = len(layer_kinds)
    nconv = sum(1 for k_ in layer_kinds if k_ == "conv")
    nattn = L - nconv
    nc = bass.Bass("TRN2", target_bir_lowering=False)

    def din(name, shape, dt=F32):
        return nc.dram_tensor(name, list(shape), dt, kind="ExternalInput").ap()

    def dscr(name, shape, dt):
        return nc.dram_tensor(name, list(shape), dt, kind=("ExternalOutput" if debug else "Internal")).ap()

    x_in = din("x", [S, D])
    p_in = din("p", [L, S, PLE])
    pos_in = din("positions", [NT, P], I32)
    norm_mix = din("norm_mix", [L, D])
    norm_ffn = din("norm_ffn", [L, D])
    c_w1 = din("conv_w_pw1", [max(nconv, 1), D, 2 * D])
    c_b1 = din("conv_b_pw1", [max(nconv, 1), 2 * D])
    c_wdw = din("conv_w_dw", [max(nconv, 1), CW, D])
    c_bdw = din("conv_b_dw", [max(nconv, 1), D])
    c_lng = din("conv_ln_g", [max(nconv, 1), D])
    c_lnb = din("conv_ln_b", [max(nconv, 1), D])
    c_w2 = din("conv_w_pw2", [max(nconv, 1), D, D])
    c_b2 = din("conv_b_pw2", [max(nconv, 1), D])
    a_wqkv = din("da_w_qkv", [max(nattn, 1), D, 3 * D])
    a_lam = din("da_lambda", [max(nattn, 1), 4 * DH])
    a_subln = din("da_subln", [max(nattn, 1), DV])
    a_wo = din("da_w_o", [max(nattn, 1), D, D])
    m_wrg = din("moe_w_rg", [L, D, NG])
    m_brg = din("moe_b_rg", [L, NG])
    m_wre = din("moe_w_re", [L, NG, D, NE])
    m_bre = din("moe_b_re", [L, NG * NE])
    m_wg = din("moe_w_gate", [L, NEXP, D, FE])
    m_wu = din("moe_w_up", [L, NEXP, D, FE])
    m_wd = din("moe_w_down", [L, NEXP, FE, D])
    ple_norm = din("ple_norm", [L, D])
    ple_wg = din("ple_w_gate", [L, D, D])
    ple_wp = din("ple_w_proj", [L, PLE, D])
    fin_norm = din("final_norm", [D])
    y_out = nc.dram_tensor("y", [S, D], F32, kind="ExternalOutput").ap()
    xres = dscr("xres", [S, D], F32)
    vscr = dscr("vscr", [KC, P, 32 + S], BF16)
    hscr = dscr("hscr", [KC, P, S], BF16)
    qscr = dscr("qscr", [NH, P, S], BF16)
    kscr = dscr("kscr", [NH, P, S], BF16)
    vtok = dscr("vtok", [S, D], BF16)
    NTL = (2 * S) // P + NEXP
    NSLOT = NTL * P
    SPARSE = True
    wgb = [dscr("wgb%d" % l_, [NEXP * P, KC * FE], BF16) for l_ in range(L)]
    wub = [dscr("wub%d" % l_, [NEXP * P, KC * FE], BF16) for l_ in range(L)]
    wdb = [dscr("wdb%d" % l_, [NEXP * P, 2 * D], BF16) for l_ in range(L)]
    hsort = dscr("hsort", [NSLOT, D], BF16)
    ysort = dscr("ysort", [NSLOT, D], F32)
    dbg_lg = dscr("dbg_lg", [S, 256], F32) if debug else None

    es = ExitStack()
    with es:
        k = Sched(nc, es)

        uniq = [0]

        def sb(name, shape, dt, stack=None):
            uniq[0] += 1
            return (stack or es).enter_context(nc.sbuf_tensor("%s_%d" % (name, uniq[0]), list(shape), dt))

        ps = es.enter_context(nc.psum_tensor("ps", [P, 8, 512], F32))
        pbank = [Res("bank%d" % i, excl=True) for i in range(8)]

        ident_b = sb("ident_b", [P, P], BF16)
        ident_f = sb("ident_f", [P, P], F32)
        ones_b = sb("ones_b", [P, P], BF16)
        eps_col = sb("eps_col", [P, 1], F32)
        r_const = Res("const")
        k.op("pool", lambda: nc.gpsimd.memset(ident_f[:], 0.0), wr=[r_const])
        k.op("pool", lambda: nc.gpsimd.affine_select(out=ident_f[:], in_=ident_f[:], pattern=[[-1, P]],
                                                     compare_op=ALU.not_equal, fill=1.0, base=0, channel_multiplier=1),
             rd=[r_const], wr=[r_const])
        k.op("pool", lambda: nc.gpsimd.tensor_copy(out=ident_b[:], in_=ident_f[:]), rd=[r_const], wr=[r_const])
        k.op("pool", lambda: nc.gpsimd.memset(ones_b[:], 1.0), wr=[r_const])
        k.op("pool", lambda: nc.gpsimd.memset(eps_col[:], EPS), wr=[r_const])

        def load_featvec(name, rows_ap, nrows, stack=None):
            out = sb(name, [P, KC, 8], F32, stack)
            with ExitStack() as st:
                tmp = sb(name + "_tmp", [8, D], F32, st)
                r_t = Res(name)
                k.op("pool", lambda: nc.gpsimd.memset(tmp[:], 0.0), wr=[r_t])
                k.dma("sp", tmp[0:nrows, :], rows_ap, wr=[r_t])
                for c in range(KC):
                    bk = c % 2
                    k.op("pe", lambda: nc.tensor.transpose(out=ps[:, bk, 0:8], in_=tmp[:, c * P:(c + 1) * P],
                                                          identity=ident_f[0:8, 0:8]), rd=[r_const, r_t], wr=[pbank[bk]])
                    k.op("dve", lambda: nc.vector.tensor_copy(out=out[:, c, :], in_=ps[:, bk, 0:8]), rd=[pbank[bk]], wr=[r_const])
                k.barrier()
            return out

        g_mix = load_featvec("g_mix", norm_mix, L)
        g_ffn = load_featvec("g_ffn", norm_ffn, L)
        g_ple = load_featvec("g_ple", ple_norm, L)

        NSTG = 2
        stage_bufs = [sb("stage%d" % i, [P, 2048], F32) for i in range(NSTG)]
        stage_res = [Res("stage%d" % i) for i in range(NSTG)]
        stage_i = [0]
        cast_rr = [0]

        def cast(eng, out, in_, rd, wr, scale=None):
            if eng == "act":
                if scale is None:
                    k.op("act", lambda: nc.scalar.copy(out=out, in_=in_), rd=rd, wr=wr)
                else:
                    k.op("act", lambda: nc.scalar.activation(out=out, in_=in_, func=AF.Identity, scale=scale), rd=rd, wr=wr)
            elif eng == "pool":
                if scale is None:
                    k.op("pool", lambda: nc.gpsimd.tensor_copy(out=out, in_=in_), rd=rd, wr=wr)
                else:
                    k.op("pool", lambda: nc.gpsimd.tensor_scalar(out=out, in0=in_, scalar1=scale, scalar2=1.0,
                                                                 op0=ALU.mult, op1=ALU.mult), rd=rd, wr=wr)
            else:
                if scale is None:
                    k.op("dve", lambda: nc.vector.tensor_copy(out=out, in_=in_), rd=rd, wr=wr)
                else:
                    k.op("dve", lambda: nc.vector.tensor_scalar(out=out, in0=in_, scalar1=scale, scalar2=None,
                                                                op0=ALU.mult), rd=rd, wr=wr)

        def load_w_bf16(dst3, src2, kc, n, gain=None, r_dst=None, engines=("act", "pool", "dve")):
            if n > 2048:
                for n0 in range(0, n, 1024):
                    load_w_bf16(dst3[:, :, n0:n0 + 1024], src2[:, n0:n0 + 1024], kc, 1024, gain, r_dst, engines)
                return
            per = max(1, 2048 // n)
            c0 = 0
            while c0 < kc:
                cn = min(per, kc - c0)
                si = stage_i[0] % NSTG
                stage_i[0] += 1
                stv = stage_bufs[si][:, 0:cn * n].rearrange("p (c n) -> p c n", n=n)
                k.dma("sp", stv, src2[c0 * P:(c0 + cn) * P, :].rearrange("(c p) n -> p c n", p=P), wr=[stage_res[si]])
                eng = engines[cast_rr[0] % len(engines)]
                cast_rr[0] += 1
                if gain is None:
                    cast(eng, dst3[:, c0:c0 + cn, :], stv, [stage_res[si]], [r_dst])
                else:
                    for c in range(cn):
                        cast(eng, dst3[:, c0 + c, :], stv[:, c, :], [stage_res[si], r_const], [r_dst],
                             scale=gain[:, c0 + c:c0 + c + 1])
                c0 += cn

        junk = sb("junk", [P, D], BF16)
        r_junk = Res("junk")
        stat = sb("stat", [P, 64], F32)
        r_stat = [Res("stat%d" % i) for i in range(16)]
        stat_i = [0]

        nhalf = sb("nhalf", [P, 1], F32)
        k.op("pool", lambda: nc.gpsimd.memset(nhalf[:], -0.5), wr=[r_const])

        def rms_rstd(x_ap, r_x, n=D):
            i = stat_i[0] % 16
            stat_i[0] += 1
            ssq = stat[:, 4 * i:4 * i + 1]
            ms = stat[:, 4 * i + 1:4 * i + 2]
            rstd = stat[:, 4 * i + 2:4 * i + 3]
            r = r_stat[i]
            k.op("act", lambda: nc.scalar.activation(out=junk[:, 0:n], in_=x_ap, func=AF.Square, accum_out=ssq),
                 rd=[r_x], wr=[r_junk, r])
            k.op("dve", lambda: nc.vector.tensor_scalar(out=ms, in0=ssq, scalar1=1.0 / n, scalar2=EPS, op0=ALU.mult, op1=ALU.add),
                 rd=[r], wr=[r])
            k.op("pool", lambda: nc.gpsimd.tensor_tensor(out=rstd, in0=ms, in1=nhalf[:], op=ALU.pow), rd=[r, r_const], wr=[r])
            return rstd, r

        combT = sb("combT", [32, S], BF16)
        comb_tok = sb("comb_tok", [P, NT, 32], F32)
        r_ctok = Res("comb_tok")
        slotA_i = sb("slotA_i", [P, NT], I32)
        slotB_i = sb("slotB_i", [P, NT], I32)
        wAB = sb("wAB", [P, 2, NT], F32)
        r_route = Res("route")
        r_htok = Res("htok")
        r_ysort = Res("ysort")
        r_comb = [Res("comb%d" % i) for i in range(NM)]
        r_xres = [Res("xres%d" % i) for i in range(NM)]
        r_hscr = [Res("hscr%d" % i) for i in range(NM)]
        r_vscr = [Res("vscr%d" % i) for i in range(NM + 1)]

        def run_pipeline(n, stages):
            ns = len(stages)
            for step in range(n + ns - 1):
                for i, f in enumerate(stages):
                    t = step - i
                    if 0 <= t < n:
                        f(t)

        class HStage:
            def __init__(self, stack, flush=True):
                self.flush = flush
                self.buf = [sb("hstage%d" % i, [P, KC, 512], BF16, stack) for i in range(2)]
                self.res = [Res("hstage0"), Res("hstage1")]
                self.xnb = [sb("hs_xnb%d" % i, [P, D], BF16, stack) for i in range(2)]
                self.r_xnb = [Res("hs_xnb0"), Res("hs_xnb1")]

            def put_T(self, t, src_bf16, r_src, bank, evac_eng="act"):
                mt, sub = t // 4, t % 4
                i = mt % 2
                pb = ps[:, bank, :].bitcast(BF16)
                for c in range(KC):
                    k.op("pe", lambda: nc.tensor.transpose(out=pb[:, c * P:(c + 1) * P], in_=src_bf16[:, c * P:(c + 1) * P],
                                                          identity=ident_b[:]), rd=[r_src, r_const], wr=[pbank[bank]], inc=(c == KC - 1))
                dst = self.buf[i][:, :, sub * P:(sub + 1) * P]
                src = pb.rearrange("p (c n) -> p c n", n=P)
                if evac_eng == "act":
                    k.op("act", lambda: nc.scalar.copy(out=dst, in_=src), rd=[pbank[bank]], wr=[self.res[i]])
                else:
                    k.op("dve", lambda: nc.vector.tensor_copy(out=dst, in_=src), rd=[pbank[bank]], wr=[self.res[i]])
                if sub == 3 and self.flush:
                    k.dma("pool", hscr[:, :, mt * 512:(mt + 1) * 512].rearrange("c p t -> p c t"), self.buf[i][:],
                          rd=[self.res[i]], wr=[r_hscr[mt]])

            def norm_put(self, t, x_ap, r_x, bank):
                i = t % 2
                rstd, r_rs = rms_rstd(x_ap, r_x)
                k.op("dve", lambda: nc.vector.tensor_scalar(out=self.xnb[i][:], in0=x_ap, scalar1=rstd, scalar2=None, op0=ALU.mult),
                     rd=[r_x, r_rs], wr=[self.r_xnb[i]])
                self.put_T(t, self.xnb[i], self.r_xnb[i], bank)

        class MoePrep:
            def __init__(self, l, stack, banks, depth=1, rdepth=4):
                self.l = l
                self.banks = banks
                self.depth = depth
                self.hs = HStage(stack)
                self.wr_f = sb("wr_f", [P, KC, 36], F32, stack)
                self.r_wr = Res("wr")
                self.rb_bc = sb("rb_bc", [P, 36], F32, stack)
                tmp = sb("wr_tmp", [P, KC, 36], F32, stack)
                r_wr = self.r_wr
                with nc.allow_non_contiguous_dma(reason="tiny router weights"):
                    k.dma("sp", tmp[:, :, 0:NG], m_wrg[l].rearrange("(c p) n -> p c n", p=P), wr=[r_wr])
                    for g in range(NG):
                        k.dma("sp", tmp[:, :, NG + g * NE:NG + (g + 1) * NE], m_wre[l, g].rearrange("(c p) n -> p c n", p=P), wr=[r_wr])
                for c in range(KC):
                    k.op("dve", lambda: nc.vector.tensor_scalar(out=self.wr_f[:, c, :], in0=tmp[:, c, :], scalar1=g_ffn[:, c, l:l + 1],
                                                                scalar2=None, op0=ALU.mult), rd=[r_wr, r_const], wr=[r_wr])
                k.dma("sp", self.rb_bc[:, 0:NG], m_brg[l].partition_broadcast(P), wr=[r_wr])
                k.dma("sp", self.rb_bc[:, NG:36], m_bre[l].partition_broadcast(P), wr=[r_wr])
                self.xn_fs = [sb("xn_f%d" % i, [P, D], F32, stack) for i in range(depth)]
                self.r_xns = [Res("xn%d" % i) for i in range(depth)]
                self.hTfs = [sb("hTf%d" % i, [P, KC, P], F32, stack) for i in range(depth)]
                self.r_hTfs = [Res("hTf%d" % i) for i in range(depth)]
                self.rdepth = rdepth
                self.rs = {}
                self.split_logits = False
                self.rt = sb("rt", [P, self.rdepth, 256], F32, stack)
                self.r_rt = [Res("rt%d" % i) for i in range(self.rdepth)]
                self.ctr = 0

            def tile(self, x_ap, r_x, t):
                self.p1(x_ap, r_x, t)
                self.p2(t)

            def p1(self, x_ap, r_x, t):
                self.p1a(x_ap, r_x, t)
                self.p1b(t)

            def p1a(self, x_ap, r_x, t):
                self.p1a1(x_ap, r_x, t)
                self.p1a2(x_ap, r_x, t)

            def p1a1(self, x_ap, r_x, t):
                self.rs[t % 4] = rms_rstd(x_ap, r_x)

            def p1a2(self, x_ap, r_x, t):
                xn_f, r_xn = self.xn_fs[t % self.depth], self.r_xns[t % self.depth]
                rstd, r_rs = self.rs[t % 4]
                k.op("act", lambda: nc.scalar.activation(out=xn_f[:], in_=x_ap, func=AF.Identity, scale=rstd),
                     rd=[r_x, r_rs], wr=[r_xn])
                xb = self.hs.xnb[t % 2]
                r_xb = self.hs.r_xnb[t % 2]
                k.op("pool", lambda: nc.gpsimd.tensor_copy(out=xb[:], in_=xn_f[:]), rd=[r_xn], wr=[r_xb])
                if SPARSE:
                    k.dma("pool", vtok[t * P:(t + 1) * P, :], xb[:], rd=[r_xb], wr=[r_htok])

            def p1b(self, t):
                i = t % self.rdepth
                b0, b1, b2 = self.banks
                xn_f, r_xn = self.xn_fs[t % self.depth], self.r_xns[t % self.depth]
                hTf, r_hTf = self.hTfs[t % self.depth], self.r_hTfs[t % self.depth]
                xb = self.hs.xnb[t % 2]
                r_xb = self.hs.r_xnb[t % 2]
                self.hs.put_T(t, xb, r_xb, b0, evac_eng="dve")
                for c in range(KC):
                    bk = b1 if c < 4 else b2
                    k.op("pe", lambda: nc.tensor.transpose(out=ps[:, bk, (c % 4) * P:(c % 4 + 1) * P], in_=xn_f[:, c * P:(c + 1) * P],
                                                          identity=ident_f[:]), rd=[r_xn, r_const], wr=[pbank[bk]], inc=(c % 4 == 3))
                for h in range(2):
                    bk = b1 if h == 0 else b2
                    src = ps[:, bk, :].rearrange("p (c n) -> p c n", n=P)
                    k.op("act", lambda: nc.scalar.copy(out=hTf[:, 4 * h:4 * h + 4, :], in_=src), rd=[pbank[bk]], wr=[r_hTf])
                if self.split_logits:
                    return
                self.p1b2(t)

            def p1b2(self, t):
                i = t % self.rdepth
                b0, b1, b2 = self.banks
                hTf, r_hTf = self.hTfs[t % self.depth], self.r_hTfs[t % self.depth]
                for c in range(KC):
                    k.op("pe", lambda: nc.tensor.matmul(ps[:, b1, 0:36], lhsT=hTf[:, c, :], rhs=self.wr_f[:, c, :],
                                                       start=(c == 0), stop=(c == KC - 1)),
                         rd=[r_hTf, self.r_wr], wr=[pbank[b1]], inc=(c == KC - 1))
                R = self.rt[:, i, :]
                rr = self.r_rt[i]
                Lg = R[:, 0:36]
                gmax = R[:, 36:37]
                ngmax = R[:, 37:38]
                gsum = R[:, 38:39]
                gw = R[:, 39:40]
                goh = R[:, 40:44]
                gex = R[:, 44:48]
                top8 = R[:, 48:80]
                nt1 = R[:, 80:84]
                mask = R[:, 84:116]
                ex = R[:, 116:148]
                den = R[:, 148:152]
                coef = R[:, 152:156]
                comb = R[:, 160:192]
                V = nc.vector
                k.op("dve", lambda: V.tensor_tensor(out=Lg, in0=ps[:, b1, 0:36], in1=self.rb_bc[:], op=ALU.add),
                     rd=[pbank[b1], self.r_wr], wr=[rr])

            def p2(self, t):
                if t % 4 != 3:
                    return
                lists = [self.p2_ops(tt) for tt in range(t - 3, t + 1)]
                n = max(len(x) for x in lists)
                for j in range(n):
                    for lst in lists:
                        if j < len(lst):
                            lst[j]()

            def p2_ops(self, t):
                i = t % self.rdepth
                mt = t // 4
                col0 = t * P
                b0, b1, b2 = self.banks
                R = self.rt[:, i, :]
                rr = self.r_rt[i]
                Lg = R[:, 0:36]
                gmax = R[:, 36:37]
                ngmax = R[:, 37:38]
                gsum = R[:, 38:39]
                gw = R[:, 39:40]
                goh = R[:, 40:44]
                gex = R[:, 44:48]
                top8 = R[:, 48:80]
                nt1 = R[:, 80:84]
                mask = R[:, 84:116]
                ex = R[:, 116:148]
                den = R[:, 148:152]
                coef = R[:, 152:156]
                comb = R[:, 160:192]
                V = nc.vector
                ops = []
                A = ops.append
                A(lambda: k.op("dve", lambda: V.tensor_reduce(out=gmax, in_=Lg[:, 0:NG], axis=AX.X, op=ALU.max), rd=[rr], wr=[rr]))
                A(lambda: k.op("dve", lambda: V.tensor_scalar(out=goh, in0=Lg[:, 0:NG], scalar1=gmax, scalar2=None, op0=ALU.is_equal),
                               rd=[rr], wr=[rr]))
                A(lambda: k.op("dve", lambda: V.tensor_scalar(out=ngmax, in0=gmax, scalar1=-1.0, scalar2=None, op0=ALU.mult), rd=[rr], wr=[rr]))
                A(lambda: k.op("act", lambda: nc.scalar.activation(out=gex, in_=Lg[:, 0:NG], func=AF.Exp, bias=ngmax, accum_out=gsum),
                               rd=[rr], wr=[rr]))

                def top_all():
                    for g in range(NG):
                        k.op("dve", lambda: V.max(out=top8[:, g * 8:(g + 1) * 8], in_=Lg[:, NG + g * NE:NG + (g + 1) * NE]), rd=[rr], wr=[rr])
                A(top_all)
                t8 = top8.rearrange("p (g e) -> p g e", e=8)
                A(lambda: k.op("dve", lambda: V.tensor_scalar(out=nt1, in0=t8[:, :, 0], scalar1=-1.0, scalar2=None, op0=ALU.mult), rd=[rr], wr=[rr]))

                def mask_all():
                    for g in range(NG):
                        le = Lg[:, NG + g * NE:NG + (g + 1) * NE]
                        k.op("dve", lambda: V.tensor_scalar(out=mask[:, g * 8:(g + 1) * 8], in0=le, scalar1=top8[:, g * 8 + 1:g * 8 + 2],
                                                            scalar2=None, op0=ALU.is_ge), rd=[rr], wr=[rr])
                A(mask_all)

                def ex_all():
                    for g in range(NG):
                        le = Lg[:, NG + g * NE:NG + (g + 1) * NE]
                        k.op("act", lambda: nc.scalar.activation(out=ex[:, g * 8:(g + 1) * 8], in_=le, func=AF.Exp, bias=nt1[:, g:g + 1]),
                             rd=[rr], wr=[rr])
                A(ex_all)
                A(lambda: k.op("dve", lambda: V.reciprocal(out=gw, in_=gsum), rd=[rr], wr=[rr]))
                A(lambda: k.op("dve", lambda: V.tensor_tensor(out=ex, in0=ex, in1=mask, op=ALU.mult), rd=[rr], wr=[rr]))
                A(lambda: k.op("dve", lambda: V.tensor_reduce(out=den, in_=ex.rearrange("p (g e) -> p g e", e=8), axis=AX.X, op=ALU.add),
                               rd=[rr], wr=[rr]))
                A(lambda: k.op("dve", lambda: V.reciprocal(out=coef, in_=den), rd=[rr], wr=[rr]))
                A(lambda: k.op("dve", lambda: V.tensor_tensor(out=coef, in0=coef, in1=goh, op=ALU.mult), rd=[rr], wr=[rr]))
                A(lambda: k.op("dve", lambda: V.tensor_scalar(out=coef, in0=coef, scalar1=gw, scalar2=None, op0=ALU.mult), rd=[rr], wr=[rr]))

                def comb_all():
                    for g in range(NG):
                        k.op("dve", lambda: V.tensor_scalar(out=comb[:, g * 8:(g + 1) * 8], in0=ex[:, g * 8:(g + 1) * 8],
                                                            scalar1=coef[:, g:g + 1], scalar2=None, op0=ALU.mult), rd=[rr], wr=[rr])
                A(comb_all)
                if SPARSE:
                    A(lambda: k.op("pool", lambda: nc.gpsimd.tensor_copy(out=comb_tok[:, t, :], in_=comb), rd=[rr], wr=[r_ctok]))

                def fin():
                    k.op("pe", lambda: nc.tensor.transpose(out=ps[0:32, b2, 0:P], in_=comb, identity=ident_f[:]),
                         rd=[rr, r_const], wr=[pbank[b2]])
                    if debug:
                        k.dma("pool", dbg_lg[t * P:(t + 1) * P, 0:36], R[:, 0:36], rd=[rr], wr=[Res("dbg")])
                    k.op("dve", lambda: V.tensor_copy(out=combT[:, col0:col0 + P], in_=ps[0:32, b2, 0:P]),
                         rd=[pbank[b2]], wr=[r_comb[mt]])
                A(fin)
                return ops

        EC = 4

        def moe_dense(l, pstack):
            sel = sb("sel", [32, NEXP, P], BF16, pstack)
            r_sel = Res("sel")
            k.op("pool", lambda: nc.gpsimd.memset(sel[:], 0.0), wr=[r_sel])
            k.op("pool", lambda: nc.gpsimd.affine_select(out=sel[:], in_=sel[:], pattern=[[-1, NEXP], [0, P]],
                                                         compare_op=ALU.not_equal, fill=1.0, base=0, channel_multiplier=1),
                 rd=[r_sel], wr=[r_sel])
            wg = [sb("wg%d" % i, [P, EC, KC, FE], BF16, pstack) for i in range(2)]
            wu = [sb("wu%d" % i, [P, EC, KC, FE], BF16, pstack) for i in range(2)]
            wd = [sb("wd%d" % i, [P, EC, 2, D], BF16, pstack) for i in range(2)]
            r_w = [Res("moew0"), Res("moew1")]
            hT = [sb("mhT%d" % i, [P, KC, 512], BF16, pstack) for i in range(2)]
            r_hT = [Res("mhT0"), Res("mhT1")]
            act = [sb("actb%d" % i, [P, EC, 2, 512], BF16, pstack) for i in range(2)]
            r_act = [Res("act0"), Res("act1")]
            cmb_sb = [sb("cmb_sb%d" % i, [P, 512], BF16, pstack) for i in range(2)]
            r_cmb = [Res("cmb0"), Res("cmb1")]
            sg = [sb("sg%d" % i, [P, 512], BF16, pstack) for i in range(3)]
            r_sg = [Res("sg%d" % i) for i in range(3)]
            ub = [sb("ub%d" % i, [P, 512], BF16, pstack) for i in range(3)]
            r_ub = [Res("ub%d" % i) for i in range(3)]
            ob = [sb("ob%d" % i, [P, D], F32, pstack) for i in range(3)]
            r_ob = [Res("ob%d" % i) for i in range(3)]
            nchunk = NEXP // EC

            def chunk_jobs(ci):
                bi = ci % 2
                jobs = []
                for e in range(EC):
                    eg = ci * EC + e
                    for which in range(3):
                        def mk(e=e, eg=eg, which=which):
                            st = {}

                            def dma():
                                si = stage_i[0] % NSTG
                                stage_i[0] += 1
                                st["si"] = si
                                if which < 2:
                                    src = (m_wg if which == 0 else m_wu)[l, eg]
                                    stv = stage_bufs[si][:, 0:KC * FE].rearrange("p (c n) -> p c n", n=FE)
                                else:
                                    src = m_wd[l, eg]
                                    stv = stage_bufs[si][:, 0:2 * D].rearrange("p (c n) -> p c n", n=D)
                                st["stv"] = stv
                                k.dma("sp", stv, src.rearrange("(c p) n -> p c n", p=P), wr=[stage_res[si]])

                            def cst():
                                si, stv = st["si"], st["stv"]
                                if which < 2:
                                    dst = (wg if which == 0 else wu)[bi][:, e]
                                    for c in range(KC):
                                        eng = "act" if which == 0 else ("pool" if c % 2 == 0 else "dve")
                                        cast(eng, dst[:, c, :], stv[:, c, :], [stage_res[si], r_const], [r_w[bi]], scale=g_ffn[:, c, l:l + 1])
                                else:
                                    cast("dve", wd[bi][:, e, 0, :], stv[:, 0, :], [stage_res[si]], [r_w[bi]])
                                    cast("act", wd[bi][:, e, 1, :], stv[:, 1, :], [stage_res[si]], [r_w[bi]])
                            return dma, cst
                        jobs.append(mk())
                return jobs

            cnt3 = [0]
            ocnt = [0]
            dcnt = [0]
            hcnt = [0]
            for d_, c_ in chunk_jobs(0):
                d_()
                c_()
            nslots = NM * EC
            for ci in range(nchunk):
                bi = ci % 2
                jobs = chunk_jobs(ci + 1) if ci + 1 < nchunk else []
                sched_d = {}
                sched_c = {}
                for j, (d_, c_) in enumerate(jobs):
                    sd = (j * nslots) // len(jobs)
                    sc = min(nslots - 1, sd + 1)
                    sched_d.setdefault(sd, []).append(d_)
                    sched_c.setdefault(sc, []).append(c_)
                for I in range(NM):
                    hi = hcnt[0] % 2
                    hcnt[0] += 1
                    k.dma("sp", hT[hi][:], hscr[:, :, I * 512:(I + 1) * 512].rearrange("c p t -> p c t"), rd=[r_hscr[I]], wr=[r_hT[hi]])
                    ai = (ci * NM + I) % 2
                    tok = slice(I * 512, (I + 1) * 512)
                    for e in range(EC):
                        slot = I * EC + e
                        late = []
                        for c_ in sched_c.get(slot, []):
                            try:
                                c_()
                            except KeyError:
                                late.append(c_)
                        for d_ in sched_d.get(slot, []):
                            d_()
                        for c_ in late:
                            c_()
                        eg = ci * EC + e
                        ci2 = (ci * NM * EC + I * EC + e) % 2
                        k.op("pe", lambda: nc.tensor.matmul(ps[:, 0, :], lhsT=sel[:, eg, :], rhs=combT[:, tok], start=True, stop=True),
                             rd=[r_sel, r_comb[I]], wr=[pbank[0]])
                        k.op("act", lambda: nc.scalar.copy(out=cmb_sb[ci2][:], in_=ps[:, 0, :]), rd=[pbank[0]], wr=[r_cmb[ci2]])
                        for fc in range(2):
                            j3 = cnt3[0] % 3
                            gset = cnt3[0] % 2
                            cnt3[0] += 1
                            bg = 1 + 2 * gset
                            bu = 2 + 2 * gset
                            for c in range(KC):
                                k.op("pe", lambda: nc.tensor.matmul(ps[:, bg, :], lhsT=wg[bi][:, e, c, fc * P:(fc + 1) * P],
                                                                   rhs=hT[hi][:, c, :], start=(c == 0), stop=(c == KC - 1)),
                                     rd=[r_w[bi], r_hT[hi]], wr=[pbank[bg]], inc=(c == KC - 1))
                            for c in range(KC):
                                k.op("pe", lambda: nc.tensor.matmul(ps[:, bu, :], lhsT=wu[bi][:, e, c, fc * P:(fc + 1) * P],
                                                                   rhs=hT[hi][:, c, :], start=(c == 0), stop=(c == KC - 1)),
                                     rd=[r_w[bi], r_hT[hi]], wr=[pbank[bu]], inc=(c == KC - 1))
                            k.op("act", lambda: nc.scalar.activation(out=sg[j3][:], in_=ps[:, bg, :], func=AF.Silu),
                                 rd=[pbank[bg]], wr=[r_sg[j3]])
                            k.op("dve", lambda: nc.vector.tensor_tensor(out=ub[j3][:], in0=ps[:, bu, :], in1=cmb_sb[ci2][:], op=ALU.mult),
                                 rd=[pbank[bu], r_cmb[ci2]], wr=[r_ub[j3]])
                            k.op("pool", lambda: nc.gpsimd.tensor_tensor(out=act[ai][:, e, fc, :], in0=ub[j3][:], in1=sg[j3][:], op=ALU.mult),
                                 rd=[r_ub[j3], r_sg[j3]], wr=[r_act[ai]])
                    for sub in range(4):
                        oi = ocnt[0] % 3
                        ocnt[0] += 1
                        for half in range(2):
                            bd = 5 + dcnt[0] % 2
                            dcnt[0] += 1
                            n = 0
                            for e in range(EC):
                                for fc in range(2):
                                    k.op("pe", lambda: nc.tensor.matmul(ps[:, bd, :], lhsT=act[ai][:, e, fc, sub * P:(sub + 1) * P],
                                                                       rhs=wd[bi][:, e, fc, half * 512:(half + 1) * 512],
                                                                       start=(n == 0), stop=(n == 2 * EC - 1)),
                                         rd=[r_act[ai], r_w[bi]], wr=[pbank[bd]], inc=(n == 2 * EC - 1))
                                    n += 1
                            if half == 0:
                                k.op("dve", lambda: nc.vector.tensor_copy(out=ob[oi][:, 0:512], in_=ps[:, bd, :]),
                                     rd=[pbank[bd]], wr=[r_ob[oi]])
                            else:
                                k.op("act", lambda: nc.scalar.copy(out=ob[oi][:, 512:1024], in_=ps[:, bd, :]),
                                     rd=[pbank[bd]], wr=[r_ob[oi]])
                        r0 = I * 512 + sub * P
                        k.dma("pool", xres[r0:r0 + P, :], ob[oi][:], rd=[r_ob[oi]], wr=[r_xres[I]], accum_op=ALU.add)

        def make_prepass(l, pstack, engines=("act", "dve", "pool")):
            wb = [sb("wb%d" % i, [P, 2048], BF16, pstack) for i in range(3)]
            r_wb = [Res("wb%d" % i) for i in range(3)]
            jobs = [(e, which) for e in range(NEXP) for which in range(3)]
            state = {"n": 0, "pend": None}

            def issue(n):
                e, which = jobs[n]
                si = stage_i[0] % NSTG
                stage_i[0] += 1
                if which < 2:
                    src = (m_wg if which == 0 else m_wu)[l, e]
                    stv = stage_bufs[si][:, 0:KC * FE].rearrange("p (c n) -> p c n", n=FE)
                else:
                    src = m_wd[l, e]
                    stv = stage_bufs[si][:, 0:2 * D].rearrange("p (c n) -> p c n", n=D)
                k.dma("sp", stv, src.rearrange("(c p) n -> p c n", p=P), wr=[stage_res[si]])
                return (n, si, stv)

            def finish(n, si, stv):
                e, which = jobs[n]
                bi = n % 3
                if which < 2:
                    dstv = wb[bi][:].rearrange("p (c n) -> p c n", n=FE)
                    for c in range(KC):
                        eng = engines[(n + c) % len(engines)]
                        cast(eng, dstv[:, c, :], stv[:, c, :], [stage_res[si], r_const], [r_wb[bi]], scale=g_ffn[:, c, l:l + 1])
                else:
                    dstv = wb[bi][:].rearrange("p (c n) -> p c n", n=D)
                    cast(engines[n % len(engines)], dstv[:, 0, :], stv[:, 0, :], [stage_res[si]], [r_wb[bi]])
                    cast(engines[(n + 1) % len(engines)], dstv[:, 1, :], stv[:, 1, :], [stage_res[si]], [r_wb[bi]])
                dst = (wgb, wub, wdb)[which][l]
                k.dma("pool", dst[e * P:(e + 1) * P, :], wb[bi][:], rd=[r_wb[bi]], wr=[r_wdram])

            def emit(nslots):
                for _ in range(nslots):
                    if state["pend"] is not None:
                        finish(*state["pend"])
                        state["pend"] = None
                    if state["n"] < len(jobs):
                        state["pend"] = issue(state["n"])
                        state["n"] += 1

            def flush():
                emit(len(jobs) + 1)
            emit.flush = flush
            return emit

        r_wdram = Res("wdram")
        r_hsort_g = Res("hsort")
        hs_filled = [False]

        def initial_zero_fill(pstack):
            if not SPARSE or hs_filled[0]:
                return
            z0 = sb("z0", [P, D], BF16, pstack)
            r_z0 = Res("z0")
            k.op("dve", lambda: nc.vector.memset(z0[:], 0.0), wr=[r_z0])
            for j in range(NTL):
                k.dma("sp", hsort[j * P:(j + 1) * P, :], z0[:], rd=[r_z0], wr=[r_hsort_g])
            hs_filled[0] = True

        def moe_sparse(l, pstack):
            V = nc.vector
            G = nc.gpsimd
            Mf = sb("Mf", [P, NT, 32], F32, pstack)
            Mb = sb("Mb", [P, NT, 32], BF16, pstack)
            U = sb("Utri", [P, P], BF16, pstack)
            rr = Res("rt_tab")
            k.op("dve", lambda: V.tensor_scalar(out=Mf[:], in0=comb_tok[:], scalar1=0.0, scalar2=None, op0=ALU.is_gt), rd=[r_ctok], wr=[rr])
            k.op("dve", lambda: V.tensor_copy(out=Mb[:], in_=Mf[:]), rd=[rr], wr=[rr])
            k.op("pool", lambda: G.memset(U[:], 1.0), wr=[rr])
            k.op("pool", lambda: G.affine_select(out=U[:], in_=U[:], pattern=[[1, P]], compare_op=ALU.is_ge, fill=0.0, base=-1,
                                                 channel_multiplier=-1), rd=[rr], wr=[rr])
            tab = sb("rtab", [P, 8, 32], F32, pstack)
            C_ = tab[:, 0, :]
            nt_ = tab[:, 1, :]
            pad = tab[:, 2, :]
            incl = tab[:, 3, :]
            offs = tab[:, 4, :]
            tmp = tab[:, 5, :]
            for i in range(NT):
                k.op("pe", lambda: nc.tensor.matmul(ps[:, 0, 0:32], lhsT=ones_b[:], rhs=Mb[:, i, :], start=(i == 0), stop=(i == NT - 1)),
                     rd=[rr, r_const], wr=[pbank[0]], inc=(i == NT - 1))
            k.op("dve", lambda: V.tensor_copy(out=C_, in_=ps[:, 0, 0:32]), rd=[pbank[0]], wr=[rr])
            k.op("dve", lambda: V.memset(nt_, 0.0), wr=[rr])
            for m in range(NT):
                k.op("dve", lambda: V.tensor_scalar(out=tmp, in0=C_, scalar1=float(P * m), scalar2=None, op0=ALU.is_gt), rd=[rr], wr=[rr])
                k.op("dve", lambda: V.tensor_tensor(out=nt_, in0=nt_, in1=tmp, op=ALU.add), rd=[rr], wr=[rr])
            k.op("dve", lambda: V.tensor_scalar(out=pad, in0=nt_, scalar1=float(P), scalar2=None, op0=ALU.mult), rd=[rr], wr=[rr])
            k.op("dve", lambda: V.tensor_copy(out=incl[:, 0:1], in_=pad[:, 0:1]), rd=[rr], wr=[rr])
            for e in range(1, NEXP):
                k.op("dve", lambda: V.tensor_tensor(out=incl[:, e:e + 1], in0=incl[:, e - 1:e], in1=pad[:, e:e + 1], op=ALU.add), rd=[rr], wr=[rr])
            k.op("dve", lambda: V.tensor_tensor(out=offs, in0=incl, in1=pad, op=ALU.subtract), rd=[rr], wr=[rr])
            jt_i = sb("jt_i", [P, NTL], I32, pstack)
            jthr = sb("jthr", [P, NTL], F32, pstack)
            te = sb("te", [P, NTL], F32, pstack)
            tmp2 = sb("tmp2", [P, NTL], F32, pstack)
            pid_i = sb("pid_i", [P, 1], I32, pstack)
            pid_f = sb("pid_f", [P, 1], F32, pstack)
            widx = sb("widx", [P, NTL], I32, pstack)
            k.op("pool", lambda: G.iota(jt_i[:], pattern=[[P, NTL]], base=0, channel_multiplier=0), wr=[rr])
            k.op("pool", lambda: G.iota(pid_i[:], pattern=[[0, 1]], base=0, channel_multiplier=1), wr=[rr])
            k.op("dve", lambda: V.tensor_copy(out=jthr[:], in_=jt_i[:]), rd=[rr], wr=[rr])
            k.op("dve", lambda: V.tensor_copy(out=pid_f[:], in_=pid_i[:]), rd=[rr], wr=[rr])
            k.op("dve", lambda: V.memset(te[:], 0.0), wr=[rr])
            for e in range(NEXP):
                k.op("dve", lambda: V.tensor_scalar(out=tmp2[:], in0=jthr[:], scalar1=incl[:, e:e + 1], scalar2=None, op0=ALU.is_ge), rd=[rr], wr=[rr])
                k.op("dve", lambda: V.tensor_tensor(out=te[:], in0=te[:], in1=tmp2[:], op=ALU.add), rd=[rr], wr=[rr])
            k.op("dve", lambda: V.tensor_scalar(out=te[:], in0=te[:], scalar1=float(NEXP - 1), scalar2=float(P), op0=ALU.min, op1=ALU.mult),
                 rd=[rr], wr=[rr])
            k.op("dve", lambda: V.tensor_scalar(out=te[:], in0=te[:], scalar1=pid_f[:, 0:1], scalar2=None, op0=ALU.add), rd=[rr], wr=[rr])
            k.op("dve", lambda: V.tensor_copy(out=widx[:], in_=te[:]), rd=[rr], wr=[rr])
            slf = sb("slf", [P, 2, NT], F32, pstack)
            sc = sb("sc", [P, 4, 6, 32], F32, pstack)
            r_sc = [Res("sc%d" % i) for i in range(4)]
            for i in range(NT):
                bk = 1 + (i % 3)
                for i2 in range(i):
                    k.op("pe", lambda: nc.tensor.matmul(ps[:, bk, 0:32], lhsT=ones_b[:], rhs=Mb[:, i2, :], start=(i2 == 0), stop=False),
                         rd=[rr, r_const], wr=[pbank[bk]], inc=False)
                k.op("pe", lambda: nc.tensor.matmul(ps[:, bk, 0:32], lhsT=U[:], rhs=Mb[:, i, :], start=(i == 0), stop=True),
                     rd=[rr, r_const], wr=[pbank[bk]])
                q = i % 4
                rq = r_sc[q]
                sl = sc[:, q, 0, :]
                slm = sc[:, q, 1, :]
                big = sc[:, q, 2, :]
                isA = sc[:, q, 3, :]
                wa = sc[:, q, 4, :]
                k.op("dve", lambda: V.tensor_tensor(out=sl, in0=ps[:, bk, 0:32], in1=offs, op=ALU.add), rd=[pbank[bk], rr], wr=[rq])
                k.op("dve", lambda: V.tensor_tensor(out=slm, in0=sl, in1=Mf[:, i, :], op=ALU.mult), rd=[rq, rr], wr=[rq])
                k.op("dve", lambda: V.tensor_reduce(out=slf[:, 1, i:i + 1], in_=slm, axis=AX.X, op=ALU.max), rd=[rq], wr=[rq, r_route])
                k.op("dve", lambda: V.tensor_scalar(out=big, in0=Mf[:, i, :], scalar1=-1.0e9, scalar2=1.0e9, op0=ALU.mult, op1=ALU.add),
                     rd=[rr], wr=[rq])
                k.op("dve", lambda: V.tensor_tensor(out=big, in0=big, in1=slm, op=ALU.add), rd=[rq], wr=[rq])
                k.op("dve", lambda: V.tensor_reduce(out=slf[:, 0, i:i + 1], in_=big, axis=AX.X, op=ALU.min), rd=[rq], wr=[rq, r_route])
                k.op("dve", lambda: V.tensor_scalar(out=isA, in0=big, scalar1=slf[:, 0, i:i + 1], scalar2=None, op0=ALU.is_equal), rd=[rq, r_route], wr=[rq])
                k.op("dve", lambda: V.tensor_tensor(out=wa, in0=isA, in1=comb_tok[:, i, :], op=ALU.mult), rd=[rq, r_ctok], wr=[rq])
                k.op("dve", lambda: V.tensor_reduce(out=wAB[:, 0, i:i + 1], in_=wa, axis=AX.X, op=ALU.add), rd=[rq], wr=[r_route])
                k.op("dve", lambda: V.tensor_reduce(out=wAB[:, 1, i:i + 1], in_=comb_tok[:, i, :], axis=AX.X, op=ALU.add), rd=[r_ctok], wr=[r_route])
                k.op("dve", lambda: V.tensor_tensor(out=wAB[:, 1, i:i + 1], in0=wAB[:, 1, i:i + 1], in1=wAB[:, 0, i:i + 1], op=ALU.subtract),
                     rd=[r_route], wr=[r_route])
            k.op("dve", lambda: V.tensor_copy(out=slotA_i[:], in_=slf[:, 0, :]), rd=[r_route], wr=[r_route])
            k.op("dve", lambda: V.tensor_copy(out=slotB_i[:], in_=slf[:, 1, :]), rd=[r_route], wr=[r_route])
            ht = [sb("ht%d" % i, [P, D], BF16, pstack) for i in range(2)]
            r_ht = [Res("ht0"), Res("ht1")]
            r_hsort = r_hsort_g
            zrow = sb("zrow", [P, D], BF16, pstack)
            r_z = Res("zrow")
            k.op("pool", lambda: G.memset(zrow[:], 0.0), wr=[r_z])
            if not hs_filled[0]:
                for j in range(NTL):
                    k.dma("sp", hsort[j * P:(j + 1) * P, :], zrow[:], rd=[r_z], wr=[r_hsort])
            for i in range(NT):
                b = i % 2
                k.dma("sp", ht[b][:], vtok[i * P:(i + 1) * P, :], rd=[r_htok], wr=[r_ht[b]])
                for sl_i in (slotA_i, slotB_i):
                    tok = k.dma_indirect(lambda s_: G.indirect_dma_start(out=hsort, out_offset=bass.IndirectOffsetOnAxis(ap=sl_i[:, i:i + 1], axis=0),
                                                                          in_=ht[b][:, :], in_offset=None), [r_ht[b], r_route], [r_hsort])
            wgt = [sb("wgt%d" % i, [P, KC * FE], BF16, pstack) for i in range(2)]
            wut = [sb("wut%d" % i, [P, KC * FE], BF16, pstack) for i in range(2)]
            wdt = [sb("wdt%d" % i, [P, 2 * D], BF16, pstack) for i in range(3)]
            r_wgu = [Res("wgu0"), Res("wgu1")]
            r_wdt = [Res("wdt%d" % i) for i in range(3)]
            hsb = [sb("hsb%d" % i, [P, D], BF16, pstack) for i in range(2)]
            r_hsb = [Res("hsb0"), Res("hsb1")]
            hsT = [sb("hsT%d" % i, [P, KC, P], BF16, pstack) for i in range(2)]
            r_hsT = [Res("hsT0"), Res("hsT1")]
            sg = [sb("ssg%d" % i, [P, 2 * P], BF16, pstack) for i in range(2)]
            r_sg = [Res("ssg0"), Res("ssg1")]
            aT = [sb("aT%d" % i, [P, 2 * P], BF16, pstack) for i in range(2)]
            r_aT = [Res("aT0"), Res("aT1")]
            ysb = [sb("ysb%d" % i, [P, D], F32, pstack) for i in range(2)]
            r_ysb = [Res("ysb0"), Res("ysb1")]

            def stA(j):
                b2_, b3_ = j % 2, j % 3
                k.dma_indirect(lambda s_: G.indirect_dma_start(out=wgt[b2_][:, :], out_offset=None, in_=wgb[l],
                                                               in_offset=bass.IndirectOffsetOnAxis(ap=widx[:, j:j + 1], axis=0)), [rr, r_wdram], [r_wgu[b2_]])
                k.dma_indirect(lambda s_: G.indirect_dma_start(out=wut[b2_][:, :], out_offset=None, in_=wub[l],
                                                               in_offset=bass.IndirectOffsetOnAxis(ap=widx[:, j:j + 1], axis=0)), [rr, r_wdram], [r_wgu[b2_]])
                k.dma_indirect(lambda s_: G.indirect_dma_start(out=wdt[b3_][:, :], out_offset=None, in_=wdb[l],
                                                               in_offset=bass.IndirectOffsetOnAxis(ap=widx[:, j:j + 1], axis=0)), [rr, r_wdram], [r_wdt[b3_]])
                k.dma("sp", hsb[b2_][:], hsort[j * P:(j + 1) * P, :], rd=[r_hsort], wr=[r_hsb[b2_]])
                bk = j % 2
                pb = ps[:, bk, :].bitcast(BF16)
                for c in range(KC):
                    k.op("pe", lambda: nc.tensor.transpose(out=pb[:, c * P:(c + 1) * P], in_=hsb[b2_][:, c * P:(c + 1) * P], identity=ident_b[:]),
                         rd=[r_hsb[b2_], r_const], wr=[pbank[bk]], inc=(c == KC - 1))
                k.op("dve", lambda: V.tensor_copy(out=hsT[b2_][:], in_=pb.rearrange("p (c n) -> p c n", n=P)), rd=[pbank[bk]], wr=[r_hsT[b2_]])

            def stB(j):
                b2_ = j % 2
                bk = 2 + j % 2
                n = 0
                for (wt, off) in ((wgt[b2_], 0), (wut[b2_], 2 * P)):
                    for fc in range(2):
                        for c in range(KC):
                            k.op("pe", lambda: nc.tensor.matmul(ps[:, bk, off + fc * P:off + (fc + 1) * P],
                                                               lhsT=wt[:, c * FE + fc * P:c * FE + (fc + 1) * P], rhs=hsT[b2_][:, c, :],
                                                               start=(n == 0), stop=(c == KC - 1), skip_group_check=True),
                                 rd=[r_wgu[b2_], r_hsT[b2_]], wr=[pbank[bk]], inc=(c == KC - 1 and fc == 1))
                            n += 1
                k.op("act", lambda: nc.scalar.activation(out=sg[b2_][:], in_=ps[:, bk, 0:2 * P], func=AF.Silu), rd=[pbank[bk]], wr=[r_sg[b2_]])
                k.op("dve", lambda: V.tensor_tensor(out=aT[b2_][:], in0=ps[:, bk, 2 * P:4 * P], in1=sg[b2_][:], op=ALU.mult),
                     rd=[pbank[bk], r_sg[b2_]], wr=[r_aT[b2_]])

            def stC(j):
                b2_, b3_ = j % 2, j % 3
                for half in range(2):
                    bk = 4 + 2 * (j % 2) + half
                    for fc in range(2):
                        k.op("pe", lambda: nc.tensor.matmul(ps[:, bk, :], lhsT=aT[b2_][:, fc * P:(fc + 1) * P],
                                                           rhs=wdt[b3_][:, fc * D + half * 512:fc * D + (half + 1) * 512],
                                                           start=(fc == 0), stop=(fc == 1)), rd=[r_aT[b2_], r_wdt[b3_]], wr=[pbank[bk]], inc=(fc == 1))
                    if half == 0:
                        k.op("act", lambda: nc.scalar.copy(out=ysb[b2_][:, 0:512], in_=ps[:, bk, :]), rd=[pbank[bk]], wr=[r_ysb[b2_]])
                    else:
                        k.op("dve", lambda: V.tensor_copy(out=ysb[b2_][:, 512:1024], in_=ps[:, bk, :]), rd=[pbank[bk]], wr=[r_ysb[b2_]])
                k.dma("sp", ysort[j * P:(j + 1) * P, :], ysb[b2_][:], rd=[r_ysb[b2_]], wr=[r_ysort])

            run_pipeline(NTL, [stA, stB, stC])
            hs_filled[0] = False
            if l + 1 < L:
                for j in range(NTL):
                    k.dma("sp", hsort[j * P:(j + 1) * P, :], zrow[:], rd=[r_z], wr=[r_hsort])
                hs_filled[0] = True

        def conv_c1(l, j, pstack, src_ap, r_src):
            w1 = sb("w1", [P, KC, 2 * D], BF16, pstack)
            r_w1 = Res("w1")
            load_w_bf16(w1[:], c_w1[j], KC, 2 * D, gain=g_mix[:, :, l], r_dst=r_w1)
            b1 = load_featvec("b1v%d" % l, c_b1[j].rearrange("(r d) -> r d", d=D), 2, pstack)
            zt = sb("zpad", [P, KC, 32], BF16, pstack)
            r_z = Res("zpad")
            k.op("pool", lambda: nc.gpsimd.memset(zt[:], 0.0), wr=[r_z])
            k.dma("pool", vscr[:, :, 0:32].rearrange("c p t -> p c t"), zt[:], rd=[r_z], wr=[r_vscr[0]])
            hs = HStage(pstack, flush=False)
            initial_zero_fill(pstack)
            pp = make_prepass(l, pstack, engines=("act", "dve")) if SPARSE else None
            xt = [sb("xt%d" % i, [P, D], F32, pstack) for i in range(3)]
            r_xt = [Res("xt%d" % i) for i in range(3)]
            sgb = [sb("sgb%d" % i, [P, 512], F32, pstack) for i in range(2)]
            r_sgb = [Res("sgb0"), Res("sgb1")]
            vb = [sb("vb%d" % i, [P, KC, 512], BF16, pstack) for i in range(2)]
            r_vb = [Res("vb0"), Res("vb1")]
            def st_A(I):
                for sub in range(4):
                    t = I * 4 + sub
                    xi = t % 3
                    k.dma("sp", xt[xi][:], src_ap[t * P:(t + 1) * P, :], rd=[r_src[I]], wr=[r_xt[xi]])
                    hs.norm_put(t, xt[xi][:], r_xt[xi], 7)
            def st_B(I):
                hi = I % 2
                hT_mt = hs.buf[hi]
                r_hmt = hs.res[hi]
                for cc in range(KC):
                    ba = 0 + 2 * (cc % 2)
                    bg = 1 + 2 * (cc % 2)
                    si = cc % 2
                    for c in range(KC):
                        k.op("pe", lambda: nc.tensor.matmul(ps[:, ba, :], lhsT=w1[:, c, cc * P:(cc + 1) * P], rhs=hT_mt[:, c, :],
                                                           start=(c == 0), stop=(c == KC - 1)), rd=[r_w1, r_hmt], wr=[pbank[ba]],
                             inc=(c == KC - 1))
                    for c in range(KC):
                        k.op("pe", lambda: nc.tensor.matmul(ps[:, bg, :], lhsT=w1[:, c, D + cc * P:D + (cc + 1) * P], rhs=hT_mt[:, c, :],
                                                           start=(c == 0), stop=(c == KC - 1)), rd=[r_w1, r_hmt], wr=[pbank[bg]],
                             inc=(c == KC - 1))
                    k.op("act", lambda: nc.scalar.activation(out=sgb[si][:], in_=ps[:, bg, :], func=AF.Sigmoid, bias=b1[:, cc, 1:2]),
                         rd=[pbank[bg], r_const], wr=[r_sgb[si]])
                    k.op("dve", lambda: nc.vector.scalar_tensor_tensor(out=vb[hi][:, cc, :], in0=ps[:, ba, :], scalar=b1[:, cc, 0:1],
                                                                       in1=sgb[si][:], op0=ALU.add, op1=ALU.mult),
                         rd=[pbank[ba], r_sgb[si], r_const], wr=[r_vb[hi]])
                k.dma("pool", vscr[:, :, 32 + I * 512:32 + (I + 1) * 512].rearrange("c p t -> p c t"), vb[hi][:],
                      rd=[r_vb[hi]], wr=[r_vscr[I + 1]])

            def st_P(I):
                if pp is not None:
                    pp(-(-97 // NM))

            run_pipeline(NM, [st_A, st_B, st_P])
            if pp is not None:
                pp.flush()

        def conv_c2(l, j, pstack, src_ap, r_src):
            w2 = sb("w2", [P, KC, D], BF16, pstack)
            r_w2 = Res("w2")
            load_w_bf16(w2[:], c_w2[j], KC, D, r_dst=r_w2)
            cv = sb("cv_vec", [P, KC, 40], F32, pstack)
            r_cv = Res("cv")
            with ExitStack() as st:
                rows = sb("cv_rows", [40, D], F32, st)
                r_rows = Res("rows")
                k.op("pool", lambda: nc.gpsimd.memset(rows[:], 0.0), wr=[r_rows])
                k.dma("sp", rows[0:CW, :], c_wdw[j], wr=[r_rows])
                k.dma("sp", rows[32:33, :], c_bdw[j:j + 1, :], wr=[r_rows])
                k.dma("sp", rows[33:34, :], c_lng[j:j + 1, :], wr=[r_rows])
                k.dma("sp", rows[34:35, :], c_lnb[j:j + 1, :], wr=[r_rows])
                for c in range(KC):
                    bk = c % 2
                    k.op("pe", lambda: nc.tensor.transpose(out=ps[:, bk, 0:36], in_=rows[0:36, c * P:(c + 1) * P],
                                                          identity=ident_f[0:36, 0:36]), rd=[r_rows, r_const], wr=[pbank[bk]])
                    k.op("dve", lambda: nc.vector.tensor_copy(out=cv[:, c, 0:36], in_=ps[:, bk, 0:36]), rd=[pbank[bk]], wr=[r_cv])
                k.barrier()
            b2bc = sb("b2bc", [P, D], F32, pstack)
            k.dma("sp", b2bc[:], c_b2[j].partition_broadcast(P), wr=[r_cv])
            diag = sb("diag", [P, KC, CW, P], BF16, pstack)
            r_diag = Res("diag")
            n = 0
            for c in range(KC):
                for t_ in range(CW):
                    if n % 2 == 0:
                        k.op("pool", lambda: nc.gpsimd.tensor_scalar(out=diag[:, c, t_, :], in0=ident_f[:], scalar1=cv[:, c, t_:t_ + 1],
                                                                     scalar2=1.0, op0=ALU.mult, op1=ALU.mult),
                             rd=[r_cv, r_const], wr=[r_diag])
                    else:
                        k.op("dve", lambda: nc.vector.tensor_scalar(out=diag[:, c, t_, :], in0=ident_f[:], scalar1=cv[:, c, t_:t_ + 1],
                                                                    scalar2=None, op0=ALU.mult), rd=[r_cv, r_const], wr=[r_diag])
                    n += 1
            prep = MoePrep(l, pstack, (2, 3, 4))
            ring = [sb("ring%d" % i, [P, KC, 32 + 512], BF16, pstack) for i in range(1)] * 2
            r_ring = [Res("ring0")] * 2
            xt = [sb("xt%d" % i, [P, D], F32, pstack) for i in range(3)]
            r_xt = [Res("xt%d" % i) for i in range(3)]
            ybuf = sb("ybuf", [P, KC, 512], F32, pstack)
            r_y = Res("ybuf")
            yb = [sb("yb%d" % i, [P, 512], BF16, pstack) for i in range(2)]
            r_yb = [Res("yb0"), Res("yb1")]
            ysq = [sb("ysq%d" % i, [P, 512], BF16, pstack) for i in range(2)]
            r_ysq = [Res("ysq0"), Res("ysq1")]
            mean = sb("mean", [P, 512], F32, pstack)
            rstd_t = sb("rstd_t", [P, 512], F32, pstack)
            r_ln = Res("ln")
            zt = sb("ztmp", [P, 2, 512], F32, pstack)
            r_zt = [Res("zt0"), Res("zt1")]
            zT = sb("zT", [P, KC, 512], BF16, pstack)
            r_zT = Res("zT")
            xcnt = [0]
            for I in range(NM):
                ri = I % 2
                k.dma("sp", ring[ri][:], vscr[:, :, I * 512:I * 512 + 544].rearrange("c p t -> p c t"),
                      rd=[r_vscr[I], r_vscr[I + 1]], wr=[r_ring[ri]])
                pend_stats = []
                for cc in range(KC):
                    bc_ = 0 + (cc % 2)
                    si = cc % 2
                    for t_ in range(CW):
                        k.op("pe", lambda: nc.tensor.matmul(ps[:, bc_, :], lhsT=diag[:, cc, t_, :], rhs=ring[ri][:, cc, 2 + t_:2 + t_ + 512],
                                                           start=(t_ == 0), stop=(t_ == CW - 1)), rd=[r_diag, r_ring[ri]], wr=[pbank[bc_]],
                             inc=(t_ == CW - 1))
                    k.op("act", lambda: nc.scalar.activation(out=ybuf[:, cc, :], in_=ps[:, bc_, :], func=AF.Identity, bias=cv[:, cc, 32:33]),
                         rd=[pbank[bc_], r_cv], wr=[r_y])
                    k.op("act", lambda: nc.scalar.activation(out=ysq[si][:], in_=ps[:, bc_, :], func=AF.Square, bias=cv[:, cc, 32:33]),
                         rd=[pbank[bc_], r_cv], wr=[r_ysq[si]])
                    k.op("pool", lambda: nc.gpsimd.tensor_copy(out=yb[si][:], in_=ybuf[:, cc, :]), rd=[r_y], wr=[r_yb[si]])

                    def stats(c2):
                        s2 = c2 % 2
                        k.op("pe", lambda: nc.tensor.matmul(ps[:, 6, :], lhsT=ones_b[:], rhs=yb[s2][:], start=(c2 == 0), stop=(c2 == KC - 1),
                                                           skip_group_check=True), rd=[r_yb[s2], r_const], wr=[pbank[6]], inc=(c2 == KC - 1))
                        k.op("pe", lambda: nc.tensor.matmul(ps[:, 7, :], lhsT=ones_b[:], rhs=ysq[s2][:], start=(c2 == 0), stop=(c2 == KC - 1),
                                                           skip_group_check=True), rd=[r_ysq[s2], r_const], wr=[pbank[7]], inc=True)
                    pend_stats.append(cc)
                    if len(pend_stats) > 1:
                        stats(pend_stats.pop(0))
                while pend_stats:
                    stats(pend_stats.pop(0))
                k.op("act", lambda: nc.scalar.activation(out=mean[:], in_=ps[:, 6, :], func=AF.Copy, scale=1.0 / D), rd=[pbank[6]], wr=[r_ln])
                k.op("act", lambda: nc.scalar.activation(out=rstd_t[:], in_=ps[:, 6, :], func=AF.Square, scale=1.0 / D), rd=[pbank[6]], wr=[r_ln])
                k.op("dve", lambda: nc.vector.scalar_tensor_tensor(out=rstd_t[:], in0=ps[:, 7, :], scalar=1.0 / D, in1=rstd_t[:],
                                                                   op0=ALU.mult, op1=ALU.subtract), rd=[pbank[7], r_ln], wr=[r_ln])
                k.op("act", lambda: nc.scalar.activation(out=rstd_t[:], in_=rstd_t[:], func=AF.Sqrt, bias=eps_col[:, 0:1]),
                     rd=[r_ln, r_const], wr=[r_ln])
                k.op("dve", lambda: nc.vector.reciprocal(out=rstd_t[:], in_=rstd_t[:]), rd=[r_ln], wr=[r_ln])
                for cc in range(KC):
                    zi = cc % 2
                    k.op("pool", lambda: nc.gpsimd.tensor_tensor(out=zt[:, zi, :], in0=ybuf[:, cc, :], in1=mean[:], op=ALU.subtract),
                         rd=[r_y, r_ln], wr=[r_zt[zi]])
                    k.op("dve", lambda: nc.vector.tensor_tensor(out=zt[:, zi, :], in0=zt[:, zi, :], in1=rstd_t[:], op=ALU.mult),
                         rd=[r_zt[zi], r_ln], wr=[r_zt[zi]])
                    k.op("act", lambda: nc.scalar.activation(out=zT[:, cc, :], in_=zt[:, zi, :], func=AF.Silu, scale=cv[:, cc, 33:34],
                                                            bias=cv[:, cc, 34:35]), rd=[r_zt[zi], r_cv], wr=[r_zT])
                def st_X(sub, I=I):
                    t = I * 4 + sub
                    xi = t % 3
                    k.dma("sp", xt[xi][:], src_ap[t * P:(t + 1) * P, :], rd=[r_src[I]], wr=[r_xt[xi]])
                    k.op("pool", lambda: nc.gpsimd.tensor_tensor(out=xt[xi][:], in0=xt[xi][:], in1=b2bc[:], op=ALU.add),
                         rd=[r_xt[xi], r_cv], wr=[r_xt[xi]])
                    for half in range(2):
                        bk = 0 + half
                        for cc in range(KC):
                            k.op("pe", lambda: nc.tensor.matmul(ps[:, bk, :], lhsT=zT[:, cc, sub * P:(sub + 1) * P],
                                                               rhs=w2[:, cc, half * 512:(half + 1) * 512], start=(cc == 0), stop=(cc == KC - 1)),
                                 rd=[r_zT, r_w2], wr=[pbank[bk]], inc=(cc == KC - 1))
                        k.op("dve", lambda: nc.vector.tensor_tensor(out=xt[xi][:, half * 512:(half + 1) * 512], in0=ps[:, bk, :],
                                                                    in1=xt[xi][:, half * 512:(half + 1) * 512], op=ALU.add),
                             rd=[pbank[bk], r_xt[xi]], wr=[r_xt[xi]])
                    k.dma("pool", xres[t * P:(t + 1) * P, :], xt[xi][:], rd=[r_xt[xi]], wr=[r_xres[I]])

                run_pipeline(4, [st_X, lambda sub, I=I: prep.p1(xt[(I * 4 + sub) % 3][:], r_xt[(I * 4 + sub) % 3], I * 4 + sub),
                                 lambda sub, I=I: prep.p2(I * 4 + sub)])

        def ple_phase(l, pstack, last, next_kind=None):
            wpg = sb("wpg", [P, KC, D], BF16, pstack)
            wpp = sb("wpp", [P, 2, D], BF16, pstack)
            r_wp = Res("wp")
            load_w_bf16(wpg[:], ple_wg[l], KC, D, gain=g_ple[:, :, l], r_dst=r_wp)
            load_w_bf16(wpp[:], ple_wp[l], 2, D, r_dst=r_wp)
            if last:
                fn_bc = sb("fn_bc", [P, D], F32, pstack)
                k.dma("sp", fn_bc[:], fin_norm.partition_broadcast(P), wr=[r_wp])
            hs2 = None
            if next_kind == "attn":
                hs2 = HStage(pstack)
            NX = 7
            rs_d = {}
            xt = [sb("pxt%d" % i, [P, D], F32, pstack) for i in range(NX)]
            r_xt = [Res("pxt%d" % i) for i in range(NX)]
            pt = [sb("ppt%d" % i, [P, PLE], F32, pstack) for i in range(4)]
            r_pt = [Res("ppt%d" % i) for i in range(4)]
            xnb = [sb("pxnb%d" % i, [P, D + PLE], BF16, pstack) for i in range(2)]
            r_xnb = [Res("pxnb0"), Res("pxnb1")]
            hT = [sb("phT%d" % i, [P, KC + 2, P], BF16, pstack) for i in range(2)]
            r_h = [Res("phT0"), Res("phT1")]
            sig = [sb("psig%d" % i, [P, D], F32, pstack) for i in range(2)]
            r_sig = [Res("psig0"), Res("psig1")]
            yo = [sb("pyo%d" % i, [P, D], F32, pstack) for i in range(2)]
            r_yo = [Res("pyo0"), Res("pyo1")]
            if SPARSE:
                yab = [sb("yab%d" % i, [P, 2, D], F32, pstack) for i in range(2)]
                r_yab = [Res("yab0"), Res("yab1")]
            def st_A(t):
                xi = t % NX
                i2 = t % 2
                mt = t // 4
                k.dma("sp", xt[xi][:], xres[t * P:(t + 1) * P, :], rd=[r_xres[mt]], wr=[r_xt[xi]])
                k.dma("sp", pt[t % 4][:], p_in[l, t * P:(t + 1) * P, :], wr=[r_pt[t % 4]])
                if SPARSE:
                    for q_, sl_i in enumerate((slotA_i, slotB_i)):
                        k.dma_indirect(lambda s_: nc.gpsimd.indirect_dma_start(out=yab[i2][:, q_, :], out_offset=None, in_=ysort,
                                                                               in_offset=bass.IndirectOffsetOnAxis(ap=sl_i[:, t:t + 1], axis=0)),
                                       [r_ysort, r_route], [r_yab[i2]])

            def st_A1(t):
                xi = t % NX
                i2 = t % 2
                mt = t // 4
                if SPARSE:
                    for q_ in range(2):
                        k.op("dve", lambda: nc.vector.scalar_tensor_tensor(out=xt[xi][:], in0=yab[i2][:, q_, :], scalar=wAB[:, q_, t:t + 1],
                                                                           in1=xt[xi][:], op0=ALU.mult, op1=ALU.add),
                             rd=[r_yab[i2], r_xt[xi], r_route], wr=[r_xt[xi]])
                rs_d[t % 4] = rms_rstd(xt[xi][:], r_xt[xi])

            def st_Ab(t):
                xi = t % NX
                i2 = t % 2
                rstd, r_rs = rs_d[t % 4]
                k.op("dve", lambda: nc.vector.tensor_scalar(out=xnb[i2][:, 0:D], in0=xt[xi][:], scalar1=rstd, scalar2=None, op0=ALU.mult),
                     rd=[r_xt[xi], r_rs], wr=[r_xnb[i2]])
                k.op("pool", lambda: nc.gpsimd.tensor_copy(out=xnb[i2][:, D:D + PLE], in_=pt[t % 4][:]), rd=[r_pt[t % 4]], wr=[r_xnb[i2]])
            def st_A2(t):
                xi = t % NX
                i2 = t % 2
                bT = 6
                pb = ps[:, bT, :].bitcast(BF16)
                for c in range(KC):
                    k.op("pe", lambda: nc.tensor.transpose(out=pb[:, c * P:(c + 1) * P], in_=xnb[i2][:, c * P:(c + 1) * P],
                                                          identity=ident_b[:]), rd=[r_xnb[i2], r_const], wr=[pbank[bT]], inc=(c == KC - 1))
                k.op("act", lambda: nc.scalar.copy(out=hT[i2][:, 0:KC, :], in_=pb.rearrange("p (c n) -> p c n", n=P)),
                     rd=[pbank[bT]], wr=[r_h[i2]])
                bT2 = 4 + (t % 2)
                pb2 = ps[:, bT2, :].bitcast(BF16)
                for c in range(2):
                    k.op("pe", lambda: nc.tensor.transpose(out=pb2[:, c * P:(c + 1) * P], in_=xnb[i2][:, D + c * P:D + (c + 1) * P],
                                                          identity=ident_b[:]), rd=[r_xnb[i2], r_const], wr=[pbank[bT2]], inc=(c == 1))
                k.op("dve", lambda: nc.vector.tensor_copy(out=hT[i2][:, KC:KC + 2, :], in_=pb2[:, 0:2 * P].rearrange("p (c n) -> p c n", n=P)),
                     rd=[pbank[bT2]], wr=[r_h[i2]])
            def st_B(t):
                xi = t % NX
                i2 = t % 2
                mt = t // 4
                for half in range(2):
                    bgk = 0 + half
                    bpk = 2 + half
                    hs_ = slice(half * 512, (half + 1) * 512)
                    for c in range(KC):
                        k.op("pe", lambda: nc.tensor.matmul(ps[:, bgk, :], lhsT=hT[i2][:, c, :], rhs=wpg[:, c, hs_], start=(c == 0),
                                                           stop=(c == KC - 1)), rd=[r_h[i2], r_wp], wr=[pbank[bgk]], inc=(c == KC - 1))
                    for c in range(2):
                        k.op("pe", lambda: nc.tensor.matmul(ps[:, bpk, :], lhsT=hT[i2][:, KC + c, :], rhs=wpp[:, c, hs_], start=(c == 0),
                                                           stop=(c == 1)), rd=[r_h[i2], r_wp], wr=[pbank[bpk]], inc=(c == 1))
                    k.op("act", lambda: nc.scalar.activation(out=sig[i2][:, hs_], in_=ps[:, bgk, :], func=AF.Sigmoid),
                         rd=[pbank[bgk]], wr=[r_sig[i2]])
                    k.op("dve", lambda: nc.vector.tensor_tensor(out=sig[i2][:, hs_], in0=ps[:, bpk, :], in1=sig[i2][:, hs_], op=ALU.mult),
                         rd=[pbank[bpk], r_sig[i2]], wr=[r_sig[i2]])
            def st_B2(t):
                xi = t % NX
                i2 = t % 2
                mt = t // 4
                k.op("pool", lambda: nc.gpsimd.tensor_tensor(out=xt[xi][:], in0=xt[xi][:], in1=sig[i2][:], op=ALU.add),
                     rd=[r_xt[xi], r_sig[i2]], wr=[r_xt[xi]])
                if last:
                    rstd2, r_rs2 = rms_rstd(xt[xi][:], r_xt[xi])
                    k.op("dve", lambda: nc.vector.scalar_tensor_tensor(out=yo[i2][:], in0=xt[xi][:], scalar=rstd2, in1=fn_bc[:],
                                                                       op0=ALU.mult, op1=ALU.mult), rd=[r_xt[xi], r_rs2, r_wp], wr=[r_yo[i2]])
                    k.dma("pool", y_out[t * P:(t + 1) * P, :], yo[i2][:], rd=[r_yo[i2]], wr=[r_xres[mt]])
                else:
                    k.dma("pool", xres[t * P:(t + 1) * P, :], xt[xi][:], rd=[r_xt[xi]], wr=[r_xres[mt]])
                    if hs2 is not None:
                        hs2.norm_put(t, xt[xi][:], r_xt[xi], 7)

            run_pipeline(NT, [st_A, st_A1, st_Ab, st_A2, st_B, st_B2])

        def attn_a0(pstack, src_ap, r_src):
            initial_zero_fill(pstack)
            hs = HStage(pstack)
            xt = [sb("a0xt%d" % i, [P, D], F32, pstack) for i in range(3)]
            r_xt = [Res("a0xt%d" % i) for i in range(3)]
            for t in range(NT):
                xi = t % 3
                k.dma("sp", xt[xi][:], src_ap[t * P:(t + 1) * P, :], rd=[r_src[t // 4]], wr=[r_xt[xi]])
                hs.norm_put(t, xt[xi][:], r_xt[xi], 7)

        def attn_a1(l, j, pstack):
            wq = sb("wqkv", [P, KC, 3 * D], BF16, pstack)
            r_wq = Res("wqkv")
            load_w_bf16(wq[:], a_wqkv[j], KC, 3 * D, gain=g_mix[:, :, l], r_dst=r_wq)
            cosr = sb("cosr", [P, NT, 64], F32, pstack)
            sinr = sb("sinr", [P, NT, 64], F32, pstack)
            r_rope = Res("rope")
            with ExitStack() as st:
                pos_i = sb("pos_i", [NT, P], I32, st)
                pos_f = sb("pos_f", [NT, P], F32, st)
                posT = sb("posT", [P, NT], F32, st)
                ang = sb("ang", [P, 2, NT, 8], F32, st)
                tmpa = sb("tmpa", [P, 2, NT, 8], F32, st)
                tmpi = sb("tmpi", [P, 2, NT, 8], I32, st)
                r_p = Res("pos")
                k.dma("sp", pos_i[:], pos_in, wr=[r_p])
                k.op("dve", lambda: nc.vector.tensor_copy(out=pos_f[:], in_=pos_i[:]), rd=[r_p], wr=[r_p])
                k.op("pe", lambda: nc.tensor.transpose(out=ps[:, 0, 0:NT], in_=pos_f[:], identity=ident_f[0:NT, 0:NT]),
                     rd=[r_p, r_const], wr=[pbank[0]])
                k.op("dve", lambda: nc.vector.tensor_copy(out=posT[:], in_=ps[:, 0, 0:NT]), rd=[pbank[0]], wr=[r_p])
                V = nc.vector
                for i in range(8):
                    inv = THETA ** (-(2.0 * i) / ROPE)
                    k.op("dve", lambda: V.tensor_scalar(out=ang[:, 0, :, i], in0=posT[:], scalar1=float(inv), scalar2=None, op0=ALU.mult),
                         rd=[r_p], wr=[r_p])
                k.op("dve", lambda: V.tensor_scalar(out=ang[:, 1], in0=ang[:, 0], scalar1=float(math.pi / 2), scalar2=None, op0=ALU.add),
                     rd=[r_p], wr=[r_p])
                TWO_PI = float(2 * math.pi)
                A = ang[:].rearrange("p a t i -> p (a t i)")
                T_ = tmpa[:].rearrange("p a t i -> p (a t i)")
                TI = tmpi[:].rearrange("p a t i -> p (a t i)")
                k.op("dve", lambda: V.tensor_scalar(out=T_, in0=A, scalar1=float(1.0 / TWO_PI), scalar2=None, op0=ALU.mult), rd=[r_p], wr=[r_p])
                k.op("dve", lambda: V.tensor_copy(out=TI, in_=T_), rd=[r_p], wr=[r_p])
                k.op("dve", lambda: V.tensor_copy(out=T_, in_=TI), rd=[r_p], wr=[r_p])
                k.op("dve", lambda: V.scalar_tensor_tensor(out=A, in0=T_, scalar=-TWO_PI, in1=A, op0=ALU.mult, op1=ALU.add), rd=[r_p], wr=[r_p])
                k.op("dve", lambda: V.tensor_scalar(out=T_, in0=A, scalar1=float(math.pi), scalar2=-TWO_PI, op0=ALU.is_gt, op1=ALU.mult),
                     rd=[r_p], wr=[r_p])
                k.op("dve", lambda: V.tensor_tensor(out=A, in0=A, in1=T_, op=ALU.add), rd=[r_p], wr=[r_p])
                k.op("dve", lambda: V.tensor_scalar(out=T_, in0=A, scalar1=float(-math.pi), scalar2=TWO_PI, op0=ALU.is_lt, op1=ALU.mult),
                     rd=[r_p], wr=[r_p])
                k.op("dve", lambda: V.tensor_tensor(out=A, in0=A, in1=T_, op=ALU.add), rd=[r_p], wr=[r_p])
                k.op("dve", lambda: V.tensor_scalar(out=A, in0=A, scalar1=float(math.pi), scalar2=float(-math.pi), op0=ALU.min, op1=ALU.max),
                     rd=[r_p], wr=[r_p])
                k.op("act", lambda: nc.scalar.activation(out=T_, in_=A, func=AF.Sin), rd=[r_p], wr=[r_p])
                for r8 in range(8):
                    k.op("dve", lambda: V.tensor_copy(out=sinr[:, :, r8 * 8:(r8 + 1) * 8], in_=tmpa[:, 0]), rd=[r_p], wr=[r_rope])
                    k.op("dve", lambda: V.tensor_copy(out=cosr[:, :, r8 * 8:(r8 + 1) * 8], in_=tmpa[:, 1]), rd=[r_p], wr=[r_rope])
                k.barrier()
            if debug == "a1r":
                return
            hT = [sb("a1hT%d" % i, [P, KC, 512], BF16, pstack) for i in range(2)]
            r_hT = [Res("a1hT0"), Res("a1hT1")]
            qk = [sb("qk%d" % i, [P, 2 * D], BF16, pstack) for i in range(2)]
            r_qk = [Res("qk0"), Res("qk1")]
            vt = [sb("vt%d" % i, [P, D], BF16, pstack) for i in range(2)]
            r_vt = [Res("vt0"), Res("vt1")]
            rtmp = sb("rtmp", [P, 2, 4, 64], F32, pstack)
            r_rt = [Res("rtmp0"), Res("rtmp1")]
            qst = [sb("qst%d" % i, [P, NH, 512], BF16, pstack) for i in range(2)]
            kst = [sb("kst%d" % i, [P, NH, 512], BF16, pstack) for i in range(2)]
            r_qst = [Res("qst0"), Res("qst1")]
            r_kst = [Res("kst0"), Res("kst1")]
            V = nc.vector
            cnt = [0]

            def st_A(t):
                I, sub = t // 4, t % 4
                hi = I % 2
                i2 = t % 2
                if sub == 0:
                    k.dma("sp", hT[hi][:], hscr[:, :, I * 512:(I + 1) * 512].rearrange("c p t -> p c t"), rd=[r_hscr[I]], wr=[r_hT[hi]])
                for jc in range(6):
                    bk = cnt[0] % 4
                    cnt[0] += 1
                    for c in range(KC):
                        k.op("pe", lambda: nc.tensor.matmul(ps[:, bk, :], lhsT=hT[hi][:, c, sub * P:(sub + 1) * P],
                                                           rhs=wq[:, c, jc * 512:(jc + 1) * 512], start=(c == 0), stop=(c == KC - 1)),
                             rd=[r_hT[hi], r_wq], wr=[pbank[bk]], inc=(c == KC - 1))
                    if jc >= 4:
                        dstv = vt[i2][:, (jc - 4) * 512:(jc - 3) * 512]
                        k.op("act", lambda: nc.scalar.copy(out=dstv, in_=ps[:, bk, :]), rd=[pbank[bk]], wr=[r_vt[i2]])
                        continue
                    dst = qk[i2][:, jc * 512:(jc + 1) * 512]
                    k.op("act", lambda: nc.scalar.copy(out=dst, in_=ps[:, bk, :]), rd=[pbank[bk]], wr=[r_qk[i2]])
                    ri = cnt[0] % 2
                    pv = ps[:, bk, :].rearrange("p (s d) -> p s d", d=64)
                    dv = dst.rearrange("p (s d) -> p s d", d=64)
                    x1 = pv[:, :, 0:8]
                    x2 = pv[:, :, 8:16]
                    cs = cosr[:, t, :].rearrange("p (s i) -> p s i", i=8)
                    sn = sinr[:, t, :].rearrange("p (s i) -> p s i", i=8)
                    T4 = rtmp[:, ri]
                    a_ = T4[:, 0].rearrange("p (s i) -> p s i", i=8)
                    b_ = T4[:, 1].rearrange("p (s i) -> p s i", i=8)
                    c_ = T4[:, 2].rearrange("p (s i) -> p s i", i=8)
                    d_ = T4[:, 3].rearrange("p (s i) -> p s i", i=8)
                    k.op("dve", lambda: V.tensor_tensor(out=a_, in0=x1, in1=cs, op=ALU.mult), rd=[pbank[bk], r_rope], wr=[r_rt[ri]])
                    k.op("dve", lambda: V.tensor_tensor(out=b_, in0=x2, in1=sn, op=ALU.mult), rd=[pbank[bk], r_rope], wr=[r_rt[ri]])
                    k.op("dve", lambda: V.tensor_tensor(out=c_, in0=x2, in1=cs, op=ALU.mult), rd=[pbank[bk], r_rope], wr=[r_rt[ri]])
                    k.op("dve", lambda: V.tensor_tensor(out=d_, in0=x1, in1=sn, op=ALU.mult), rd=[pbank[bk], r_rope], wr=[r_rt[ri]])
                    k.op("pool", lambda: nc.gpsimd.tensor_tensor(out=dv[:, :, 0:8], in0=a_, in1=b_, op=ALU.subtract),
                         rd=[r_rt[ri]], wr=[r_qk[i2]])
                    k.op("pool", lambda: nc.gpsimd.tensor_tensor(out=dv[:, :, 8:16], in0=c_, in1=d_, op=ALU.add),
                         rd=[r_rt[ri]], wr=[r_qk[i2]])

            def st_B(t):
                I, sub = t // 4, t % 4
                hi = I % 2
                i2 = t % 2
                k.dma("pool", vtok[t * P:(t + 1) * P, :], vt[i2][:], rd=[r_vt[i2]], wr=[r_vscr[0]])
                for which in range(2):
                    bk = 4 + which * 2 + (t % 2)
                    pb = ps[:, bk, :].bitcast(BF16)
                    for h in range(NH):
                        k.op("pe", lambda: nc.tensor.transpose(out=pb[:, h * P:(h + 1) * P],
                                                              in_=qk[i2][:, which * D + h * P:which * D + (h + 1) * P], identity=ident_b[:]),
                             rd=[r_qk[i2], r_const], wr=[pbank[bk]], inc=(h == NH - 1))
                    stg = (qst if which == 0 else kst)[hi]
                    r_stg = (r_qst if which == 0 else r_kst)[hi]
                    if which == 0:
                        k.op("dve", lambda: V.tensor_copy(out=stg[:, :, sub * P:(sub + 1) * P], in_=pb.rearrange("p (h n) -> p h n", n=P)),
                             rd=[pbank[bk]], wr=[r_stg])
                    else:
                        k.op("act", lambda: nc.scalar.copy(out=stg[:, :, sub * P:(sub + 1) * P], in_=pb.rearrange("p (h n) -> p h n", n=P)),
                             rd=[pbank[bk]], wr=[r_stg])
                if sub == 3:
                    k.dma("pool", qscr[:, :, I * 512:(I + 1) * 512].rearrange("h p t -> p h t"), qst[hi][:], rd=[r_qst[hi]], wr=[r_vscr[0]])
                    k.dma("pool", kscr[:, :, I * 512:(I + 1) * 512].rearrange("h p t -> p h t"), kst[hi][:], rd=[r_kst[hi]], wr=[r_vscr[0]])

            run_pipeline(NT, [st_A, st_B])

        def attn_a2(l, j, pstack, o_all, r_o, lam_init):
            V = nc.vector
            lamt = sb("lamt", [P, 4 * DH + 8], F32, pstack)
            r_lam = Res("lam")
            k.dma("sp", lamt[:, 0:4 * DH], a_lam[j].partition_broadcast(P), wr=[r_lam])
            for q in range(2):
                k.op("dve", lambda: V.tensor_tensor(out=lamt[:, 2 * q * DH:(2 * q + 1) * DH], in0=lamt[:, 2 * q * DH:(2 * q + 1) * DH],
                                                    in1=lamt[:, (2 * q + 1) * DH:(2 * q + 2) * DH], op=ALU.mult), rd=[r_lam], wr=[r_lam])
                k.op("dve", lambda: V.tensor_reduce(out=lamt[:, 4 * DH + q:4 * DH + q + 1], in_=lamt[:, 2 * q * DH:(2 * q + 1) * DH],
                                                    axis=AX.X, op=ALU.add), rd=[r_lam], wr=[r_lam])
            k.op("act", lambda: nc.scalar.activation(out=lamt[:, 4 * DH + 2:4 * DH + 4], in_=lamt[:, 4 * DH:4 * DH + 2], func=AF.Exp),
                 rd=[r_lam], wr=[r_lam])
            nlam = lamt[:, 4 * DH + 4:4 * DH + 5]
            k.op("dve", lambda: V.tensor_tensor(out=nlam, in0=lamt[:, 4 * DH + 3:4 * DH + 4], in1=lamt[:, 4 * DH + 2:4 * DH + 3],
                                                op=ALU.subtract), rd=[r_lam], wr=[r_lam])
            k.op("dve", lambda: V.tensor_scalar(out=nlam, in0=nlam, scalar1=float(-lam_init), scalar2=None, op0=ALU.add), rd=[r_lam], wr=[r_lam])
            gsub = sb("gsub", [P, DV], F32, pstack)
            k.dma("sp", gsub[:], a_subln[j].partition_broadcast(P), wr=[r_lam])
            k.op("dve", lambda: V.tensor_scalar(out=gsub[:], in0=gsub[:], scalar1=float(1.0 - lam_init), scalar2=None, op0=ALU.mult),
                 rd=[r_lam], wr=[r_lam])
            QT = [sb("QT%d" % i, [P, 2, S], BF16, pstack) for i in range(2)]
            KT = [sb("KT%d" % i, [P, S], BF16, pstack) for i in range(2)]
            Va = [sb("Va%d" % i, [P, NT, 132], BF16, pstack) for i in range(2)]
            r_hd = [Res("hd0"), Res("hd1")]
            eT = [sb("eT%d" % i, [P, 2, 512], BF16, pstack) for i in range(3)]
            r_eT = [Res("eT%d" % i) for i in range(3)]
            of = sb("of", [P, 2, DV], F32, pstack)
            r_of = [Res("of0"), Res("of1")]
            fs = sb("fs", [P, 2, 8], F32, pstack)
            for i in range(2):
                k.op("pool", lambda: nc.gpsimd.memset(Va[i][:, :, 128:132], 1.0), wr=[r_hd[i]])
            ecnt = [0]
            scnt = [0]
            fcnt = [0]
            r_acc = [pbank[4 + i // 2] for i in range(8)]

            def load_head(h):
                bi = h % 2
                k.dma("sp", QT[bi][:, 0, :], qscr[h], rd=[r_vscr[0]], wr=[r_hd[bi]])
                k.dma("sp", QT[bi][:, 1, :], qscr[h], rd=[r_vscr[0]], wr=[r_hd[bi]])
                k.op("pool", lambda: nc.gpsimd.memset(QT[bi][64:128, 0, :], 0.0), wr=[r_hd[bi]])
                k.op("pool", lambda: nc.gpsimd.memset(QT[bi][0:64, 1, :], 0.0), wr=[r_hd[bi]])
                k.dma("sp", KT[bi][:], kscr[h], rd=[r_vscr[0]], wr=[r_hd[bi]])
                k.dma("sp", Va[bi][:, :, 0:DV], vtok[:, h * DV:(h + 1) * DV].rearrange("(j p) d -> p j d", p=P),
                      rd=[r_vscr[0]], wr=[r_hd[bi]])

            load_head(0)
            steps = [(h, I, jt) for h in range(NH) for I in range(NM) for jt in range(4 * I + 4)]
            st_info = {}

            def acc_ap(rp, c):
                return 4 + rp, c * 129

            def emit_S(n):
                h, I, jt = steps[n]
                bi = h % 2
                r = jt - 4 * I
                q0 = max(r, 0) * P
                sset = n % 2
                sb0 = 2 * sset
                for c in range(2):
                    k.op("pe", lambda: nc.tensor.matmul(ps[:, sb0 + c, q0:512], lhsT=KT[bi][:, jt * P:(jt + 1) * P],
                                                       rhs=QT[bi][:, c, I * 512 + q0:(I + 1) * 512], start=True, stop=True),
                         rd=[r_hd[bi]], wr=[pbank[sb0 + c]])
                ei = n % 3
                k.op("act", lambda: nc.scalar.activation(out=eT[ei][:, :, q0:512], in_=ps[:, sb0:sb0 + 2, q0:512], func=AF.Exp,
                                                        scale=float(DH ** -0.5)),
                     rd=[pbank[sb0], pbank[sb0 + 1]], wr=[r_eT[ei]])
                if r >= 0:
                    k.op("pool", lambda: nc.gpsimd.affine_select(out=eT[ei][:, :, q0:q0 + P], in_=eT[ei][:, :, q0:q0 + P],
                                                                 pattern=[[0, 2], [1, P]], compare_op=ALU.is_ge, fill=0.0, base=0,
                                                                 channel_multiplier=-1), rd=[r_eT[ei]], wr=[r_eT[ei]])

            def emit_V(n):
                h, I, jt = steps[n]
                bi = h % 2
                if I == 0 and jt == 0 and h + 1 < NH:
                    load_head(h + 1)
                r = jt - 4 * I
                ei = n % 3
                for rp in range(max(r, 0), 4):
                    for c in range(2):
                        bk, off = acc_ap(rp, c)
                        st = (jt == 0 and c == 0)
                        last = (jt == 4 * I + rp)
                        k.op("pe", lambda: nc.tensor.matmul(ps[:, bk, off:off + 129], lhsT=eT[ei][:, c, rp * P:(rp + 1) * P],
                                                           rhs=Va[bi][:, jt, 0:129], start=st, stop=last, skip_group_check=True),
                             rd=[r_eT[ei], r_hd[bi]], wr=[pbank[bk]], inc=(last and c == 1) or (rp == 3 and c == 1))
                if r >= 0:
                    t = 4 * I + r
                    fi = fcnt[0] % 2
                    fcnt[0] += 1
                    b0_, o0 = acc_ap(r, 0)
                    b1_, o1 = acc_ap(r, 1)
                    F = fs[:, fi, :]
                    rf = r_of[fi]
                    ra = pbank[b0_]
                    k.op("dve", lambda: V.reciprocal(out=F[:, 0:1], in_=ps[:, b0_, o0 + 128:o0 + 129]), rd=[ra], wr=[rf])
                    k.op("dve", lambda: V.reciprocal(out=F[:, 1:2], in_=ps[:, b1_, o1 + 128:o1 + 129]), rd=[ra], wr=[rf])
                    k.op("dve", lambda: V.tensor_tensor(out=F[:, 2:3], in0=F[:, 1:2], in1=nlam, op=ALU.mult), rd=[rf, r_lam], wr=[rf])
                    k.op("dve", lambda: V.tensor_scalar(out=of[:, fi, :], in0=ps[:, b0_, o0:o0 + 128], scalar1=F[:, 0:1], scalar2=None,
                                                        op0=ALU.mult), rd=[ra, rf], wr=[rf])
                    k.op("dve", lambda: V.scalar_tensor_tensor(out=of[:, fi, :], in0=ps[:, b1_, o1:o1 + 128], scalar=F[:, 2:3],
                                                               in1=of[:, fi, :], op0=ALU.mult, op1=ALU.add), rd=[ra, rf], wr=[rf])
                    k.op("pool", lambda: nc.gpsimd.tensor_tensor(out=sq[:, fi, :], in0=of[:, fi, :], in1=of[:, fi, :], op=ALU.mult),
                         rd=[rf], wr=[r_sq[fi]])
                    k.op("dve", lambda: V.tensor_reduce(out=F[:, 3:4], in_=sq[:, fi, :], axis=AX.X, op=ALU.add), rd=[r_sq[fi]], wr=[rf])
                    k.op("dve", lambda: V.tensor_scalar(out=F[:, 4:5], in0=F[:, 3:4], scalar1=1.0 / DV, scalar2=EPS, op0=ALU.mult, op1=ALU.add),
                         rd=[rf], wr=[rf])
                    k.op("pool", lambda: nc.gpsimd.tensor_tensor(out=F[:, 5:6], in0=F[:, 4:5], in1=nhalf[:], op=ALU.pow),
                         rd=[rf, r_const], wr=[rf])
                    k.op("dve", lambda: V.scalar_tensor_tensor(out=o_all[:, t, h * DV:(h + 1) * DV], in0=of[:, fi, :], scalar=F[:, 5:6],
                                                               in1=gsub[:], op0=ALU.mult, op1=ALU.mult), rd=[rf, r_lam], wr=[r_o[t // 4]])

            sq = sb("sq", [P, 2, DV], F32, pstack)
            r_sq = [Res("sq0"), Res("sq1")]
            pp = make_prepass(l, pstack, engines=("dve", "pool")) if SPARSE else None
            every = max(1, len(steps) // 100)
            LA = 1
            for n in range(len(steps) + LA):
                if n < len(steps):
                    emit_S(n)
                if n - LA >= 0:
                    emit_V(n - LA)
                if pp is not None and n % every == 0:
                    pp(1)
            if pp is not None:
                pp.flush()

        def attn_a3(l, j, pstack, o_all, r_o, src_ap, r_src):
            wo = sb("wo", [P, KC, D], BF16, pstack)
            r_wo = Res("wo")
            load_w_bf16(wo[:], a_wo[j], KC, D, r_dst=r_wo)
            prep = MoePrep(l, pstack, (2, 3, 4), depth=2, rdepth=8)
            xt = [sb("xt%d" % i, [P, D], F32, pstack) for i in range(4)]
            r_xt = [Res("xt%d" % i) for i in range(4)]
            oT = [sb("oT%d" % i, [P, KC, P], BF16, pstack) for i in range(2)]
            r_oT = [Res("oT0"), Res("oT1")]
            def st_X(t):
                xi = t % 4
                i2 = t % 2
                I = t // 4
                k.dma("sp", xt[xi][:], src_ap[t * P:(t + 1) * P, :], rd=[r_src[I]], wr=[r_xt[xi]])
                bT = 6 + (t % 2)
                pb = ps[:, bT, :].bitcast(BF16)
                for c in range(KC):
                    k.op("pe", lambda: nc.tensor.transpose(out=pb[:, c * P:(c + 1) * P], in_=o_all[:, t, c * P:(c + 1) * P],
                                                          identity=ident_b[:]), rd=[r_o[I], r_const], wr=[pbank[bT]], inc=(c == KC - 1))
                k.op("act", lambda: nc.scalar.copy(out=oT[i2][:], in_=pb.rearrange("p (c n) -> p c n", n=P)), rd=[pbank[bT]], wr=[r_oT[i2]])

            def st_X2(t):
                xi = t % 4
                i2 = t % 2
                I = t // 4
                for half in range(2):
                    bk = 0 + half
                    for c in range(KC):
                        k.op("pe", lambda: nc.tensor.matmul(ps[:, bk, :], lhsT=oT[i2][:, c, :], rhs=wo[:, c, half * 512:(half + 1) * 512],
                                                           start=(c == 0), stop=(c == KC - 1)), rd=[r_oT[i2], r_wo], wr=[pbank[bk]],
                             inc=(c == KC - 1))
                    k.op("dve", lambda: nc.vector.tensor_tensor(out=xt[xi][:, half * 512:(half + 1) * 512], in0=ps[:, bk, :],
                                                                in1=xt[xi][:, half * 512:(half + 1) * 512], op=ALU.add),
                         rd=[pbank[bk], r_xt[xi]], wr=[r_xt[xi]])
                k.dma("pool", xres[t * P:(t + 1) * P, :], xt[xi][:], rd=[r_xt[xi]], wr=[r_xres[I]])

            prep.split_logits = True
            run_pipeline(NT, [st_X, st_X2, lambda t: prep.p1a1(xt[t % 4][:], r_xt[t % 4], t),
                              lambda t: prep.p1a2(xt[t % 4][:], r_xt[t % 4], t), prep.p1b, prep.p1b2, prep.p2])

        jc = 0
        ja = 0
        r_xin = [Res("xin%d" % i) for i in range(NM)]
        for l, kind in enumerate(layer_kinds):
            src_ap, r_src = (x_in, r_xin) if l == 0 else (xres, r_xres)
            if kind == "conv":
                with ExitStack() as pstack:
                    conv_c1(l, jc, pstack, src_ap, r_src)
                    k.barrier()
                if debug == "c1":
                    break
                with ExitStack() as pstack:
                    conv_c2(l, jc, pstack, src_ap, r_src)
                    k.barrier()
                if debug == "c2":
                    break
                jc += 1
            else:
                if l == 0:
                    with ExitStack() as pstack:
                        attn_a0(pstack, src_ap, r_src)
                        k.barrier()
                if debug == "a0":
                    break
                with ExitStack() as pstack:
                    attn_a1(l, ja, pstack)
                    k.barrier()
                if debug in ("a1", "a1r", "a1x"):
                    break
                with ExitStack() as ostack:
                    o_all = sb("o_all", [P, NT, D], BF16, ostack)
                    r_o = [Res("o%d" % i) for i in range(NM)]
                    with ExitStack() as pstack:
                        attn_a2(l, ja, pstack, o_all, r_o, lam_inits[l])
                        k.barrier()
                    if debug == "a2":
                        break
                    with ExitStack() as pstack:
                        attn_a3(l, ja, pstack, o_all, r_o, src_ap, r_src)
                        k.barrier()
                ja += 1
            if debug in ("a1", "a2", "a3"):
                break
            with ExitStack() as pstack:
                if SPARSE:
                    moe_sparse(l, pstack)
                else:
                    moe_dense(l, pstack)
                k.barrier()
            if debug == "moe":
                break
            with ExitStack() as pstack:
                last = (l == L - 1)
                ple_phase(l, pstack, last, None if last else layer_kinds[l + 1])
                k.barrier()
        k.final_wait("sp")
        print("ninst", k.ninst, "cnt", k.cnt)
    return nc


_NC_CACHE = {}


def _in_map(inputs, b, S, L):
    f = lambda a: np.ascontiguousarray(a, dtype=np.float32)
    m = {
        "x": f(inputs["x"][b]),
        "p": f(inputs["p"][:, b]),
        "positions": np.ascontiguousarray(inputs["positions"][b].reshape(S // P, P).astype(np.int32)),
    }
    for name in ("norm_mix", "norm_ffn", "conv_w_pw1", "conv_b_pw1", "conv_w_dw", "conv_b_dw", "conv_ln_g", "conv_ln_b",
                 "conv_w_pw2", "conv_b_pw2", "da_w_qkv", "da_subln", "da_w_o", "moe_w_rg", "moe_b_rg",
                 "ple_norm", "ple_w_gate", "ple_w_proj", "final_norm"):
        m[name] = f(inputs[name])
    m["moe_w_re"] = f(inputs["moe_w_re"])
    m["da_lambda"] = f(inputs["da_lambda"]).reshape(-1, 4 * DH)
    m["moe_b_re"] = f(inputs["moe_b_re"]).reshape(L, NG * NE)
    m["moe_w_gate"] = f(inputs["moe_w_gate"]).reshape(L, NEXP, D, FE)
    m["moe_w_up"] = f(inputs["moe_w_up"]).reshape(L, NEXP, D, FE)
    m["moe_w_down"] = f(inputs["moe_w_down"]).reshape(L, NEXP, FE, D)
    return m


def run(inputs, layer_kinds, n_cores=None, lam_i0=0, debug=None):
    B, S, _ = inputs["x"].shape
    L = len(layer_kinds)
    lam_inits = [0.8 - 0.6 * math.exp(-0.3 * (i + lam_i0)) for i in range(L)]
    key = (S, tuple(layer_kinds), debug)
    if key not in _NC_CACHE:
        _NC_CACHE[key] = build_program(S, layer_kinds, lam_inits, debug)
    nc = _NC_CACHE[key]
    n = B if n_cores is None else n_cores
    in_maps = [_in_map(inputs, b, S, L) for b in range(n)]
    res = run_bass_kernel_spmd(nc, in_maps, core_ids=list(range(n)))
    if debug:
        return res.results
    return np.stack([np.asarray(r["y"], dtype=np.float32) for r in res.results], axis=0)


def kernel(**inputs):
    inputs = {k_: np.asarray(v) for k_, v in inputs.items()}
    return run(inputs, ["conv", "attn"])
```

```python
import numpy as np
import math
from contextlib import ExitStack
import concourse.bass as bass
import concourse.mybir as mybir
from concourse.alu_op_type import AluOpType as ALU
from concourse.bass_utils import run_bass_kernel_spmd

F32 = mybir.dt.float32
BF16 = mybir.dt.bfloat16
I32 = mybir.dt.int32
U32 = mybir.dt.uint32
AF = mybir.ActivationFunctionType
AX = mybir.AxisListType

D = 1024
KC = 8
P = 128
CW = 31
NG = 4
NE = 8
NEXP = 32
FE = 256
PLE = 256
EPS = 1e-6
NH = 8
DH = 64
DV = 128
ROPE = 16
THETA = 500000.0
NDSEM = 16


class Res:
    __slots__ = ("name", "w", "rd", "excl")

    def __init__(self, name="", excl=False):
        self.name = name
        self.w = {}
        self.rd = {}
        self.excl = excl


class Sched:
    def __init__(self, nc, es):
        self.nc = nc
        self.E = {"pe": nc.tensor, "dve": nc.vector, "act": nc.scalar, "pool": nc.gpsimd, "sp": nc.sync}
        self.sems = []
        self.esem = {}
        self.cnt = {}
        self.seen = {}
        for e in self.E:
            self.esem[e] = len(self.sems)
            self.sems.append(es.enter_context(nc.semaphore("s_" + e)))
            self.cnt[e] = 0
            self.seen[e] = {}
        self.dsem = {}
        self.dnext = {}
        self.dval = {}
        for q in ("sp", "pool", "act"):
            self.dsem[q] = []
            for i in range(NDSEM):
                self.dsem[q].append(len(self.sems))
                self.dval[len(self.sems)] = 0
                self.sems.append(es.enter_context(nc.semaphore("d_%s%d" % (q, i))))
            self.dnext[q] = 0
        self.ninst = 0

    def _wait(self, eng, s, v):
        if v <= 0 or self.seen[eng].get(s, 0) >= v:
            return
        self.E[eng].wait_ge(self.sems[s], v)
        self.seen[eng][s] = v

    def _deps(self, eng, rd, wr, own):
        for r in rd:
            for s, v in r.w.items():
                self._wait(eng, s, v)
            if r.excl:
                for s, v in r.rd.items():
                    if s != own:
                        self._wait(eng, s, v)
        pe_own = self.esem["pe"]
        for w in wr:
            for s, v in w.w.items():
                if s != own or own != pe_own:
                    self._wait(eng, s, v)
            for s, v in w.rd.items():
                if s != own or own != pe_own:
                    self._wait(eng, s, v)

    def _mark(self, tok, rd, wr):
        s, v = tok
        for r in rd:
            if r.rd.get(s, 0) < v:
                r.rd[s] = v
        for w in wr:
            if w.w.get(s, 0) < v:
                w.w[s] = v

    def op(self, eng, fn, rd=(), wr=(), inc=True):
        own = self.esem[eng]
        self._deps(eng, rd, wr, own)
        ins = fn()
        self.ninst += 1
        if inc:
            self.cnt[eng] += 1
            ins.then_inc(self.sems[own], 1)
            tok = (own, self.cnt[eng])
        else:
            tok = (own, self.cnt[eng] + 1)
        self._mark(tok, rd, wr)
        return tok

    def dma(self, q, out, in_, rd=(), wr=(), **kw):
        self._deps(q, rd, wr, -1)
        i = self.dnext[q]
        self.dnext[q] = (i + 1) % NDSEM
        s = self.dsem[q][i]
        self._wait(q, s, self.dval[s])
        self.dval[s] += 16
        self.E[q].dma_start(out=out, in_=in_, **kw).then_inc(self.sems[s], 16)
        self.ninst += 1
        tok = (s, self.dval[s])
        self._mark(tok, rd, wr)
        return tok

    def dma_indirect(self, fn, rd=(), wr=()):
        q = "pool"
        self._deps(q, rd, wr, -1)
        i = self.dnext[q]
        self.dnext[q] = (i + 1) % NDSEM
        s = self.dsem[q][i]
        self._wait(q, s, self.dval[s])
        self.dval[s] += 16
        fn(None).then_inc(self.sems[s], 16)
        self.ninst += 1
        tok = (s, self.dval[s])
        self._mark(tok, rd, wr)
        return tok

    def barrier(self):
        for x in self.E:
            for e in self.E:
                if e != x or True:
                    self._wait(x, self.esem[e], self.cnt[e])
            for s, v in self.dval.items():
                self._wait(x, s, v)

    def final_wait(self, eng="sp"):
        for e in self.E:
            self._wait(eng, self.esem[e], self.cnt[e])
        for s, v in self.dval.items():
            self._wait(eng, s, v)


def build_program(S, layer_kinds, lam_inits, debug=None):
    NT = S // P
    NM = S // 512
    L = len(layer_kinds)
    nconv = sum(1 for k_ in layer_kinds if k_ == "conv")
    nattn = L - nconv
    nc = bass.Bass("TRN2", target_bir_lowering=False)

    def din(name, shape, dt=F32):
        return nc.dram_tensor(name, list(shape), dt, kind="ExternalInput").ap()

    def dscr(name, shape, dt):
        return nc.dram_tensor(name, list(shape), dt, kind=("ExternalOutput" if debug else "Internal")).ap()

    x_in = din("x", [S, D])
    p_in = din("p", [L, S, PLE])
    pos_in = din("positions", [NT, P], I32)
    norm_mix = din("norm_mix", [L, D])
    norm_ffn = din("norm_ffn", [L, D])
    c_w1 = din("conv_w_pw1", [max(nconv, 1), D, 2 * D])
    c_b1 = din("conv_b_pw1", [max(nconv, 1), 2 * D])
    c_wdw = din("conv_w_dw", [max(nconv, 1), CW, D])
    c_bdw = din("conv_b_dw", [max(nconv, 1), D])
    c_lng = din("conv_ln_g", [max(nconv, 1), D])
    c_lnb = din("conv_ln_b", [max(nconv, 1), D])
    c_w2 = din("conv_w_pw2", [max(nconv, 1), D, D])
    c_b2 = din("conv_b_pw2", [max(nconv, 1), D])
    a_wqkv = din("da_w_qkv", [max(nattn, 1), D, 3 * D])
    a_lam = din("da_lambda", [max(nattn, 1), 4 * DH])
    a_subln = din("da_subln", [max(nattn, 1), DV])
    a_wo = din("da_w_o", [max(nattn, 1), D, D])
    m_wrg = din("moe_w_rg", [L, D, NG])
    m_brg = din("moe_b_rg", [L, NG])
    m_wre = din("moe_w_re", [L, NG, D, NE])
    m_bre = din("moe_b_re", [L, NG * NE])
    m_wg = din("moe_w_gate", [L, NEXP, D, FE])
    m_wu = din("moe_w_up", [L, NEXP, D, FE])
    m_wd = din("moe_w_down", [L, NEXP, FE, D])
    ple_norm = din("ple_norm", [L, D])
    ple_wg = din("ple_w_gate", [L, D, D])
    ple_wp = din("ple_w_proj", [L, PLE, D])
    fin_norm = din("final_norm", [D])
    y_out = nc.dram_tensor("y", [S, D], F32, kind="ExternalOutput").ap()
    xres = dscr("xres", [S, D], F32)
    vscr = dscr("vscr", [KC, P, 32 + S], BF16)
    hscr = dscr("hscr", [KC, P, S], BF16)
    qscr = dscr("qscr", [NH, P, S], BF16)
    kscr = dscr("kscr", [NH, P, S], BF16)
    vtok = dscr("vtok", [S, D], BF16)
    NTL = (2 * S) // P + NEXP
    NSLOT = NTL * P
    SPARSE = True
    wgb = [dscr("wgb%d" % l_, [NEXP * P, KC * FE], BF16) for l_ in range(L)]
    wub = [dscr("wub%d" % l_, [NEXP * P, KC * FE], BF16) for l_ in range(L)]
    wdb = [dscr("wdb%d" % l_, [NEXP * P, 2 * D], BF16) for l_ in range(L)]
    hsort = dscr("hsort", [NSLOT, D], BF16)
    ysort = dscr("ysort", [NSLOT, D], F32)
    dbg_lg = dscr("dbg_lg", [S, 256], F32) if debug else None

    es = ExitStack()
    with es:
        k = Sched(nc, es)

        uniq = [0]

        def sb(name, shape, dt, stack=None):
            uniq[0] += 1
            return (stack or es).enter_context(nc.sbuf_tensor("%s_%d" % (name, uniq[0]), list(shape), dt))

        ps = es.enter_context(nc.psum_tensor("ps", [P, 8, 512], F32))
        pbank = [Res("bank%d" % i, excl=True) for i in range(8)]

        ident_b = sb("ident_b", [P, P], BF16)
        ident_f = sb("ident_f", [P, P], F32)
        ones_b = sb("ones_b", [P, P], BF16)
        eps_col = sb("eps_col", [P, 1], F32)
        r_const = Res("const")
        k.op("pool", lambda: nc.gpsimd.memset(ident_f[:], 0.0), wr=[r_const])
        k.op("pool", lambda: nc.gpsimd.affine_select(out=ident_f[:], in_=ident_f[:], pattern=[[-1, P]],
                                                     compare_op=ALU.not_equal, fill=1.0, base=0, channel_multiplier=1),
             rd=[r_const], wr=[r_const])
        k.op("pool", lambda: nc.gpsimd.tensor_copy(out=ident_b[:], in_=ident_f[:]), rd=[r_const], wr=[r_const])
        k.op("pool", lambda: nc.gpsimd.memset(ones_b[:], 1.0), wr=[r_const])
        k.op("pool", lambda: nc.gpsimd.memset(eps_col[:], EPS), wr=[r_const])

        def load_featvec(name, rows_ap, nrows, stack=None):
            out = sb(name, [P, KC, 8], F32, stack)
            with ExitStack() as st:
                tmp = sb(name + "_tmp", [8, D], F32, st)
                r_t = Res(name)
                k.op("pool", lambda: nc.gpsimd.memset(tmp[:], 0.0), wr=[r_t])
                k.dma("sp", tmp[0:nrows, :], rows_ap, wr=[r_t])
                for c in range(KC):
                    bk = c % 2
                    k.op("pe", lambda: nc.tensor.transpose(out=ps[:, bk, 0:8], in_=tmp[:, c * P:(c + 1) * P],
                                                          identity=ident_f[0:8, 0:8]), rd=[r_const, r_t], wr=[pbank[bk]])
                    k.op("dve", lambda: nc.vector.tensor_copy(out=out[:, c, :], in_=ps[:, bk, 0:8]), rd=[pbank[bk]], wr=[r_const])
                k.barrier()
            return out

        g_mix = load_featvec("g_mix", norm_mix, L)
        g_ffn = load_featvec("g_ffn", norm_ffn, L)
        g_ple = load_featvec("g_ple", ple_norm, L)

        NSTG = 2
        stage_bufs = [sb("stage%d" % i, [P, 2048], F32) for i in range(NSTG)]
        stage_res = [Res("stage%d" % i) for i in range(NSTG)]
        stage_i = [0]
        cast_rr = [0]

        def cast(eng, out, in_, rd, wr, scale=None):
            if eng == "act":
                if scale is None:
                    k.op("act", lambda: nc.scalar.copy(out=out, in_=in_), rd=rd, wr=wr)
                else:
                    k.op("act", lambda: nc.scalar.activation(out=out, in_=in_, func=AF.Identity, scale=scale), rd=rd, wr=wr)
            elif eng == "pool":
                if scale is None:
                    k.op("pool", lambda: nc.gpsimd.tensor_copy(out=out, in_=in_), rd=rd, wr=wr)
                else:
                    k.op("pool", lambda: nc.gpsimd.tensor_scalar(out=out, in0=in_, scalar1=scale, scalar2=1.0,
                                                                 op0=ALU.mult, op1=ALU.mult), rd=rd, wr=wr)
            else:
                if scale is None:
                    k.op("dve", lambda: nc.vector.tensor_copy(out=out, in_=in_), rd=rd, wr=wr)
                else:
                    k.op("dve", lambda: nc.vector.tensor_scalar(out=out, in0=in_, scalar1=scale, scalar2=None,
                                                                op0=ALU.mult), rd=rd, wr=wr)

        def load_w_bf16(dst3, src2, kc, n, gain=None, r_dst=None, engines=("act", "pool", "dve")):
            if n > 2048:
                for n0 in range(0, n, 1024):
                    load_w_bf16(dst3[:, :, n0:n0 + 1024], src2[:, n0:n0 + 1024], kc, 1024, gain, r_dst, engines)
                return
            per = max(1, 2048 // n)
            c0 = 0
            while c0 < kc:
                cn = min(per, kc - c0)
                si = stage_i[0] % NSTG
                stage_i[0] += 1
                stv = stage_bufs[si][:, 0:cn * n].rearrange("p (c n) -> p c n", n=n)
                k.dma("sp", stv, src2[c0 * P:(c0 + cn) * P, :].rearrange("(c p) n -> p c n", p=P), wr=[stage_res[si]])
                eng = engines[cast_rr[0] % len(engines)]
                cast_rr[0] += 1
                if gain is None:
                    cast(eng, dst3[:, c0:c0 + cn, :], stv, [stage_res[si]], [r_dst])
                else:
                    for c in range(cn):
                        cast(eng, dst3[:, c0 + c, :], stv[:, c, :], [stage_res[si], r_const], [r_dst],
                             scale=gain[:, c0 + c:c0 + c + 1])
                c0 += cn

        junk = sb("junk", [P, D], BF16)
        r_junk = Res("junk")
        stat = sb("stat", [P, 64], F32)
        r_stat = [Res("stat%d" % i) for i in range(16)]
        stat_i = [0]

        nhalf = sb("nhalf", [P, 1], F32)
        k.op("pool", lambda: nc.gpsimd.memset(nhalf[:], -0.5), wr=[r_const])

        def rms_rstd(x_ap, r_x, n=D):
            i = stat_i[0] % 16
            stat_i[0] += 1
            ssq = stat[:, 4 * i:4 * i + 1]
            ms = stat[:, 4 * i + 1:4 * i + 2]
            rstd = stat[:, 4 * i + 2:4 * i + 3]
            r = r_stat[i]
            k.op("act", lambda: nc.scalar.activation(out=junk[:, 0:n], in_=x_ap, func=AF.Square, accum_out=ssq),
                 rd=[r_x], wr=[r_junk, r])
            k.op("dve", lambda: nc.vector.tensor_scalar(out=ms, in0=ssq, scalar1=1.0 / n, scalar2=EPS, op0=ALU.mult, op1=ALU.add),
                 rd=[r], wr=[r])
            k.op("pool", lambda: nc.gpsimd.tensor_tensor(out=rstd, in0=ms, in1=nhalf[:], op=ALU.pow), rd=[r, r_const], wr=[r])
            return rstd, r

        combT = sb("combT", [32, S], BF16)
        comb_tok = sb("comb_tok", [P, NT, 32], F32)
        r_ctok = Res("comb_tok")
        slotA_i = sb("slotA_i", [P, NT], I32)
        slotB_i = sb("slotB_i", [P, NT], I32)
        wAB = sb("wAB", [P, 2, NT], F32)
        r_route = Res("route")
        r_htok = Res("htok")
        r_ysort = Res("ysort")
        r_comb = [Res("comb%d" % i) for i in range(NM)]
        r_xres = [Res("xres%d" % i) for i in range(NM)]
        r_hscr = [Res("hscr%d" % i) for i in range(NM)]
        r_vscr = [Res("vscr%d" % i) for i in range(NM + 1)]

        def run_pipeline(n, stages):
            ns = len(stages)
            for step in range(n + ns - 1):
                for i, f in enumerate(stages):
                    t = step - i
                    if 0 <= t < n:
                        f(t)

        class HStage:
            def __init__(self, stack, flush=True):
                self.flush = flush
                self.buf = [sb("hstage%d" % i, [P, KC, 512], BF16, stack) for i in range(2)]
                self.res = [Res("hstage0"), Res("hstage1")]
                self.xnb = [sb("hs_xnb%d" % i, [P, D], BF16, stack) for i in range(2)]
                self.r_xnb = [Res("hs_xnb0"), Res("hs_xnb1")]

            def put_T(self, t, src_bf16, r_src, bank, evac_eng="act"):
                mt, sub = t // 4, t % 4
                i = mt % 2
                pb = ps[:, bank, :].bitcast(BF16)
                for c in range(KC):
                    k.op("pe", lambda: nc.tensor.transpose(out=pb[:, c * P:(c + 1) * P], in_=src_bf16[:, c * P:(c + 1) * P],
                                                          identity=ident_b[:]), rd=[r_src, r_const], wr=[pbank[bank]], inc=(c == KC - 1))
                dst = self.buf[i][:, :, sub * P:(sub + 1) * P]
                src = pb.rearrange("p (c n) -> p c n", n=P)
                if evac_eng == "act":
                    k.op("act", lambda: nc.scalar.copy(out=dst, in_=src), rd=[pbank[bank]], wr=[self.res[i]])
                else:
                    k.op("dve", lambda: nc.vector.tensor_copy(out=dst, in_=src), rd=[pbank[bank]], wr=[self.res[i]])
                if sub == 3 and self.flush:
                    k.dma("pool", hscr[:, :, mt * 512:(mt + 1) * 512].rearrange("c p t -> p c t"), self.buf[i][:],
                          rd=[self.res[i]], wr=[r_hscr[mt]])

            def norm_put(self, t, x_ap, r_x, bank):
                i = t % 2
                rstd, r_rs = rms_rstd(x_ap, r_x)
                k.op("dve", lambda: nc.vector.tensor_scalar(out=self.xnb[i][:], in0=x_ap, scalar1=rstd, scalar2=None, op0=ALU.mult),
                     rd=[r_x, r_rs], wr=[self.r_xnb[i]])
                self.put_T(t, self.xnb[i], self.r_xnb[i], bank)

        class MoePrep:
            def __init__(self, l, stack, banks, depth=1, rdepth=4):
                self.l = l
                self.banks = banks
                self.depth = depth
                self.hs = HStage(stack)
                self.wr_f = sb("wr_f", [P, KC, 36], F32, stack)
                self.r_wr = Res("wr")
                self.rb_bc = sb("rb_bc", [P, 36], F32, stack)
                tmp = sb("wr_tmp", [P, KC, 36], F32, stack)
                r_wr = self.r_wr
                with nc.allow_non_contiguous_dma(reason="tiny router weights"):
                    k.dma("sp", tmp[:, :, 0:NG], m_wrg[l].rearrange("(c p) n -> p c n", p=P), wr=[r_wr])
                    for g in range(NG):
                        k.dma("sp", tmp[:, :, NG + g * NE:NG + (g + 1) * NE], m_wre[l, g].rearrange("(c p) n -> p c n", p=P), wr=[r_wr])
                for c in range(KC):
                    k.op("dve", lambda: nc.vector.tensor_scalar(out=self.wr_f[:, c, :], in0=tmp[:, c, :], scalar1=g_ffn[:, c, l:l + 1],
                                                                scalar2=None, op0=ALU.mult), rd=[r_wr, r_const], wr=[r_wr])
                k.dma("sp", self.rb_bc[:, 0:NG], m_brg[l].partition_broadcast(P), wr=[r_wr])
                k.dma("sp", self.rb_bc[:, NG:36], m_bre[l].partition_broadcast(P), wr=[r_wr])
                self.xn_fs = [sb("xn_f%d" % i, [P, D], F32, stack) for i in range(depth)]
                self.r_xns = [Res("xn%d" % i) for i in range(depth)]
                self.hTfs = [sb("hTf%d" % i, [P, KC, P], F32, stack) for i in range(depth)]
                self.r_hTfs = [Res("hTf%d" % i) for i in range(depth)]
                self.rdepth = rdepth
                self.rs = {}
                self.split_logits = False
                self.rt = sb("rt", [P, self.rdepth, 256], F32, stack)
                self.r_rt = [Res("rt%d" % i) for i in range(self.rdepth)]
                self.ctr = 0

            def tile(self, x_ap, r_x, t):
                self.p1(x_ap, r_x, t)
                self.p2(t)

            def p1(self, x_ap, r_x, t):
                self.p1a(x_ap, r_x, t)
                self.p1b(t)

            def p1a(self, x_ap, r_x, t):
                self.p1a1(x_ap, r_x, t)
                self.p1a2(x_ap, r_x, t)

            def p1a1(self, x_ap, r_x, t):
                self.rs[t % 4] = rms_rstd(x_ap, r_x)

            def p1a2(self, x_ap, r_x, t):
                xn_f, r_xn = self.xn_fs[t % self.depth], self.r_xns[t % self.depth]
                rstd, r_rs = self.rs[t % 4]
                k.op("act", lambda: nc.scalar.activation(out=xn_f[:], in_=x_ap, func=AF.Identity, scale=rstd),
                     rd=[r_x, r_rs], wr=[r_xn])
                xb = self.hs.xnb[t % 2]
                r_xb = self.hs.r_xnb[t % 2]
                k.op("pool", lambda: nc.gpsimd.tensor_copy(out=xb[:], in_=xn_f[:]), rd=[r_xn], wr=[r_xb])
                if SPARSE:
                    k.dma("pool", vtok[t * P:(t + 1) * P, :], xb[:], rd=[r_xb], wr=[r_htok])

            def p1b(self, t):
                i = t % self.rdepth
                b0, b1, b2 = self.banks
                xn_f, r_xn = self.xn_fs[t % self.depth], self.r_xns[t % self.depth]
                hTf, r_hTf = self.hTfs[t % self.depth], self.r_hTfs[t % self.depth]
                xb = self.hs.xnb[t % 2]
                r_xb = self.hs.r_xnb[t % 2]
                self.hs.put_T(t, xb, r_xb, b0, evac_eng="dve")
                for c in range(KC):
                    bk = b1 if c < 4 else b2
                    k.op("pe", lambda: nc.tensor.transpose(out=ps[:, bk, (c % 4) * P:(c % 4 + 1) * P], in_=xn_f[:, c * P:(c + 1) * P],
                                                          identity=ident_f[:]), rd=[r_xn, r_const], wr=[pbank[bk]], inc=(c % 4 == 3))
                for h in range(2):
                    bk = b1 if h == 0 else b2
                    src = ps[:, bk, :].rearrange("p (c n) -> p c n", n=P)
                    k.op("act", lambda: nc.scalar.copy(out=hTf[:, 4 * h:4 * h + 4, :], in_=src), rd=[pbank[bk]], wr=[r_hTf])
                if self.split_logits:
                    return
                self.p1b2(t)

            def p1b2(self, t):
                i = t % self.rdepth
                b0, b1, b2 = self.banks
                hTf, r_hTf = self.hTfs[t % self.depth], self.r_hTfs[t % self.depth]
                for c in range(KC):
                    k.op("pe", lambda: nc.tensor.matmul(ps[:, b1, 0:36], lhsT=hTf[:, c, :], rhs=self.wr_f[:, c, :],
                                                       start=(c == 0), stop=(c == KC - 1)),
                         rd=[r_hTf, self.r_wr], wr=[pbank[b1]], inc=(c == KC - 1))
                R = self.rt[:, i, :]
                rr = self.r_rt[i]
                Lg = R[:, 0:36]
                gmax = R[:, 36:37]
                ngmax = R[:, 37:38]
                gsum = R[:, 38:39]
                gw = R[:, 39:40]
                goh = R[:, 40:44]
                gex = R[:, 44:48]
                top8 = R[:, 48:80]
                nt1 = R[:, 80:84]
                mask = R[:, 84:116]
                ex = R[:, 116:148]
                den = R[:, 148:152]
                coef = R[:, 152:156]
                comb = R[:, 160:192]
                V = nc.vector
                k.op("dve", lambda: V.tensor_tensor(out=Lg, in0=ps[:, b1, 0:36], in1=self.rb_bc[:], op=ALU.add),
                     rd=[pbank[b1], self.r_wr], wr=[rr])

            def p2(self, t):
                if t % 4 != 3:
                    return
                lists = [self.p2_ops(tt) for tt in range(t - 3, t + 1)]
                n = max(len(x) for x in lists)
                for j in range(n):
                    for lst in lists:
                        if j < len(lst):
                            lst[j]()

            def p2_ops(self, t):
                i = t % self.rdepth
                mt = t // 4
                col0 = t * P
                b0, b1, b2 = self.banks
                R = self.rt[:, i, :]
                rr = self.r_rt[i]
                Lg = R[:, 0:36]
                gmax = R[:, 36:37]
                ngmax = R[:, 37:38]
                gsum = R[:, 38:39]
                gw = R[:, 39:40]
                goh = R[:, 40:44]
                gex = R[:, 44:48]
                top8 = R[:, 48:80]
                nt1 = R[:, 80:84]
                mask = R[:, 84:116]
                ex = R[:, 116:148]
                den = R[:, 148:152]
                coef = R[:, 152:156]
                comb = R[:, 160:192]
                V = nc.vector
                ops = []
                A = ops.append
                A(lambda: k.op("dve", lambda: V.tensor_reduce(out=gmax, in_=Lg[:, 0:NG], axis=AX.X, op=ALU.max), rd=[rr], wr=[rr]))
                A(lambda: k.op("dve", lambda: V.tensor_scalar(out=goh, in0=Lg[:, 0:NG], scalar1=gmax, scalar2=None, op0=ALU.is_equal),
                               rd=[rr], wr=[rr]))
                A(lambda: k.op("dve", lambda: V.tensor_scalar(out=ngmax, in0=gmax, scalar1=-1.0, scalar2=None, op0=ALU.mult), rd=[rr], wr=[rr]))
                A(lambda: k.op("act", lambda: nc.scalar.activation(out=gex, in_=Lg[:, 0:NG], func=AF.Exp, bias=ngmax, accum_out=gsum),
                               rd=[rr], wr=[rr]))

                def top_all():
                    for g in range(NG):
                        k.op("dve", lambda: V.max(out=top8[:, g * 8:(g + 1) * 8], in_=Lg[:, NG + g * NE:NG + (g + 1) * NE]), rd=[rr], wr=[rr])
                A(top_all)
                t8 = top8.rearrange("p (g e) -> p g e", e=8)
                A(lambda: k.op("dve", lambda: V.tensor_scalar(out=nt1, in0=t8[:, :, 0], scalar1=-1.0, scalar2=None, op0=ALU.mult), rd=[rr], wr=[rr]))

                def mask_all():
                    for g in range(NG):
                        le = Lg[:, NG + g * NE:NG + (g + 1) * NE]
                        k.op("dve", lambda: V.tensor_scalar(out=mask[:, g * 8:(g + 1) * 8], in0=le, scalar1=top8[:, g * 8 + 1:g * 8 + 2],
                                                            scalar2=None, op0=ALU.is_ge), rd=[rr], wr=[rr])
                A(mask_all)

                def ex_all():
                    for g in range(NG):
                        le = Lg[:, NG + g * NE:NG + (g + 1) * NE]
                        k.op("act", lambda: nc.scalar.activation(out=ex[:, g * 8:(g + 1) * 8], in_=le, func=AF.Exp, bias=nt1[:, g:g + 1]),
                             rd=[rr], wr=[rr])
                A(ex_all)
                A(lambda: k.op("dve", lambda: V.reciprocal(out=gw, in_=gsum), rd=[rr], wr=[rr]))
                A(lambda: k.op("dve", lambda: V.tensor_tensor(out=ex, in0=ex, in1=mask, op=ALU.mult), rd=[rr], wr=[rr]))
                A(lambda: k.op("dve", lambda: V.tensor_reduce(out=den, in_=ex.rearrange("p (g e) -> p g e", e=8), axis=AX.X, op=ALU.add),
                               rd=[rr], wr=[rr]))
                A(lambda: k.op("dve", lambda: V.reciprocal(out=coef, in_=den), rd=[rr], wr=[rr]))
                A(lambda: k.op("dve", lambda: V.tensor_tensor(out=coef, in0=coef, in1=goh, op=ALU.mult), rd=[rr], wr=[rr]))
                A(lambda: k.op("dve", lambda: V.tensor_scalar(out=coef, in0=coef, scalar1=gw, scalar2=None, op0=ALU.mult), rd=[rr], wr=[rr]))

                def comb_all():
                    for g in range(NG):
                        k.op("dve", lambda: V.tensor_scalar(out=comb[:, g * 8:(g + 1) * 8], in0=ex[:, g * 8:(g + 1) * 8],
                                                            scalar1=coef[:, g:g + 1], scalar2=None, op0=ALU.mult), rd=[rr], wr=[rr])
                A(comb_all)
                if SPARSE:
                    A(lambda: k.op("pool", lambda: nc.gpsimd.tensor_copy(out=comb_tok[:, t, :], in_=comb), rd=[rr], wr=[r_ctok]))

                def fin():
                    k.op("pe", lambda: nc.tensor.transpose(out=ps[0:32, b2, 0:P], in_=comb, identity=ident_f[:]),
                         rd=[rr, r_const], wr=[pbank[b2]])
                    if debug:
                        k.dma("pool", dbg_lg[t * P:(t + 1) * P, 0:36], R[:, 0:36], rd=[rr], wr=[Res("dbg")])
                    k.op("dve", lambda: V.tensor_copy(out=combT[:, col0:col0 + P], in_=ps[0:32, b2, 0:P]),
                         rd=[pbank[b2]], wr=[r_comb[mt]])
                A(fin)
                return ops

        EC = 4

        def moe_dense(l, pstack):
            sel = sb("sel", [32, NEXP, P], BF16, pstack)
            r_sel = Res("sel")
            k.op("pool", lambda: nc.gpsimd.memset(sel[:], 0.0), wr=[r_sel])
            k.op("pool", lambda: nc.gpsimd.affine_select(out=sel[:], in_=sel[:], pattern=[[-1, NEXP], [0, P]],
                                                         compare_op=ALU.not_equal, fill=1.0, base=0, channel_multiplier=1),
                 rd=[r_sel], wr=[r_sel])
            wg = [sb("wg%d" % i, [P, EC, KC, FE], BF16, pstack) for i in range(2)]
            wu = [sb("wu%d" % i, [P, EC, KC, FE], BF16, pstack) for i in range(2)]
            wd = [sb("wd%d" % i, [P, EC, 2, D], BF16, pstack) for i in range(2)]
            r_w = [Res("moew0"), Res("moew1")]
            hT = [sb("mhT%d" % i, [P, KC, 512], BF16, pstack) for i in range(2)]
            r_hT = [Res("mhT0"), Res("mhT1")]
            act = [sb("actb%d" % i, [P, EC, 2, 512], BF16, pstack) for i in range(2)]
            r_act = [Res("act0"), Res("act1")]
            cmb_sb = [sb("cmb_sb%d" % i, [P, 512], BF16, pstack) for i in range(2)]
            r_cmb = [Res("cmb0"), Res("cmb1")]
            sg = [sb("sg%d" % i, [P, 512], BF16, pstack) for i in range(3)]
            r_sg = [Res("sg%d" % i) for i in range(3)]
            ub = [sb("ub%d" % i, [P, 512], BF16, pstack) for i in range(3)]
            r_ub = [Res("ub%d" % i) for i in range(3)]
            ob = [sb("ob%d" % i, [P, D], F32, pstack) for i in range(3)]
            r_ob = [Res("ob%d" % i) for i in range(3)]
            nchunk = NEXP // EC

            def chunk_jobs(ci):
                bi = ci % 2
                jobs = []
                for e in range(EC):
                    eg = ci * EC + e
                    for which in range(3):
                        def mk(e=e, eg=eg, which=which):
                            st = {}

                            def dma():
                                si = stage_i[0] % NSTG
                                stage_i[0] += 1
                                st["si"] = si
                                if which < 2:
                                    src = (m_wg if which == 0 else m_wu)[l, eg]
                                    stv = stage_bufs[si][:, 0:KC * FE].rearrange("p (c n) -> p c n", n=FE)
                                else:
                                    src = m_wd[l, eg]
                                    stv = stage_bufs[si][:, 0:2 * D].rearrange("p (c n) -> p c n", n=D)
                                st["stv"] = stv
                                k.dma("sp", stv, src.rearrange("(c p) n -> p c n", p=P), wr=[stage_res[si]])

                            def cst():
                                si, stv = st["si"], st["stv"]
                                if which < 2:
                                    dst = (wg if which == 0 else wu)[bi][:, e]
                                    for c in range(KC):
                                        eng = "act" if which == 0 else ("pool" if c % 2 == 0 else "dve")
                                        cast(eng, dst[:, c, :], stv[:, c, :], [stage_res[si], r_const], [r_w[bi]], scale=g_ffn[:, c, l:l + 1])
                                else:
                                    cast("dve", wd[bi][:, e, 0, :], stv[:, 0, :], [stage_res[si]], [r_w[bi]])
                                    cast("act", wd[bi][:, e, 1, :], stv[:, 1, :], [stage_res[si]], [r_w[bi]])
                            return dma, cst
                        jobs.append(mk())
                return jobs

            cnt3 = [0]
            ocnt = [0]
            dcnt = [0]
            hcnt = [0]
            for d_, c_ in chunk_jobs(0):
                d_()
                c_()
            nslots = NM * EC
            for ci in range(nchunk):
                bi = ci % 2
                jobs = chunk_jobs(ci + 1) if ci + 1 < nchunk else []
                sched_d = {}
                sched_c = {}
                for j, (d_, c_) in enumerate(jobs):
                    sd = (j * nslots) // len(jobs)
                    sc = min(nslots - 1, sd + 1)
                    sched_d.setdefault(sd, []).append(d_)
                    sched_c.setdefault(sc, []).append(c_)
                for I in range(NM):
                    hi = hcnt[0] % 2
                    hcnt[0] += 1
                    k.dma("sp", hT[hi][:], hscr[:, :, I * 512:(I + 1) * 512].rearrange("c p t -> p c t"), rd=[r_hscr[I]], wr=[r_hT[hi]])
                    ai = (ci * NM + I) % 2
                    tok = slice(I * 512, (I + 1) * 512)
                    for e in range(EC):
                        slot = I * EC + e
                        late = []
                        for c_ in sched_c.get(slot, []):
                            try:
                                c_()
                            except KeyError:
                                late.append(c_)
                        for d_ in sched_d.get(slot, []):
                            d_()
                        for c_ in late:
                            c_()
                        eg = ci * EC + e
                        ci2 = (ci * NM * EC + I * EC + e) % 2
                        k.op("pe", lambda: nc.tensor.matmul(ps[:, 0, :], lhsT=sel[:, eg, :], rhs=combT[:, tok], start=True, stop=True),
                             rd=[r_sel, r_comb[I]], wr=[pbank[0]])
                        k.op("act", lambda: nc.scalar.copy(out=cmb_sb[ci2][:], in_=ps[:, 0, :]), rd=[pbank[0]], wr=[r_cmb[ci2]])
                        for fc in range(2):
                            j3 = cnt3[0] % 3
                            gset = cnt3[0] % 2
                            cnt3[0] += 1
                            bg = 1 + 2 * gset
                            bu = 2 + 2 * gset
                            for c in range(KC):
                                k.op("pe", lambda: nc.tensor.matmul(ps[:, bg, :], lhsT=wg[bi][:, e, c, fc * P:(fc + 1) * P],
                                                                   rhs=hT[hi][:, c, :], start=(c == 0), stop=(c == KC - 1)),
                                     rd=[r_w[bi], r_hT[hi]], wr=[pbank[bg]], inc=(c == KC - 1))
                            for c in range(KC):
                                k.op("pe", lambda: nc.tensor.matmul(ps[:, bu, :], lhsT=wu[bi][:, e, c, fc * P:(fc + 1) * P],
                                                                   rhs=hT[hi][:, c, :], start=(c == 0), stop=(c == KC - 1)),
                                     rd=[r_w[bi], r_hT[hi]], wr=[pbank[bu]], inc=(c == KC - 1))
                            k.op("act", lambda: nc.scalar.activation(out=sg[j3][:], in_=ps[:, bg, :], func=AF.Silu),
                                 rd=[pbank[bg]], wr=[r_sg[j3]])
                            k.op("dve", lambda: nc.vector.tensor_tensor(out=ub[j3][:], in0=ps[:, bu, :], in1=cmb_sb[ci2][:], op=ALU.mult),
                                 rd=[pbank[bu], r_cmb[ci2]], wr=[r_ub[j3]])
                            k.op("pool", lambda: nc.gpsimd.tensor_tensor(out=act[ai][:, e, fc, :], in0=ub[j3][:], in1=sg[j3][:], op=ALU.mult),
                                 rd=[r_ub[j3], r_sg[j3]], wr=[r_act[ai]])
                    for sub in range(4):
                        oi = ocnt[0] % 3
                        ocnt[0] += 1
                        for half in range(2):
                            bd = 5 + dcnt[0] % 2
                            dcnt[0] += 1
                            n = 0
                            for e in range(EC):
                                for fc in range(2):
                                    k.op("pe", lambda: nc.tensor.matmul(ps[:, bd, :], lhsT=act[ai][:, e, fc, sub * P:(sub + 1) * P],
                                                                       rhs=wd[bi][:, e, fc, half * 512:(half + 1) * 512],
                                                                       start=(n == 0), stop=(n == 2 * EC - 1)),
                                         rd=[r_act[ai], r_w[bi]], wr=[pbank[bd]], inc=(n == 2 * EC - 1))
                                    n += 1
                            if half == 0:
                                k.op("dve", lambda: nc.vector.tensor_copy(out=ob[oi][:, 0:512], in_=ps[:, bd, :]),
                                     rd=[pbank[bd]], wr=[r_ob[oi]])
                            else:
                                k.op("act", lambda: nc.scalar.copy(out=ob[oi][:, 512:1024], in_=ps[:, bd, :]),
                                     rd=[pbank[bd]], wr=[r_ob[oi]])
                        r0 = I * 512 + sub * P
                        k.dma("pool", xres[r0:r0 + P, :], ob[oi][:], rd=[r_ob[oi]], wr=[r_xres[I]], accum_op=ALU.add)

        def make_prepass(l, pstack, engines=("act", "dve", "pool")):
            wb = [sb("wb%d" % i, [P, 2048], BF16, pstack) for i in range(3)]
            r_wb = [Res("wb%d" % i) for i in range(3)]
            jobs = [(e, which) for e in range(NEXP) for which in range(3)]
            state = {"n": 0, "pend": None}

            def issue(n):
                e, which = jobs[n]
                si = stage_i[0] % NSTG
                stage_i[0] += 1
                if which < 2:
                    src = (m_wg if which == 0 else m_wu)[l, e]
                    stv = stage_bufs[si][:, 0:KC * FE].rearrange("p (c n) -> p c n", n=FE)
                else:
                    src = m_wd[l, e]
                    stv = stage_bufs[si][:, 0:2 * D].rearrange("p (c n) -> p c n", n=D)
                k.dma("sp", stv, src.rearrange("(c p) n -> p c n", p=P), wr=[stage_res[si]])
                return (n, si, stv)

            def finish(n, si, stv):
                e, which = jobs[n]
                bi = n % 3
                if which < 2:
                    dstv = wb[bi][:].rearrange("p (c n) -> p c n", n=FE)
                    for c in range(KC):
                        eng = engines[(n + c) % len(engines)]
                        cast(eng, dstv[:, c, :], stv[:, c, :], [stage_res[si], r_const], [r_wb[bi]], scale=g_ffn[:, c, l:l + 1])
                else:
                    dstv = wb[bi][:].rearrange("p (c n) -> p c n", n=D)
                    cast(engines[n % len(engines)], dstv[:, 0, :], stv[:, 0, :], [stage_res[si]], [r_wb[bi]])
                    cast(engines[(n + 1) % len(engines)], dstv[:, 1, :], stv[:, 1, :], [stage_res[si]], [r_wb[bi]])
                dst = (wgb, wub, wdb)[which][l]
                k.dma("pool", dst[e * P:(e + 1) * P, :], wb[bi][:], rd=[r_wb[bi]], wr=[r_wdram])

            def emit(nslots):
                for _ in range(nslots):
                    if state["pend"] is not None:
                        finish(*state["pend"])
                        state["pend"] = None
                    if state["n"] < len(jobs):
                        state["pend"] = issue(state["n"])
                        state["n"] += 1

            def flush():
                emit(len(jobs) + 1)
            emit.flush = flush
            return emit

        r_wdram = Res("wdram")

        def moe_sparse(l, pstack):
            V = nc.vector
            G = nc.gpsimd
            Mf = sb("Mf", [P, NT, 32], F32, pstack)
            Mb = sb("Mb", [P, NT, 32], BF16, pstack)
            U = sb("Utri", [P, P], BF16, pstack)
            rr = Res("rt_tab")
            k.op("dve", lambda: V.tensor_scalar(out=Mf[:], in0=comb_tok[:], scalar1=0.0, scalar2=None, op0=ALU.is_gt), rd=[r_ctok], wr=[rr])
            k.op("dve", lambda: V.tensor_copy(out=Mb[:], in_=Mf[:]), rd=[rr], wr=[rr])
            k.op("pool", lambda: G.memset(U[:], 1.0), wr=[rr])
            k.op("pool", lambda: G.affine_select(out=U[:], in_=U[:], pattern=[[1, P]], compare_op=ALU.is_ge, fill=0.0, base=-1,
                                                 channel_multiplier=-1), rd=[rr], wr=[rr])
            tab = sb("rtab", [P, 8, 32], F32, pstack)
            C_ = tab[:, 0, :]
            nt_ = tab[:, 1, :]
            pad = tab[:, 2, :]
            incl = tab[:, 3, :]
            offs = tab[:, 4, :]
            tmp = tab[:, 5, :]
            for i in range(NT):
                k.op("pe", lambda: nc.tensor.matmul(ps[:, 0, 0:32], lhsT=ones_b[:], rhs=Mb[:, i, :], start=(i == 0), stop=(i == NT - 1)),
                     rd=[rr, r_const], wr=[pbank[0]], inc=(i == NT - 1))
            k.op("dve", lambda: V.tensor_copy(out=C_, in_=ps[:, 0, 0:32]), rd=[pbank[0]], wr=[rr])
            k.op("dve", lambda: V.memset(nt_, 0.0), wr=[rr])
            for m in range(NT):
                k.op("dve", lambda: V.tensor_scalar(out=tmp, in0=C_, scalar1=float(P * m), scalar2=None, op0=ALU.is_gt), rd=[rr], wr=[rr])
                k.op("dve", lambda: V.tensor_tensor(out=nt_, in0=nt_, in1=tmp, op=ALU.add), rd=[rr], wr=[rr])
            k.op("dve", lambda: V.tensor_scalar(out=pad, in0=nt_, scalar1=float(P), scalar2=None, op0=ALU.mult), rd=[rr], wr=[rr])
            k.op("dve", lambda: V.tensor_copy(out=incl[:, 0:1], in_=pad[:, 0:1]), rd=[rr], wr=[rr])
            for e in range(1, NEXP):
                k.op("dve", lambda: V.tensor_tensor(out=incl[:, e:e + 1], in0=incl[:, e - 1:e], in1=pad[:, e:e + 1], op=ALU.add), rd=[rr], wr=[rr])
            k.op("dve", lambda: V.tensor_tensor(out=offs, in0=incl, in1=pad, op=ALU.subtract), rd=[rr], wr=[rr])
            jt_i = sb("jt_i", [P, NTL], I32, pstack)
            jthr = sb("jthr", [P, NTL], F32, pstack)
            te = sb("te", [P, NTL], F32, pstack)
            tmp2 = sb("tmp2", [P, NTL], F32, pstack)
            pid_i = sb("pid_i", [P, 1], I32, pstack)
            pid_f = sb("pid_f", [P, 1], F32, pstack)
            widx = sb("widx", [P, NTL], I32, pstack)
            k.op("pool", lambda: G.iota(jt_i[:], pattern=[[P, NTL]], base=0, channel_multiplier=0), wr=[rr])
            k.op("pool", lambda: G.iota(pid_i[:], pattern=[[0, 1]], base=0, channel_multiplier=1), wr=[rr])
            k.op("dve", lambda: V.tensor_copy(out=jthr[:], in_=jt_i[:]), rd=[rr], wr=[rr])
            k.op("dve", lambda: V.tensor_copy(out=pid_f[:], in_=pid_i[:]), rd=[rr], wr=[rr])
            k.op("dve", lambda: V.memset(te[:], 0.0), wr=[rr])
            for e in range(NEXP):
                k.op("dve", lambda: V.tensor_scalar(out=tmp2[:], in0=jthr[:], scalar1=incl[:, e:e + 1], scalar2=None, op0=ALU.is_ge), rd=[rr], wr=[rr])
                k.op("dve", lambda: V.tensor_tensor(out=te[:], in0=te[:], in1=tmp2[:], op=ALU.add), rd=[rr], wr=[rr])
            k.op("dve", lambda: V.tensor_scalar(out=te[:], in0=te[:], scalar1=float(NEXP - 1), scalar2=float(P), op0=ALU.min, op1=ALU.mult),
                 rd=[rr], wr=[rr])
            k.op("dve", lambda: V.tensor_scalar(out=te[:], in0=te[:], scalar1=pid_f[:, 0:1], scalar2=None, op0=ALU.add), rd=[rr], wr=[rr])
            k.op("dve", lambda: V.tensor_copy(out=widx[:], in_=te[:]), rd=[rr], wr=[rr])
            slf = sb("slf", [P, 2, NT], F32, pstack)
            sc = sb("sc", [P, 4, 6, 32], F32, pstack)
            r_sc = [Res("sc%d" % i) for i in range(4)]
            for i in range(NT):
                bk = 1 + (i % 3)
                for i2 in range(i):
                    k.op("pe", lambda: nc.tensor.matmul(ps[:, bk, 0:32], lhsT=ones_b[:], rhs=Mb[:, i2, :], start=(i2 == 0), stop=False),
                         rd=[rr, r_const], wr=[pbank[bk]], inc=False)
                k.op("pe", lambda: nc.tensor.matmul(ps[:, bk, 0:32], lhsT=U[:], rhs=Mb[:, i, :], start=(i == 0), stop=True),
                     rd=[rr, r_const], wr=[pbank[bk]])
                q = i % 4
                rq = r_sc[q]
                sl = sc[:, q, 0, :]
                slm = sc[:, q, 1, :]
                big = sc[:, q, 2, :]
                isA = sc[:, q, 3, :]
                wa = sc[:, q, 4, :]
                k.op("dve", lambda: V.tensor_tensor(out=sl, in0=ps[:, bk, 0:32], in1=offs, op=ALU.add), rd=[pbank[bk], rr], wr=[rq])
                k.op("dve", lambda: V.tensor_tensor(out=slm, in0=sl, in1=Mf[:, i, :], op=ALU.mult), rd=[rq, rr], wr=[rq])
                k.op("dve", lambda: V.tensor_reduce(out=slf[:, 1, i:i + 1], in_=slm, axis=AX.X, op=ALU.max), rd=[rq], wr=[rq, r_route])
                k.op("dve", lambda: V.tensor_scalar(out=big, in0=Mf[:, i, :], scalar1=-1.0e9, scalar2=1.0e9, op0=ALU.mult, op1=ALU.add),
                     rd=[rr], wr=[rq])
                k.op("dve", lambda: V.tensor_tensor(out=big, in0=big, in1=slm, op=ALU.add), rd=[rq], wr=[rq])
                k.op("dve", lambda: V.tensor_reduce(out=slf[:, 0, i:i + 1], in_=big, axis=AX.X, op=ALU.min), rd=[rq], wr=[rq, r_route])
                k.op("dve", lambda: V.tensor_scalar(out=isA, in0=big, scalar1=slf[:, 0, i:i + 1], scalar2=None, op0=ALU.is_equal), rd=[rq, r_route], wr=[rq])
                k.op("dve", lambda: V.tensor_tensor(out=wa, in0=isA, in1=comb_tok[:, i, :], op=ALU.mult), rd=[rq, r_ctok], wr=[rq])
                k.op("dve", lambda: V.tensor_reduce(out=wAB[:, 0, i:i + 1], in_=wa, axis=AX.X, op=ALU.add), rd=[rq], wr=[r_route])
                k.op("dve", lambda: V.tensor_reduce(out=wAB[:, 1, i:i + 1], in_=comb_tok[:, i, :], axis=AX.X, op=ALU.add), rd=[r_ctok], wr=[r_route])
                k.op("dve", lambda: V.tensor_tensor(out=wAB[:, 1, i:i + 1], in0=wAB[:, 1, i:i + 1], in1=wAB[:, 0, i:i + 1], op=ALU.subtract),
                     rd=[r_route], wr=[r_route])
            k.op("dve", lambda: V.tensor_copy(out=slotA_i[:], in_=slf[:, 0, :]), rd=[r_route], wr=[r_route])
            k.op("dve", lambda: V.tensor_copy(out=slotB_i[:], in_=slf[:, 1, :]), rd=[r_route], wr=[r_route])
            ht = [sb("ht%d" % i, [P, D], BF16, pstack) for i in range(2)]
            r_ht = [Res("ht0"), Res("ht1")]
            r_hsort = Res("hsort")
            zrow = sb("zrow", [P, D], BF16, pstack)
            r_z = Res("zrow")
            k.op("pool", lambda: G.memset(zrow[:], 0.0), wr=[r_z])
            for j in range(NTL):
                k.dma("sp", hsort[j * P:(j + 1) * P, :], zrow[:], rd=[r_z], wr=[r_hsort])
            for i in range(NT):
                b = i % 2
                k.dma("sp", ht[b][:], vtok[i * P:(i + 1) * P, :], rd=[r_htok], wr=[r_ht[b]])
                for sl_i in (slotA_i, slotB_i):
                    tok = k.dma_indirect(lambda s_: G.indirect_dma_start(out=hsort, out_offset=bass.IndirectOffsetOnAxis(ap=sl_i[:, i:i + 1], axis=0),
                                                                          in_=ht[b][:, :], in_offset=None), [r_ht[b], r_route], [r_hsort])
            wgt = [sb("wgt%d" % i, [P, KC * FE], BF16, pstack) for i in range(2)]
            wut = [sb("wut%d" % i, [P, KC * FE], BF16, pstack) for i in range(2)]
            wdt = [sb("wdt%d" % i, [P, 2 * D], BF16, pstack) for i in range(3)]
            r_wgu = [Res("wgu0"), Res("wgu1")]
            r_wdt = [Res("wdt%d" % i) for i in range(3)]
            hsb = [sb("hsb%d" % i, [P, D], BF16, pstack) for i in range(2)]
            r_hsb = [Res("hsb0"), Res("hsb1")]
            hsT = [sb("hsT%d" % i, [P, KC, P], BF16, pstack) for i in range(2)]
            r_hsT = [Res("hsT0"), Res("hsT1")]
            sg = [sb("ssg%d" % i, [P, 2 * P], BF16, pstack) for i in range(2)]
            r_sg = [Res("ssg0"), Res("ssg1")]
            aT = [sb("aT%d" % i, [P, 2 * P], BF16, pstack) for i in range(2)]
            r_aT = [Res("aT0"), Res("aT1")]
            ysb = [sb("ysb%d" % i, [P, D], F32, pstack) for i in range(2)]
            r_ysb = [Res("ysb0"), Res("ysb1")]

            def stA(j):
                b2_, b3_ = j % 2, j % 3
                k.dma_indirect(lambda s_: G.indirect_dma_start(out=wgt[b2_][:, :], out_offset=None, in_=wgb[l],
                                                               in_offset=bass.IndirectOffsetOnAxis(ap=widx[:, j:j + 1], axis=0)), [rr, r_wdram], [r_wgu[b2_]])
                k.dma_indirect(lambda s_: G.indirect_dma_start(out=wut[b2_][:, :], out_offset=None, in_=wub[l],
                                                               in_offset=bass.IndirectOffsetOnAxis(ap=widx[:, j:j + 1], axis=0)), [rr, r_wdram], [r_wgu[b2_]])
                k.dma_indirect(lambda s_: G.indirect_dma_start(out=wdt[b3_][:, :], out_offset=None, in_=wdb[l],
                                                               in_offset=bass.IndirectOffsetOnAxis(ap=widx[:, j:j + 1], axis=0)), [rr, r_wdram], [r_wdt[b3_]])
                k.dma("sp", hsb[b2_][:], hsort[j * P:(j + 1) * P, :], rd=[r_hsort], wr=[r_hsb[b2_]])
                bk = j % 2
                pb = ps[:, bk, :].bitcast(BF16)
                for c in range(KC):
                    k.op("pe", lambda: nc.tensor.transpose(out=pb[:, c * P:(c + 1) * P], in_=hsb[b2_][:, c * P:(c + 1) * P], identity=ident_b[:]),
                         rd=[r_hsb[b2_], r_const], wr=[pbank[bk]], inc=(c == KC - 1))
                k.op("dve", lambda: V.tensor_copy(out=hsT[b2_][:], in_=pb.rearrange("p (c n) -> p c n", n=P)), rd=[pbank[bk]], wr=[r_hsT[b2_]])

            def stB(j):
                b2_ = j % 2
                bk = 2 + j % 2
                n = 0
                for (wt, off) in ((wgt[b2_], 0), (wut[b2_], 2 * P)):
                    for fc in range(2):
                        for c in range(KC):
                            k.op("pe", lambda: nc.tensor.matmul(ps[:, bk, off + fc * P:off + (fc + 1) * P],
                                                               lhsT=wt[:, c * FE + fc * P:c * FE + (fc + 1) * P], rhs=hsT[b2_][:, c, :],
                                                               start=(n == 0), stop=(c == KC - 1), skip_group_check=True),
                                 rd=[r_wgu[b2_], r_hsT[b2_]], wr=[pbank[bk]], inc=(c == KC - 1 and fc == 1))
                            n += 1
                k.op("act", lambda: nc.scalar.activation(out=sg[b2_][:], in_=ps[:, bk, 0:2 * P], func=AF.Silu), rd=[pbank[bk]], wr=[r_sg[b2_]])
                k.op("dve", lambda: V.tensor_tensor(out=aT[b2_][:], in0=ps[:, bk, 2 * P:4 * P], in1=sg[b2_][:], op=ALU.mult),
                     rd=[pbank[bk], r_sg[b2_]], wr=[r_aT[b2_]])

            def stC(j):
                b2_, b3_ = j % 2, j % 3
                for half in range(2):
                    bk = 4 + 2 * (j % 2) + half
                    for fc in range(2):
                        k.op("pe", lambda: nc.tensor.matmul(ps[:, bk, :], lhsT=aT[b2_][:, fc * P:(fc + 1) * P],
                                                           rhs=wdt[b3_][:, fc * D + half * 512:fc * D + (half + 1) * 512],
                                                           start=(fc == 0), stop=(fc == 1)), rd=[r_aT[b2_], r_wdt[b3_]], wr=[pbank[bk]], inc=(fc == 1))
                    if half == 0:
                        k.op("act", lambda: nc.scalar.copy(out=ysb[b2_][:, 0:512], in_=ps[:, bk, :]), rd=[pbank[bk]], wr=[r_ysb[b2_]])
                    else:
                        k.op("dve", lambda: V.tensor_copy(out=ysb[b2_][:, 512:1024], in_=ps[:, bk, :]), rd=[pbank[bk]], wr=[r_ysb[b2_]])
                k.dma("sp", ysort[j * P:(j + 1) * P, :], ysb[b2_][:], rd=[r_ysb[b2_]], wr=[r_ysort])

            run_pipeline(NTL, [stA, stB, stC])

        def conv_c1(l, j, pstack, src_ap, r_src):
            w1 = sb("w1", [P, KC, 2 * D], BF16, pstack)
            r_w1 = Res("w1")
            load_w_bf16(w1[:], c_w1[j], KC, 2 * D, gain=g_mix[:, :, l], r_dst=r_w1)
            b1 = load_featvec("b1v%d" % l, c_b1[j].rearrange("(r d) -> r d", d=D), 2, pstack)
            zt = sb("zpad", [P, KC, 32], BF16, pstack)
            r_z = Res("zpad")
            k.op("pool", lambda: nc.gpsimd.memset(zt[:], 0.0), wr=[r_z])
            k.dma("pool", vscr[:, :, 0:32].rearrange("c p t -> p c t"), zt[:], rd=[r_z], wr=[r_vscr[0]])
            hs = HStage(pstack, flush=False)
            pp = make_prepass(l, pstack) if SPARSE else None
            xt = [sb("xt%d" % i, [P, D], F32, pstack) for i in range(3)]
            r_xt = [Res("xt%d" % i) for i in range(3)]
            sgb = [sb("sgb%d" % i, [P, 512], F32, pstack) for i in range(2)]
            r_sgb = [Res("sgb0"), Res("sgb1")]
            vb = [sb("vb%d" % i, [P, KC, 512], BF16, pstack) for i in range(2)]
            r_vb = [Res("vb0"), Res("vb1")]
            def st_A(I):
                for sub in range(4):
                    t = I * 4 + sub
                    xi = t % 3
                    k.dma("sp", xt[xi][:], src_ap[t * P:(t + 1) * P, :], rd=[r_src[I]], wr=[r_xt[xi]])
                    hs.norm_put(t, xt[xi][:], r_xt[xi], 7)
            def st_B(I):
                hi = I % 2
                hT_mt = hs.buf[hi]
                r_hmt = hs.res[hi]
                for cc in range(KC):
                    ba = 0 + 2 * (cc % 2)
                    bg = 1 + 2 * (cc % 2)
                    si = cc % 2
                    for c in range(KC):
                        k.op("pe", lambda: nc.tensor.matmul(ps[:, ba, :], lhsT=w1[:, c, cc * P:(cc + 1) * P], rhs=hT_mt[:, c, :],
                                                           start=(c == 0), stop=(c == KC - 1)), rd=[r_w1, r_hmt], wr=[pbank[ba]],
                             inc=(c == KC - 1))
                    for c in range(KC):
                        k.op("pe", lambda: nc.tensor.matmul(ps[:, bg, :], lhsT=w1[:, c, D + cc * P:D + (cc + 1) * P], rhs=hT_mt[:, c, :],
                                                           start=(c == 0), stop=(c == KC - 1)), rd=[r_w1, r_hmt], wr=[pbank[bg]],
                             inc=(c == KC - 1))
                    k.op("act", lambda: nc.scalar.activation(out=sgb[si][:], in_=ps[:, bg, :], func=AF.Sigmoid, bias=b1[:, cc, 1:2]),
                         rd=[pbank[bg], r_const], wr=[r_sgb[si]])
                    k.op("dve", lambda: nc.vector.scalar_tensor_tensor(out=vb[hi][:, cc, :], in0=ps[:, ba, :], scalar=b1[:, cc, 0:1],
                                                                       in1=sgb[si][:], op0=ALU.add, op1=ALU.mult),
                         rd=[pbank[ba], r_sgb[si], r_const], wr=[r_vb[hi]])
                k.dma("pool", vscr[:, :, 32 + I * 512:32 + (I + 1) * 512].rearrange("c p t -> p c t"), vb[hi][:],
                      rd=[r_vb[hi]], wr=[r_vscr[I + 1]])

            def st_P(I):
                if pp is not None:
                    pp(-(-97 // NM))

            run_pipeline(NM, [st_A, st_B, st_P])
            if pp is not None:
                pp.flush()

        def conv_c2(l, j, pstack, src_ap, r_src):
            w2 = sb("w2", [P, KC, D], BF16, pstack)
            r_w2 = Res("w2")
            load_w_bf16(w2[:], c_w2[j], KC, D, r_dst=r_w2)
            cv = sb("cv_vec", [P, KC, 40], F32, pstack)
            r_cv = Res("cv")
            with ExitStack() as st:
                rows = sb("cv_rows", [40, D], F32, st)
                r_rows = Res("rows")
                k.op("pool", lambda: nc.gpsimd.memset(rows[:], 0.0), wr=[r_rows])
                k.dma("sp", rows[0:CW, :], c_wdw[j], wr=[r_rows])
                k.dma("sp", rows[32:33, :], c_bdw[j:j + 1, :], wr=[r_rows])
                k.dma("sp", rows[33:34, :], c_lng[j:j + 1, :], wr=[r_rows])
                k.dma("sp", rows[34:35, :], c_lnb[j:j + 1, :], wr=[r_rows])
                for c in range(KC):
                    bk = c % 2
                    k.op("pe", lambda: nc.tensor.transpose(out=ps[:, bk, 0:36], in_=rows[0:36, c * P:(c + 1) * P],
                                                          identity=ident_f[0:36, 0:36]), rd=[r_rows, r_const], wr=[pbank[bk]])
                    k.op("dve", lambda: nc.vector.tensor_copy(out=cv[:, c, 0:36], in_=ps[:, bk, 0:36]), rd=[pbank[bk]], wr=[r_cv])
                k.barrier()
            b2bc = sb("b2bc", [P, D], F32, pstack)
            k.dma("sp", b2bc[:], c_b2[j].partition_broadcast(P), wr=[r_cv])
            diag = sb("diag", [P, KC, CW, P], BF16, pstack)
            r_diag = Res("diag")
            n = 0
            for c in range(KC):
                for t_ in range(CW):
                    if n % 2 == 0:
                        k.op("pool", lambda: nc.gpsimd.tensor_scalar(out=diag[:, c, t_, :], in0=ident_f[:], scalar1=cv[:, c, t_:t_ + 1],
                                                                     scalar2=1.0, op0=ALU.mult, op1=ALU.mult),
                             rd=[r_cv, r_const], wr=[r_diag])
                    else:
                        k.op("dve", lambda: nc.vector.tensor_scalar(out=diag[:, c, t_, :], in0=ident_f[:], scalar1=cv[:, c, t_:t_ + 1],
                                                                    scalar2=None, op0=ALU.mult), rd=[r_cv, r_const], wr=[r_diag])
                    n += 1
            prep = MoePrep(l, pstack, (2, 3, 4))
            ring = [sb("ring%d" % i, [P, KC, 32 + 512], BF16, pstack) for i in range(1)] * 2
            r_ring = [Res("ring0")] * 2
            xt = [sb("xt%d" % i, [P, D], F32, pstack) for i in range(3)]
            r_xt = [Res("xt%d" % i) for i in range(3)]
            ybuf = sb("ybuf", [P, KC, 512], F32, pstack)
            r_y = Res("ybuf")
            yb = [sb("yb%d" % i, [P, 512], BF16, pstack) for i in range(2)]
            r_yb = [Res("yb0"), Res("yb1")]
            ysq = [sb("ysq%d" % i, [P, 512], BF16, pstack) for i in range(2)]
            r_ysq = [Res("ysq0"), Res("ysq1")]
            mean = sb("mean", [P, 512], F32, pstack)
            rstd_t = sb("rstd_t", [P, 512], F32, pstack)
            r_ln = Res("ln")
            zt = sb("ztmp", [P, 2, 512], F32, pstack)
            r_zt = [Res("zt0"), Res("zt1")]
            zT = sb("zT", [P, KC, 512], BF16, pstack)
            r_zT = Res("zT")
            xcnt = [0]
            for I in range(NM):
                ri = I % 2
                k.dma("sp", ring[ri][:], vscr[:, :, I * 512:I * 512 + 544].rearrange("c p t -> p c t"),
                      rd=[r_vscr[I], r_vscr[I + 1]], wr=[r_ring[ri]])
                pend_stats = []
                for cc in range(KC):
                    bc_ = 0 + (cc % 2)
                    si = cc % 2
                    for t_ in range(CW):
                        k.op("pe", lambda: nc.tensor.matmul(ps[:, bc_, :], lhsT=diag[:, cc, t_, :], rhs=ring[ri][:, cc, 2 + t_:2 + t_ + 512],
                                                           start=(t_ == 0), stop=(t_ == CW - 1)), rd=[r_diag, r_ring[ri]], wr=[pbank[bc_]],
                             inc=(t_ == CW - 1))
                    k.op("act", lambda: nc.scalar.activation(out=ybuf[:, cc, :], in_=ps[:, bc_, :], func=AF.Identity, bias=cv[:, cc, 32:33]),
                         rd=[pbank[bc_], r_cv], wr=[r_y])
                    k.op("act", lambda: nc.scalar.activation(out=ysq[si][:], in_=ps[:, bc_, :], func=AF.Square, bias=cv[:, cc, 32:33]),
                         rd=[pbank[bc_], r_cv], wr=[r_ysq[si]])
                    k.op("pool", lambda: nc.gpsimd.tensor_copy(out=yb[si][:], in_=ybuf[:, cc, :]), rd=[r_y], wr=[r_yb[si]])

                    def stats(c2):
                        s2 = c2 % 2
                        k.op("pe", lambda: nc.tensor.matmul(ps[:, 6, :], lhsT=ones_b[:], rhs=yb[s2][:], start=(c2 == 0), stop=(c2 == KC - 1),
                                                           skip_group_check=True), rd=[r_yb[s2], r_const], wr=[pbank[6]], inc=(c2 == KC - 1))
                        k.op("pe", lambda: nc.tensor.matmul(ps[:, 7, :], lhsT=ones_b[:], rhs=ysq[s2][:], start=(c2 == 0), stop=(c2 == KC - 1),
                                                           skip_group_check=True), rd=[r_ysq[s2], r_const], wr=[pbank[7]], inc=True)
                    pend_stats.append(cc)
                    if len(pend_stats) > 1:
                        stats(pend_stats.pop(0))
                while pend_stats:
                    stats(pend_stats.pop(0))
                k.op("act", lambda: nc.scalar.activation(out=mean[:], in_=ps[:, 6, :], func=AF.Copy, scale=1.0 / D), rd=[pbank[6]], wr=[r_ln])
                k.op("act", lambda: nc.scalar.activation(out=rstd_t[:], in_=ps[:, 6, :], func=AF.Square, scale=1.0 / D), rd=[pbank[6]], wr=[r_ln])
                k.op("dve", lambda: nc.vector.scalar_tensor_tensor(out=rstd_t[:], in0=ps[:, 7, :], scalar=1.0 / D, in1=rstd_t[:],
                                                                   op0=ALU.mult, op1=ALU.subtract), rd=[pbank[7], r_ln], wr=[r_ln])
                k.op("act", lambda: nc.scalar.activation(out=rstd_t[:], in_=rstd_t[:], func=AF.Sqrt, bias=eps_col[:, 0:1]),
                     rd=[r_ln, r_const], wr=[r_ln])
                k.op("dve", lambda: nc.vector.reciprocal(out=rstd_t[:], in_=rstd_t[:]), rd=[r_ln], wr=[r_ln])
                for cc in range(KC):
                    zi = cc % 2
                    k.op("pool", lambda: nc.gpsimd.tensor_tensor(out=zt[:, zi, :], in0=ybuf[:, cc, :], in1=mean[:], op=ALU.subtract),
                         rd=[r_y, r_ln], wr=[r_zt[zi]])
                    k.op("dve", lambda: nc.vector.tensor_tensor(out=zt[:, zi, :], in0=zt[:, zi, :], in1=rstd_t[:], op=ALU.mult),
                         rd=[r_zt[zi], r_ln], wr=[r_zt[zi]])
                    k.op("act", lambda: nc.scalar.activation(out=zT[:, cc, :], in_=zt[:, zi, :], func=AF.Silu, scale=cv[:, cc, 33:34],
                                                            bias=cv[:, cc, 34:35]), rd=[r_zt[zi], r_cv], wr=[r_zT])
                def st_X(sub, I=I):
                    t = I * 4 + sub
                    xi = t % 3
                    k.dma("sp", xt[xi][:], src_ap[t * P:(t + 1) * P, :], rd=[r_src[I]], wr=[r_xt[xi]])
                    k.op("pool", lambda: nc.gpsimd.tensor_tensor(out=xt[xi][:], in0=xt[xi][:], in1=b2bc[:], op=ALU.add),
                         rd=[r_xt[xi], r_cv], wr=[r_xt[xi]])
                    for half in range(2):
                        bk = 0 + half
                        for cc in range(KC):
                            k.op("pe", lambda: nc.tensor.matmul(ps[:, bk, :], lhsT=zT[:, cc, sub * P:(sub + 1) * P],
                                                               rhs=w2[:, cc, half * 512:(half + 1) * 512], start=(cc == 0), stop=(cc == KC - 1)),
                                 rd=[r_zT, r_w2], wr=[pbank[bk]], inc=(cc == KC - 1))
                        k.op("dve", lambda: nc.vector.tensor_tensor(out=xt[xi][:, half * 512:(half + 1) * 512], in0=ps[:, bk, :],
                                                                    in1=xt[xi][:, half * 512:(half + 1) * 512], op=ALU.add),
                             rd=[pbank[bk], r_xt[xi]], wr=[r_xt[xi]])
                    k.dma("pool", xres[t * P:(t + 1) * P, :], xt[xi][:], rd=[r_xt[xi]], wr=[r_xres[I]])

                run_pipeline(4, [st_X, lambda sub, I=I: prep.p1(xt[(I * 4 + sub) % 3][:], r_xt[(I * 4 + sub) % 3], I * 4 + sub),
                                 lambda sub, I=I: prep.p2(I * 4 + sub)])

        def ple_phase(l, pstack, last, next_kind=None):
            wpg = sb("wpg", [P, KC, D], BF16, pstack)
            wpp = sb("wpp", [P, 2, D], BF16, pstack)
            r_wp = Res("wp")
            load_w_bf16(wpg[:], ple_wg[l], KC, D, gain=g_ple[:, :, l], r_dst=r_wp)
            load_w_bf16(wpp[:], ple_wp[l], 2, D, r_dst=r_wp)
            if last:
                fn_bc = sb("fn_bc", [P, D], F32, pstack)
                k.dma("sp", fn_bc[:], fin_norm.partition_broadcast(P), wr=[r_wp])
            hs2 = None
            if next_kind == "attn":
                hs2 = HStage(pstack)
            NX = 7
            rs_d = {}
            xt = [sb("pxt%d" % i, [P, D], F32, pstack) for i in range(NX)]
            r_xt = [Res("pxt%d" % i) for i in range(NX)]
            pt = [sb("ppt%d" % i, [P, PLE], F32, pstack) for i in range(4)]
            r_pt = [Res("ppt%d" % i) for i in range(4)]
            xnb = [sb("pxnb%d" % i, [P, D + PLE], BF16, pstack) for i in range(2)]
            r_xnb = [Res("pxnb0"), Res("pxnb1")]
            hT = [sb("phT%d" % i, [P, KC + 2, P], BF16, pstack) for i in range(2)]
            r_h = [Res("phT0"), Res("phT1")]
            sig = [sb("psig%d" % i, [P, D], F32, pstack) for i in range(2)]
            r_sig = [Res("psig0"), Res("psig1")]
            yo = [sb("pyo%d" % i, [P, D], F32, pstack) for i in range(2)]
            r_yo = [Res("pyo0"), Res("pyo1")]
            if SPARSE:
                yab = [sb("yab%d" % i, [P, 2, D], F32, pstack) for i in range(2)]
                r_yab = [Res("yab0"), Res("yab1")]
            def st_A(t):
                xi = t % NX
                i2 = t % 2
                mt = t // 4
                k.dma("sp", xt[xi][:], xres[t * P:(t + 1) * P, :], rd=[r_xres[mt]], wr=[r_xt[xi]])
                k.dma("sp", pt[t % 4][:], p_in[l, t * P:(t + 1) * P, :], wr=[r_pt[t % 4]])
                if SPARSE:
                    for q_, sl_i in enumerate((slotA_i, slotB_i)):
                        k.dma_indirect(lambda s_: nc.gpsimd.indirect_dma_start(out=yab[i2][:, q_, :], out_offset=None, in_=ysort,
                                                                               in_offset=bass.IndirectOffsetOnAxis(ap=sl_i[:, t:t + 1], axis=0)),
                                       [r_ysort, r_route], [r_yab[i2]])

            def st_A1(t):
                xi = t % NX
                i2 = t % 2
                mt = t // 4
                if SPARSE:
                    for q_ in range(2):
                        k.op("dve", lambda: nc.vector.scalar_tensor_tensor(out=xt[xi][:], in0=yab[i2][:, q_, :], scalar=wAB[:, q_, t:t + 1],
                                                                           in1=xt[xi][:], op0=ALU.mult, op1=ALU.add),
                             rd=[r_yab[i2], r_xt[xi], r_route], wr=[r_xt[xi]])
                rs_d[t % 4] = rms_rstd(xt[xi][:], r_xt[xi])

            def st_Ab(t):
                xi = t % NX
                i2 = t % 2
                rstd, r_rs = rs_d[t % 4]
                k.op("dve", lambda: nc.vector.tensor_scalar(out=xnb[i2][:, 0:D], in0=xt[xi][:], scalar1=rstd, scalar2=None, op0=ALU.mult),
                     rd=[r_xt[xi], r_rs], wr=[r_xnb[i2]])
                k.op("pool", lambda: nc.gpsimd.tensor_copy(out=xnb[i2][:, D:D + PLE], in_=pt[t % 4][:]), rd=[r_pt[t % 4]], wr=[r_xnb[i2]])
            def st_A2(t):
                xi = t % NX
                i2 = t % 2
                bT = 6
                pb = ps[:, bT, :].bitcast(BF16)
                for c in range(KC):
                    k.op("pe", lambda: nc.tensor.transpose(out=pb[:, c * P:(c + 1) * P], in_=xnb[i2][:, c * P:(c + 1) * P],
                                                          identity=ident_b[:]), rd=[r_xnb[i2], r_const], wr=[pbank[bT]], inc=(c == KC - 1))
                k.op("act", lambda: nc.scalar.copy(out=hT[i2][:, 0:KC, :], in_=pb.rearrange("p (c n) -> p c n", n=P)),
                     rd=[pbank[bT]], wr=[r_h[i2]])
                bT2 = 4 + (t % 2)
                pb2 = ps[:, bT2, :].bitcast(BF16)
                for c in range(2):
                    k.op("pe", lambda: nc.tensor.transpose(out=pb2[:, c * P:(c + 1) * P], in_=xnb[i2][:, D + c * P:D + (c + 1) * P],
                                                          identity=ident_b[:]), rd=[r_xnb[i2], r_const], wr=[pbank[bT2]], inc=(c == 1))
                k.op("dve", lambda: nc.vector.tensor_copy(out=hT[i2][:, KC:KC + 2, :], in_=pb2[:, 0:2 * P].rearrange("p (c n) -> p c n", n=P)),
                     rd=[pbank[bT2]], wr=[r_h[i2]])
            def st_B(t):
                xi = t % NX
                i2 = t % 2
                mt = t // 4
                for half in range(2):
                    bgk = 0 + half
                    bpk = 2 + half
                    hs_ = slice(half * 512, (half + 1) * 512)
                    for c in range(KC):
                        k.op("pe", lambda: nc.tensor.matmul(ps[:, bgk, :], lhsT=hT[i2][:, c, :], rhs=wpg[:, c, hs_], start=(c == 0),
                                                           stop=(c == KC - 1)), rd=[r_h[i2], r_wp], wr=[pbank[bgk]], inc=(c == KC - 1))
                    for c in range(2):
                        k.op("pe", lambda: nc.tensor.matmul(ps[:, bpk, :], lhsT=hT[i2][:, KC + c, :], rhs=wpp[:, c, hs_], start=(c == 0),
                                                           stop=(c == 1)), rd=[r_h[i2], r_wp], wr=[pbank[bpk]], inc=(c == 1))
                    k.op("act", lambda: nc.scalar.activation(out=sig[i2][:, hs_], in_=ps[:, bgk, :], func=AF.Sigmoid),
                         rd=[pbank[bgk]], wr=[r_sig[i2]])
                    k.op("dve", lambda: nc.vector.tensor_tensor(out=sig[i2][:, hs_], in0=ps[:, bpk, :], in1=sig[i2][:, hs_], op=ALU.mult),
                         rd=[pbank[bpk], r_sig[i2]], wr=[r_sig[i2]])
            def st_B2(t):
                xi = t % NX
                i2 = t % 2
                mt = t // 4
                k.op("pool", lambda: nc.gpsimd.tensor_tensor(out=xt[xi][:], in0=xt[xi][:], in1=sig[i2][:], op=ALU.add),
                     rd=[r_xt[xi], r_sig[i2]], wr=[r_xt[xi]])
                if last:
                    rstd2, r_rs2 = rms_rstd(xt[xi][:], r_xt[xi])
                    k.op("dve", lambda: nc.vector.scalar_tensor_tensor(out=yo[i2][:], in0=xt[xi][:], scalar=rstd2, in1=fn_bc[:],
                                                                       op0=ALU.mult, op1=ALU.mult), rd=[r_xt[xi], r_rs2, r_wp], wr=[r_yo[i2]])
                    k.dma("pool", y_out[t * P:(t + 1) * P, :], yo[i2][:], rd=[r_yo[i2]], wr=[r_xres[mt]])
                else:
                    k.dma("pool", xres[t * P:(t + 1) * P, :], xt[xi][:], rd=[r_xt[xi]], wr=[r_xres[mt]])
                    if hs2 is not None:
                        hs2.norm_put(t, xt[xi][:], r_xt[xi], 7)

            run_pipeline(NT, [st_A, st_A1, st_Ab, st_A2, st_B, st_B2])

        def attn_a0(pstack, src_ap, r_src):
            hs = HStage(pstack)
            xt = [sb("a0xt%d" % i, [P, D], F32, pstack) for i in range(3)]
            r_xt = [Res("a0xt%d" % i) for i in range(3)]
            for t in range(NT):
                xi = t % 3
                k.dma("sp", xt[xi][:], src_ap[t * P:(t + 1) * P, :], rd=[r_src[t // 4]], wr=[r_xt[xi]])
                hs.norm_put(t, xt[xi][:], r_xt[xi], 7)

        def attn_a1(l, j, pstack):
            wq = sb("wqkv", [P, KC, 3 * D], BF16, pstack)
            r_wq = Res("wqkv")
            load_w_bf16(wq[:], a_wqkv[j], KC, 3 * D, gain=g_mix[:, :, l], r_dst=r_wq)
            cosr = sb("cosr", [P, NT, 64], F32, pstack)
            sinr = sb("sinr", [P, NT, 64], F32, pstack)
            r_rope = Res("rope")
            with ExitStack() as st:
                pos_i = sb("pos_i", [NT, P], I32, st)
                pos_f = sb("pos_f", [NT, P], F32, st)
                posT = sb("posT", [P, NT], F32, st)
                ang = sb("ang", [P, 2, NT, 8], F32, st)
                tmpa = sb("tmpa", [P, 2, NT, 8], F32, st)
                tmpi = sb("tmpi", [P, 2, NT, 8], I32, st)
                r_p = Res("pos")
                k.dma("sp", pos_i[:], pos_in, wr=[r_p])
                k.op("dve", lambda: nc.vector.tensor_copy(out=pos_f[:], in_=pos_i[:]), rd=[r_p], wr=[r_p])
                k.op("pe", lambda: nc.tensor.transpose(out=ps[:, 0, 0:NT], in_=pos_f[:], identity=ident_f[0:NT, 0:NT]),
                     rd=[r_p, r_const], wr=[pbank[0]])
                k.op("dve", lambda: nc.vector.tensor_copy(out=posT[:], in_=ps[:, 0, 0:NT]), rd=[pbank[0]], wr=[r_p])
                V = nc.vector
                for i in range(8):
                    inv = THETA ** (-(2.0 * i) / ROPE)
                    k.op("dve", lambda: V.tensor_scalar(out=ang[:, 0, :, i], in0=posT[:], scalar1=float(inv), scalar2=None, op0=ALU.mult),
                         rd=[r_p], wr=[r_p])
                k.op("dve", lambda: V.tensor_scalar(out=ang[:, 1], in0=ang[:, 0], scalar1=float(math.pi / 2), scalar2=None, op0=ALU.add),
                     rd=[r_p], wr=[r_p])
                TWO_PI = float(2 * math.pi)
                A = ang[:].rearrange("p a t i -> p (a t i)")
                T_ = tmpa[:].rearrange("p a t i -> p (a t i)")
                TI = tmpi[:].rearrange("p a t i -> p (a t i)")
                k.op("dve", lambda: V.tensor_scalar(out=T_, in0=A, scalar1=float(1.0 / TWO_PI), scalar2=None, op0=ALU.mult), rd=[r_p], wr=[r_p])
                k.op("dve", lambda: V.tensor_copy(out=TI, in_=T_), rd=[r_p], wr=[r_p])
                k.op("dve", lambda: V.tensor_copy(out=T_, in_=TI), rd=[r_p], wr=[r_p])
                k.op("dve", lambda: V.scalar_tensor_tensor(out=A, in0=T_, scalar=-TWO_PI, in1=A, op0=ALU.mult, op1=ALU.add), rd=[r_p], wr=[r_p])
                k.op("dve", lambda: V.tensor_scalar(out=T_, in0=A, scalar1=float(math.pi), scalar2=-TWO_PI, op0=ALU.is_gt, op1=ALU.mult),
                     rd=[r_p], wr=[r_p])
                k.op("dve", lambda: V.tensor_tensor(out=A, in0=A, in1=T_, op=ALU.add), rd=[r_p], wr=[r_p])
                k.op("dve", lambda: V.tensor_scalar(out=T_, in0=A, scalar1=float(-math.pi), scalar2=TWO_PI, op0=ALU.is_lt, op1=ALU.mult),
                     rd=[r_p], wr=[r_p])
                k.op("dve", lambda: V.tensor_tensor(out=A, in0=A, in1=T_, op=ALU.add), rd=[r_p], wr=[r_p])
                k.op("dve", lambda: V.tensor_scalar(out=A, in0=A, scalar1=float(math.pi), scalar2=float(-math.pi), op0=ALU.min, op1=ALU.max),
                     rd=[r_p], wr=[r_p])
                k.op("act", lambda: nc.scalar.activation(out=T_, in_=A, func=AF.Sin), rd=[r_p], wr=[r_p])
                for r8 in range(8):
                    k.op("dve", lambda: V.tensor_copy(out=sinr[:, :, r8 * 8:(r8 + 1) * 8], in_=tmpa[:, 0]), rd=[r_p], wr=[r_rope])
                    k.op("dve", lambda: V.tensor_copy(out=cosr[:, :, r8 * 8:(r8 + 1) * 8], in_=tmpa[:, 1]), rd=[r_p], wr=[r_rope])
                k.barrier()
            if debug == "a1r":
                return
            hT = [sb("a1hT%d" % i, [P, KC, 512], BF16, pstack) for i in range(2)]
            r_hT = [Res("a1hT0"), Res("a1hT1")]
            qk = [sb("qk%d" % i, [P, 2 * D], BF16, pstack) for i in range(2)]
            r_qk = [Res("qk0"), Res("qk1")]
            vt = [sb("vt%d" % i, [P, D], BF16, pstack) for i in range(2)]
            r_vt = [Res("vt0"), Res("vt1")]
            rtmp = sb("rtmp", [P, 2, 4, 64], F32, pstack)
            r_rt = [Res("rtmp0"), Res("rtmp1")]
            qst = [sb("qst%d" % i, [P, NH, 512], BF16, pstack) for i in range(2)]
            kst = [sb("kst%d" % i, [P, NH, 512], BF16, pstack) for i in range(2)]
            r_qst = [Res("qst0"), Res("qst1")]
            r_kst = [Res("kst0"), Res("kst1")]
            V = nc.vector
            cnt = [0]

            def st_A(t):
                I, sub = t // 4, t % 4
                hi = I % 2
                i2 = t % 2
                if sub == 0:
                    k.dma("sp", hT[hi][:], hscr[:, :, I * 512:(I + 1) * 512].rearrange("c p t -> p c t"), rd=[r_hscr[I]], wr=[r_hT[hi]])
                for jc in range(6):
                    bk = cnt[0] % 4
                    cnt[0] += 1
                    for c in range(KC):
                        k.op("pe", lambda: nc.tensor.matmul(ps[:, bk, :], lhsT=hT[hi][:, c, sub * P:(sub + 1) * P],
                                                           rhs=wq[:, c, jc * 512:(jc + 1) * 512], start=(c == 0), stop=(c == KC - 1)),
                             rd=[r_hT[hi], r_wq], wr=[pbank[bk]], inc=(c == KC - 1))
                    if jc >= 4:
                        dstv = vt[i2][:, (jc - 4) * 512:(jc - 3) * 512]
                        k.op("act", lambda: nc.scalar.copy(out=dstv, in_=ps[:, bk, :]), rd=[pbank[bk]], wr=[r_vt[i2]])
                        continue
                    dst = qk[i2][:, jc * 512:(jc + 1) * 512]
                    k.op("act", lambda: nc.scalar.copy(out=dst, in_=ps[:, bk, :]), rd=[pbank[bk]], wr=[r_qk[i2]])
                    ri = cnt[0] % 2
                    pv = ps[:, bk, :].rearrange("p (s d) -> p s d", d=64)
                    dv = dst.rearrange("p (s d) -> p s d", d=64)
                    x1 = pv[:, :, 0:8]
                    x2 = pv[:, :, 8:16]
                    cs = cosr[:, t, :].rearrange("p (s i) -> p s i", i=8)
                    sn = sinr[:, t, :].rearrange("p (s i) -> p s i", i=8)
                    T4 = rtmp[:, ri]
                    a_ = T4[:, 0].rearrange("p (s i) -> p s i", i=8)
                    b_ = T4[:, 1].rearrange("p (s i) -> p s i", i=8)
                    c_ = T4[:, 2].rearrange("p (s i) -> p s i", i=8)
                    d_ = T4[:, 3].rearrange("p (s i) -> p s i", i=8)
                    k.op("dve", lambda: V.tensor_tensor(out=a_, in0=x1, in1=cs, op=ALU.mult), rd=[pbank[bk], r_rope], wr=[r_rt[ri]])
                    k.op("dve", lambda: V.tensor_tensor(out=b_, in0=x2, in1=sn, op=ALU.mult), rd=[pbank[bk], r_rope], wr=[r_rt[ri]])
                    k.op("dve", lambda: V.tensor_tensor(out=c_, in0=x2, in1=cs, op=ALU.mult), rd=[pbank[bk], r_rope], wr=[r_rt[ri]])
                    k.op("dve", lambda: V.tensor_tensor(out=d_, in0=x1, in1=sn, op=ALU.mult), rd=[pbank[bk], r_rope], wr=[r_rt[ri]])
                    k.op("pool", lambda: nc.gpsimd.tensor_tensor(out=dv[:, :, 0:8], in0=a_, in1=b_, op=ALU.subtract),
                         rd=[r_rt[ri]], wr=[r_qk[i2]])
                    k.op("pool", lambda: nc.gpsimd.tensor_tensor(out=dv[:, :, 8:16], in0=c_, in1=d_, op=ALU.add),
                         rd=[r_rt[ri]], wr=[r_qk[i2]])

            def st_B(t):
                I, sub = t // 4, t % 4
                hi = I % 2
                i2 = t % 2
                k.dma("pool", vtok[t * P:(t + 1) * P, :], vt[i2][:], rd=[r_vt[i2]], wr=[r_vscr[0]])
                for which in range(2):
                    bk = 4 + which * 2 + (t % 2)
                    pb = ps[:, bk, :].bitcast(BF16)
                    for h in range(NH):
                        k.op("pe", lambda: nc.tensor.transpose(out=pb[:, h * P:(h + 1) * P],
                                                              in_=qk[i2][:, which * D + h * P:which * D + (h + 1) * P], identity=ident_b[:]),
                             rd=[r_qk[i2], r_const], wr=[pbank[bk]], inc=(h == NH - 1))
                    stg = (qst if which == 0 else kst)[hi]
                    r_stg = (r_qst if which == 0 else r_kst)[hi]
                    if which == 0:
                        k.op("dve", lambda: V.tensor_copy(out=stg[:, :, sub * P:(sub + 1) * P], in_=pb.rearrange("p (h n) -> p h n", n=P)),
                             rd=[pbank[bk]], wr=[r_stg])
                    else:
                        k.op("act", lambda: nc.scalar.copy(out=stg[:, :, sub * P:(sub + 1) * P], in_=pb.rearrange("p (h n) -> p h n", n=P)),
                             rd=[pbank[bk]], wr=[r_stg])
                if sub == 3:
                    k.dma("pool", qscr[:, :, I * 512:(I + 1) * 512].rearrange("h p t -> p h t"), qst[hi][:], rd=[r_qst[hi]], wr=[r_vscr[0]])
                    k.dma("pool", kscr[:, :, I * 512:(I + 1) * 512].rearrange("h p t -> p h t"), kst[hi][:], rd=[r_kst[hi]], wr=[r_vscr[0]])

            run_pipeline(NT, [st_A, st_B])

        def attn_a2(l, j, pstack, o_all, r_o, lam_init):
            V = nc.vector
            lamt = sb("lamt", [P, 4 * DH + 8], F32, pstack)
            r_lam = Res("lam")
            k.dma("sp", lamt[:, 0:4 * DH], a_lam[j].partition_broadcast(P), wr=[r_lam])
            for q in range(2):
                k.op("dve", lambda: V.tensor_tensor(out=lamt[:, 2 * q * DH:(2 * q + 1) * DH], in0=lamt[:, 2 * q * DH:(2 * q + 1) * DH],
                                                    in1=lamt[:, (2 * q + 1) * DH:(2 * q + 2) * DH], op=ALU.mult), rd=[r_lam], wr=[r_lam])
                k.op("dve", lambda: V.tensor_reduce(out=lamt[:, 4 * DH + q:4 * DH + q + 1], in_=lamt[:, 2 * q * DH:(2 * q + 1) * DH],
                                                    axis=AX.X, op=ALU.add), rd=[r_lam], wr=[r_lam])
            k.op("act", lambda: nc.scalar.activation(out=lamt[:, 4 * DH + 2:4 * DH + 4], in_=lamt[:, 4 * DH:4 * DH + 2], func=AF.Exp),
                 rd=[r_lam], wr=[r_lam])
            nlam = lamt[:, 4 * DH + 4:4 * DH + 5]
            k.op("dve", lambda: V.tensor_tensor(out=nlam, in0=lamt[:, 4 * DH + 3:4 * DH + 4], in1=lamt[:, 4 * DH + 2:4 * DH + 3],
                                                op=ALU.subtract), rd=[r_lam], wr=[r_lam])
            k.op("dve", lambda: V.tensor_scalar(out=nlam, in0=nlam, scalar1=float(-lam_init), scalar2=None, op0=ALU.add), rd=[r_lam], wr=[r_lam])
            gsub = sb("gsub", [P, DV], F32, pstack)
            k.dma("sp", gsub[:], a_subln[j].partition_broadcast(P), wr=[r_lam])
            k.op("dve", lambda: V.tensor_scalar(out=gsub[:], in0=gsub[:], scalar1=float(1.0 - lam_init), scalar2=None, op0=ALU.mult),
                 rd=[r_lam], wr=[r_lam])
            QT = [sb("QT%d" % i, [P, 2, S], BF16, pstack) for i in range(2)]
            KT = [sb("KT%d" % i, [P, S], BF16, pstack) for i in range(2)]
            Va = [sb("Va%d" % i, [P, NT, 132], BF16, pstack) for i in range(2)]
            r_hd = [Res("hd0"), Res("hd1")]
            eT = [sb("eT%d" % i, [P, 2, 512], BF16, pstack) for i in range(3)]
            r_eT = [Res("eT%d" % i) for i in range(3)]
            of = sb("of", [P, 2, DV], F32, pstack)
            r_of = [Res("of0"), Res("of1")]
            fs = sb("fs", [P, 2, 8], F32, pstack)
            for i in range(2):
                k.op("pool", lambda: nc.gpsimd.memset(Va[i][:, :, 128:132], 1.0), wr=[r_hd[i]])
            ecnt = [0]
            scnt = [0]
            fcnt = [0]
            r_acc = [pbank[4 + i // 2] for i in range(8)]

            def load_head(h):
                bi = h % 2
                k.dma("sp", QT[bi][:, 0, :], qscr[h], rd=[r_vscr[0]], wr=[r_hd[bi]])
                k.dma("sp", QT[bi][:, 1, :], qscr[h], rd=[r_vscr[0]], wr=[r_hd[bi]])
                k.op("pool", lambda: nc.gpsimd.memset(QT[bi][64:128, 0, :], 0.0), wr=[r_hd[bi]])
                k.op("pool", lambda: nc.gpsimd.memset(QT[bi][0:64, 1, :], 0.0), wr=[r_hd[bi]])
                k.dma("sp", KT[bi][:], kscr[h], rd=[r_vscr[0]], wr=[r_hd[bi]])
                k.dma("sp", Va[bi][:, :, 0:DV], vtok[:, h * DV:(h + 1) * DV].rearrange("(j p) d -> p j d", p=P),
                      rd=[r_vscr[0]], wr=[r_hd[bi]])

            load_head(0)
            steps = [(h, I, jt) for h in range(NH) for I in range(NM) for jt in range(4 * I + 4)]
            st_info = {}

            def acc_ap(rp, c):
                return 4 + rp, c * 129

            def emit_S(n):
                h, I, jt = steps[n]
                bi = h % 2
                r = jt - 4 * I
                q0 = max(r, 0) * P
                sset = n % 2
                sb0 = 2 * sset
                for c in range(2):
                    k.op("pe", lambda: nc.tensor.matmul(ps[:, sb0 + c, q0:512], lhsT=KT[bi][:, jt * P:(jt + 1) * P],
                                                       rhs=QT[bi][:, c, I * 512 + q0:(I + 1) * 512], start=True, stop=True),
                         rd=[r_hd[bi]], wr=[pbank[sb0 + c]])
                ei = n % 3
                k.op("act", lambda: nc.scalar.activation(out=eT[ei][:, :, q0:512], in_=ps[:, sb0:sb0 + 2, q0:512], func=AF.Exp,
                                                        scale=float(DH ** -0.5)),
                     rd=[pbank[sb0], pbank[sb0 + 1]], wr=[r_eT[ei]])
                if r >= 0:
                    k.op("pool", lambda: nc.gpsimd.affine_select(out=eT[ei][:, :, q0:q0 + P], in_=eT[ei][:, :, q0:q0 + P],
                                                                 pattern=[[0, 2], [1, P]], compare_op=ALU.is_ge, fill=0.0, base=0,
                                                                 channel_multiplier=-1), rd=[r_eT[ei]], wr=[r_eT[ei]])

            def emit_V(n):
                h, I, jt = steps[n]
                bi = h % 2
                if I == 0 and jt == 0 and h + 1 < NH:
                    load_head(h + 1)
                r = jt - 4 * I
                ei = n % 3
                for rp in range(max(r, 0), 4):
                    for c in range(2):
                        bk, off = acc_ap(rp, c)
                        st = (jt == 0 and c == 0)
                        last = (jt == 4 * I + rp)
                        k.op("pe", lambda: nc.tensor.matmul(ps[:, bk, off:off + 129], lhsT=eT[ei][:, c, rp * P:(rp + 1) * P],
                                                           rhs=Va[bi][:, jt, 0:129], start=st, stop=last, skip_group_check=True),
                             rd=[r_eT[ei], r_hd[bi]], wr=[pbank[bk]], inc=(last and c == 1) or (rp == 3 and c == 1))
                if r >= 0:
                    t = 4 * I + r
                    fi = fcnt[0] % 2
                    fcnt[0] += 1
                    b0_, o0 = acc_ap(r, 0)
                    b1_, o1 = acc_ap(r, 1)
                    F = fs[:, fi, :]
                    rf = r_of[fi]
                    ra = pbank[b0_]
                    k.op("dve", lambda: V.reciprocal(out=F[:, 0:1], in_=ps[:, b0_, o0 + 128:o0 + 129]), rd=[ra], wr=[rf])
                    k.op("dve", lambda: V.reciprocal(out=F[:, 1:2], in_=ps[:, b1_, o1 + 128:o1 + 129]), rd=[ra], wr=[rf])
                    k.op("dve", lambda: V.tensor_tensor(out=F[:, 2:3], in0=F[:, 1:2], in1=nlam, op=ALU.mult), rd=[rf, r_lam], wr=[rf])
                    k.op("dve", lambda: V.tensor_scalar(out=of[:, fi, :], in0=ps[:, b0_, o0:o0 + 128], scalar1=F[:, 0:1], scalar2=None,
                                                        op0=ALU.mult), rd=[ra, rf], wr=[rf])
                    k.op("dve", lambda: V.scalar_tensor_tensor(out=of[:, fi, :], in0=ps[:, b1_, o1:o1 + 128], scalar=F[:, 2:3],
                                                               in1=of[:, fi, :], op0=ALU.mult, op1=ALU.add), rd=[ra, rf], wr=[rf])
                    k.op("pool", lambda: nc.gpsimd.tensor_tensor(out=sq[:, fi, :], in0=of[:, fi, :], in1=of[:, fi, :], op=ALU.mult),
                         rd=[rf], wr=[r_sq[fi]])
                    k.op("dve", lambda: V.tensor_reduce(out=F[:, 3:4], in_=sq[:, fi, :], axis=AX.X, op=ALU.add), rd=[r_sq[fi]], wr=[rf])
                    k.op("dve", lambda: V.tensor_scalar(out=F[:, 4:5], in0=F[:, 3:4], scalar1=1.0 / DV, scalar2=EPS, op0=ALU.mult, op1=ALU.add),
                         rd=[rf], wr=[rf])
                    k.op("pool", lambda: nc.gpsimd.tensor_tensor(out=F[:, 5:6], in0=F[:, 4:5], in1=nhalf[:], op=ALU.pow),
                         rd=[rf, r_const], wr=[rf])
                    k.op("dve", lambda: V.scalar_tensor_tensor(out=o_all[:, t, h * DV:(h + 1) * DV], in0=of[:, fi, :], scalar=F[:, 5:6],
                                                               in1=gsub[:], op0=ALU.mult, op1=ALU.mult), rd=[rf, r_lam], wr=[r_o[t // 4]])

            sq = sb("sq", [P, 2, DV], F32, pstack)
            r_sq = [Res("sq0"), Res("sq1")]
            pp = make_prepass(l, pstack, engines=("dve", "pool")) if SPARSE else None
            every = max(1, len(steps) // 100)
            LA = 1
            for n in range(len(steps) + LA):
                if n < len(steps):
                    emit_S(n)
                if n - LA >= 0:
                    emit_V(n - LA)
                if pp is not None and n % every == 0:
                    pp(1)
            if pp is not None:
                pp.flush()

        def attn_a3(l, j, pstack, o_all, r_o, src_ap, r_src):
            wo = sb("wo", [P, KC, D], BF16, pstack)
            r_wo = Res("wo")
            load_w_bf16(wo[:], a_wo[j], KC, D, r_dst=r_wo)
            prep = MoePrep(l, pstack, (2, 3, 4), depth=2, rdepth=8)
            xt = [sb("xt%d" % i, [P, D], F32, pstack) for i in range(4)]
            r_xt = [Res("xt%d" % i) for i in range(4)]
            oT = [sb("oT%d" % i, [P, KC, P], BF16, pstack) for i in range(2)]
            r_oT = [Res("oT0"), Res("oT1")]
            def st_X(t):
                xi = t % 4
                i2 = t % 2
                I = t // 4
                k.dma("sp", xt[xi][:], src_ap[t * P:(t + 1) * P, :], rd=[r_src[I]], wr=[r_xt[xi]])
                bT = 6 + (t % 2)
                pb = ps[:, bT, :].bitcast(BF16)
                for c in range(KC):
                    k.op("pe", lambda: nc.tensor.transpose(out=pb[:, c * P:(c + 1) * P], in_=o_all[:, t, c * P:(c + 1) * P],
                                                          identity=ident_b[:]), rd=[r_o[I], r_const], wr=[pbank[bT]], inc=(c == KC - 1))
                k.op("act", lambda: nc.scalar.copy(out=oT[i2][:], in_=pb.rearrange("p (c n) -> p c n", n=P)), rd=[pbank[bT]], wr=[r_oT[i2]])

            def st_X2(t):
                xi = t % 4
                i2 = t % 2
                I = t // 4
                for half in range(2):
                    bk = 0 + half
                    for c in range(KC):
                        k.op("pe", lambda: nc.tensor.matmul(ps[:, bk, :], lhsT=oT[i2][:, c, :], rhs=wo[:, c, half * 512:(half + 1) * 512],
                                                           start=(c == 0), stop=(c == KC - 1)), rd=[r_oT[i2], r_wo], wr=[pbank[bk]],
                             inc=(c == KC - 1))
                    k.op("dve", lambda: nc.vector.tensor_tensor(out=xt[xi][:, half * 512:(half + 1) * 512], in0=ps[:, bk, :],
                                                                in1=xt[xi][:, half * 512:(half + 1) * 512], op=ALU.add),
                         rd=[pbank[bk], r_xt[xi]], wr=[r_xt[xi]])
                k.dma("pool", xres[t * P:(t + 1) * P, :], xt[xi][:], rd=[r_xt[xi]], wr=[r_xres[I]])

            prep.split_logits = True
            run_pipeline(NT, [st_X, st_X2, lambda t: prep.p1a1(xt[t % 4][:], r_xt[t % 4], t),
                              lambda t: prep.p1a2(xt[t % 4][:], r_xt[t % 4], t), prep.p1b, prep.p1b2, prep.p2])

        jc = 0
        ja = 0
        r_xin = [Res("xin%d" % i) for i in range(NM)]
        for l, kind in enumerate(layer_kinds):
            src_ap, r_src = (x_in, r_xin) if l == 0 else (xres, r_xres)
            if kind == "conv":
                with ExitStack() as pstack:
                    conv_c1(l, jc, pstack, src_ap, r_src)
                    k.barrier()
                if debug == "c1":
                    break
                with ExitStack() as pstack:
                    conv_c2(l, jc, pstack, src_ap, r_src)
                    k.barrier()
                if debug == "c2":
                    break
                jc += 1
            else:
                if l == 0:
                    with ExitStack() as pstack:
                        attn_a0(pstack, src_ap, r_src)
                        k.barrier()
                if debug == "a0":
                    break
                with ExitStack() as pstack:
                    attn_a1(l, ja, pstack)
                    k.barrier()
                if debug in ("a1", "a1r", "a1x"):
                    break
                with ExitStack() as ostack:
                    o_all = sb("o_all", [P, NT, D], BF16, ostack)
                    r_o = [Res("o%d" % i) for i in range(NM)]
                    with ExitStack() as pstack:
                        attn_a2(l, ja, pstack, o_all, r_o, lam_inits[l])
                        k.barrier()
                    if debug == "a2":
                        break
                    with ExitStack() as pstack:
                        attn_a3(l, ja, pstack, o_all, r_o, src_ap, r_src)
                        k.barrier()
                ja += 1
            if debug in ("a1", "a2", "a3"):
                break
            with ExitStack() as pstack:
                if SPARSE:
                    moe_sparse(l, pstack)
                else:
                    moe_dense(l, pstack)
                k.barrier()
            if debug == "moe":
                break
            with ExitStack() as pstack:
                last = (l == L - 1)
                ple_phase(l, pstack, last, None if last else layer_kinds[l + 1])
                k.barrier()
        k.final_wait("sp")
        print("ninst", k.ninst, "cnt", k.cnt)
    return nc


_NC_CACHE = {}


def _in_map(inputs, b, S, L):
    f = lambda a: np.ascontiguousarray(a, dtype=np.float32)
    m = {
        "x": f(inputs["x"][b]),
        "p": f(inputs["p"][:, b]),
        "positions": np.ascontiguousarray(inputs["positions"][b].reshape(S // P, P).astype(np.int32)),
    }
    for name in ("norm_mix", "norm_ffn", "conv_w_pw1", "conv_b_pw1", "conv_w_dw", "conv_b_dw", "conv_ln_g", "conv_ln_b",
                 "conv_w_pw2", "conv_b_pw2", "da_w_qkv", "da_subln", "da_w_o", "moe_w_rg", "moe_b_rg",
                 "ple_norm", "ple_w_gate", "ple_w_proj", "final_norm"):
        m[name] = f(inputs[name])
    m["moe_w_re"] = f(inputs["moe_w_re"])
    m["da_lambda"] = f(inputs["da_lambda"]).reshape(-1, 4 * DH)
    m["moe_b_re"] = f(inputs["moe_b_re"]).reshape(L, NG * NE)
    m["moe_w_gate"] = f(inputs["moe_w_gate"]).reshape(L, NEXP, D, FE)
    m["moe_w_up"] = f(inputs["moe_w_up"]).reshape(L, NEXP, D, FE)
    m["moe_w_down"] = f(inputs["moe_w_down"]).reshape(L, NEXP, FE, D)
    return m


def run(inputs, layer_kinds, n_cores=None, lam_i0=0, debug=None):
    B, S, _ = inputs["x"].shape
    L = len(layer_kinds)
    lam_inits = [0.8 - 0.6 * math.exp(-0.3 * (i + lam_i0)) for i in range(L)]
    key = (S, tuple(layer_kinds), debug)
    if key not in _NC_CACHE:
        _NC_CACHE[key] = build_program(S, layer_kinds, lam_inits, debug)
    nc = _NC_CACHE[key]
    n = B if n_cores is None else n_cores
    in_maps = [_in_map(inputs, b, S, L) for b in range(n)]
    res = run_bass_kernel_spmd(nc, in_maps, core_ids=list(range(n)))
    if debug:
        return res.results
    return np.stack([np.asarray(r["y"], dtype=np.float32) for r in res.results], axis=0)


def kernel(**inputs):
    inputs = {k_: np.asarray(v) for k_, v in inputs.items()}
    return run(inputs, ["conv", "attn"])
```
